# Optimizing a Trainium2 kernel written in Bass

```python
import jax, jax.numpy as jnp
from jax import lax
import numpy as np

D_MODEL = 1024
BATCH = 2
SEQ = 8192
DEPTH = 1

ATT_HEADS = 8
ATT_KV_HEADS = 2
HEAD_DIM = 64
ATT_WIDTH = ATT_HEADS * HEAD_DIM
KV_WIDTH = ATT_KV_HEADS * HEAD_DIM
WINDOW = 128
ATT_BLOCK = 128
ROPE_DIM = HEAD_DIM // 4
ROPE_THETA = 500000.0
SSD_HEADS = 8
SSD_HEAD_DIM = 64
SSD_WIDTH = SSD_HEADS * SSD_HEAD_DIM
SSD_GROUPS = 2
SSD_STATE = 128
CONV_K = 4
CHUNK = 128
XBC_WIDTH = SSD_WIDTH + 2 * SSD_GROUPS * SSD_STATE
MIX_WIDTH = ATT_WIDTH + SSD_WIDTH
IN_WIDTH = ATT_WIDTH + 2 * KV_WIDTH + SSD_WIDTH + XBC_WIDTH + SSD_HEADS
N_GROUPS = 4
EXPERTS_PER_GROUP = 8
N_EXPERTS = N_GROUPS * EXPERTS_PER_GROUP
TOP_K = 2
D_EXPERT = 256
EPS = 1e-6

kernel_name = "hymba_ssd_swa_sink_hmoe_adaln_layer"


def rms_norm(x, w):
    xf = x.astype(jnp.float32)
    y = xf * lax.rsqrt(jnp.mean(xf * xf, axis=-1, keepdims=True) + EPS)
    return (y * w.astype(jnp.float32)).astype(x.dtype)


def partial_rope(x, positions):
    half = ROPE_DIM // 2
    inv_freq = jnp.power(ROPE_THETA, -jnp.arange(half, dtype=jnp.float32) * 2.0 / ROPE_DIM)
    ang = positions.astype(jnp.float32)[..., None] * inv_freq
    cos = jnp.cos(ang)[:, :, None, :]
    sin = jnp.sin(ang)[:, :, None, :]
    xr = x[..., :ROPE_DIM].astype(jnp.float32)
    x1, x2 = xr[..., :half], xr[..., half:]
    rot = jnp.concatenate([x1 * cos - x2 * sin, x2 * cos + x1 * sin], axis=-1).astype(x.dtype)
    return jnp.concatenate([rot, x[..., ROPE_DIM:]], axis=-1)


def sliding_window_attention(q, k, v, sinks):
    b, s = q.shape[:2]
    nb = s // ATT_BLOCK
    grp = ATT_HEADS // ATT_KV_HEADS
    qb = q.reshape(b, nb, ATT_BLOCK, ATT_KV_HEADS, grp, HEAD_DIM)

    def band(t):
        tb = t.reshape(b, nb, ATT_BLOCK, ATT_KV_HEADS, HEAD_DIM)
        prev = jnp.pad(tb[:, :-1], ((0, 0), (1, 0), (0, 0), (0, 0), (0, 0)))
        return jnp.concatenate([prev, tb], axis=2)

    kb, vb = band(k), band(v)
    scores = jnp.einsum('bnqkgd,bnskd->bnkgqs', qb, kb).astype(jnp.float32) * (HEAD_DIM ** -0.5)
    qi = jnp.arange(ATT_BLOCK)[:, None]
    si = jnp.arange(2 * ATT_BLOCK)[None, :]
    diff = qi + ATT_BLOCK - si
    band_mask = (diff >= 0) & (diff < WINDOW)
    blk = jnp.arange(nb)[:, None, None]
    valid = band_mask[None] & ((blk > 0) | (si[None] >= ATT_BLOCK))
    scores = jnp.where(valid[None, :, None, None], scores, -jnp.inf)
    sink = sinks.astype(jnp.float32).reshape(ATT_KV_HEADS, grp)[None, None, :, :, None, None]
    m = jnp.maximum(jnp.max(scores, axis=-1, keepdims=True), sink)
    p = jnp.exp(scores - m)
    probs = (p / (jnp.sum(p, axis=-1, keepdims=True) + jnp.exp(sink - m))).astype(v.dtype)
    out = jnp.einsum('bnkgqs,bnskd->bnqkgd', probs, vb)
    return out.reshape(b, s, ATT_WIDTH)


def causal_depthwise_conv(x, w, bias):
    ch = x.shape[-1]
    y = lax.conv_general_dilated(x, w[:, None, :], window_strides=(1,),
                                 padding=[(CONV_K - 1, 0)],
                                 dimension_numbers=('NWC', 'WIO', 'NWC'),
                                 feature_group_count=ch)
    return y + bias


def ssd_scan(x, dt, A, B, C):
    b, s, h, p = x.shape
    n = B.shape[-1]
    nc = s // CHUNK
    hpg = h // SSD_GROUPS
    f32 = jnp.float32
    xdt = (x.astype(f32) * dt[..., None]).reshape(b, nc, CHUNK, h, p)
    a = (dt * A).reshape(b, nc, CHUNK, h)
    a_cum = jnp.cumsum(a, axis=2)
    Bc = B.astype(f32).reshape(b, nc, CHUNK, SSD_GROUPS, n)
    Cc = C.astype(f32).reshape(b, nc, CHUNK, SSD_GROUPS, n)
    seg = a_cum[:, :, :, None, :] - a_cum[:, :, None, :, :]
    causal = jnp.tril(jnp.ones((CHUNK, CHUNK), dtype=bool))
    decay = jnp.exp(jnp.where(causal[None, None, :, :, None], seg, -jnp.inf))
    cb = jnp.repeat(jnp.einsum('bclgn,bcsgn->bclsg', Cc, Bc), hpg, axis=-1)
    y_diag = jnp.einsum('bclsh,bcshp->bclhp', cb * decay, xdt)
    B_h = jnp.repeat(Bc, hpg, axis=3)
    decay_to_end = jnp.exp(a_cum[:, :, -1:, :] - a_cum)
    states = jnp.einsum('bclhn,bclh,bclhp->bchpn', B_h, decay_to_end, xdt)
    chunk_decay = jnp.exp(a_cum[:, :, -1, :])

    def step(carry, inp):
        st, dec = inp
        return carry * dec[:, :, None, None] + st, carry

    init = jnp.zeros((b, h, p, n), f32)
    _, prev = lax.scan(step, init, (jnp.moveaxis(states, 1, 0), jnp.moveaxis(chunk_decay, 1, 0)))
    prev = jnp.moveaxis(prev, 0, 1)
    C_h = jnp.repeat(Cc, hpg, axis=3)
    y_off = jnp.einsum('bclhn,bchpn,bclh->bclhp', C_h, prev, jnp.exp(a_cum))
    return (y_diag + y_off).reshape(b, s, h, p).astype(x.dtype)


def hybrid_mixer(h, positions, w_in, conv_w, conv_b, dt_bias, a_log, d_skip, ssd_norm_w,
                 q_norm_w, k_norm_w, sinks, w_out):
    b, s, _ = h.shape
    proj = h @ w_in
    cuts = [ATT_WIDTH, ATT_WIDTH + KV_WIDTH, ATT_WIDTH + 2 * KV_WIDTH,
            ATT_WIDTH + 2 * KV_WIDTH + SSD_WIDTH,
            ATT_WIDTH + 2 * KV_WIDTH + SSD_WIDTH + XBC_WIDTH]
    q, k, v, z, xbc, dt = jnp.split(proj, cuts, axis=-1)
    q = partial_rope(rms_norm(q.reshape(b, s, ATT_HEADS, HEAD_DIM), q_norm_w), positions)
    k = partial_rope(rms_norm(k.reshape(b, s, ATT_KV_HEADS, HEAD_DIM), k_norm_w), positions)
    v = v.reshape(b, s, ATT_KV_HEADS, HEAD_DIM)
    att = sliding_window_attention(q, k, v, sinks)
    xbc = jax.nn.silu(causal_depthwise_conv(xbc, conv_w, conv_b))
    xs, Bm, Cm = jnp.split(xbc, [SSD_WIDTH, SSD_WIDTH + SSD_GROUPS * SSD_STATE], axis=-1)
    xs = xs.reshape(b, s, SSD_HEADS, SSD_HEAD_DIM)
    Bm = Bm.reshape(b, s, SSD_GROUPS, SSD_STATE)
    Cm = Cm.reshape(b, s, SSD_GROUPS, SSD_STATE)
    dt = jax.nn.softplus(dt.astype(jnp.float32) + dt_bias.astype(jnp.float32))
    A = -jnp.exp(a_log.astype(jnp.float32))
    y = ssd_scan(xs, dt, A, Bm, Cm) + d_skip[:, None] * xs
    y = y.reshape(b, s, SSD_WIDTH) * jax.nn.silu(z)
    y = rms_norm(y.reshape(b, s, SSD_GROUPS, SSD_WIDTH // SSD_GROUPS),
                 ssd_norm_w.reshape(SSD_GROUPS, SSD_WIDTH // SSD_GROUPS)).reshape(b, s, SSD_WIDTH)
    return jnp.concatenate([att, y], axis=-1) @ w_out


def hierarchical_moe(h, w_group, b_group, w_expert, b_expert, w_gate, w_up, w_down):
    b, s, d = h.shape
    t = h.reshape(b * s, d)
    g_probs = jax.nn.softmax((t @ w_group + b_group).astype(jnp.float32), axis=-1)
    g_p, g_idx = lax.top_k(g_probs, 1)
    e_logits = (t @ w_expert + b_expert).astype(jnp.float32).reshape(-1, N_GROUPS, EXPERTS_PER_GROUP)
    e_sel = jnp.take_along_axis(e_logits, g_idx[:, :, None], axis=1)[:, 0]
    e_p, e_idx = lax.top_k(jax.nn.softmax(e_sel, axis=-1), TOP_K)
    e_p = e_p / jnp.sum(e_p, axis=-1, keepdims=True)
    group_w = jax.nn.one_hot(g_idx[:, 0], N_GROUPS, dtype=jnp.float32) * g_p
    exp_w = jnp.einsum('tk,tke->te', e_p, jax.nn.one_hot(e_idx, EXPERTS_PER_GROUP, dtype=jnp.float32))
    combine = (group_w[:, :, None] * exp_w[:, None, :]).reshape(-1, N_EXPERTS).astype(h.dtype)
    hg = jnp.einsum('td,edf->tef', t, w_gate)
    hu = jnp.einsum('td,edf->tef', t, w_up)
    act = jax.nn.silu(hg) * hu * combine[:, :, None]
    y = jnp.einsum('tef,efd->td', act, w_down)
    return y.reshape(b, s, d)


def setup_inputs(seed: int = 0) -> dict:
    key = jax.random.key(seed)
    ks = jax.random.split(key, 26)
    f32 = jnp.float32
    nrm = lambda k, shape, scale: jax.random.normal(k, shape, f32) * scale
    dt0 = jnp.exp(jax.random.uniform(ks[9], (SSD_HEADS,), f32, np.log(1e-3), np.log(1e-1)))
    return {
        "x": nrm(ks[0], (BATCH, SEQ, D_MODEL), 1.0),
        "c": nrm(ks[1], (BATCH, D_MODEL), 1.0),
        "positions": jnp.broadcast_to(jnp.arange(SEQ, dtype=jnp.int32)[None, :], (BATCH, SEQ)),
        "norm1_w": 1.0 + nrm(ks[2], (D_MODEL,), 0.02),
        "norm2_w": 1.0 + nrm(ks[3], (D_MODEL,), 0.02),
        "w_ada": nrm(ks[4], (D_MODEL, 6 * D_MODEL), 0.5 * D_MODEL ** -0.5),
        "b_ada": nrm(ks[5], (6 * D_MODEL,), 0.02),
        "w_in": nrm(ks[6], (D_MODEL, IN_WIDTH), D_MODEL ** -0.5),
        "conv_w": nrm(ks[7], (CONV_K, XBC_WIDTH), CONV_K ** -0.5),
        "conv_b": nrm(ks[8], (XBC_WIDTH,), 0.02),
        "dt_bias": dt0 + jnp.log(-jnp.expm1(-dt0)),
        "a_log": jnp.log(jax.random.uniform(ks[10], (SSD_HEADS,), f32, 1.0, 16.0)),
        "d_skip": 1.0 + nrm(ks[11], (SSD_HEADS,), 0.1),
        "ssd_norm_w": 1.0 + nrm(ks[12], (SSD_WIDTH,), 0.02),
        "q_norm_w": 1.0 + nrm(ks[13], (HEAD_DIM,), 0.02),
        "k_norm_w": 1.0 + nrm(ks[14], (HEAD_DIM,), 0.02),
        "sinks": nrm(ks[15], (ATT_HEADS,), 0.5),
        "w_out": nrm(ks[16], (MIX_WIDTH, D_MODEL), MIX_WIDTH ** -0.5),
        "w_group": nrm(ks[17], (D_MODEL, N_GROUPS), D_MODEL ** -0.5),
        "b_group": nrm(ks[18], (N_GROUPS,), 0.01),
        "w_expert": nrm(ks[19], (D_MODEL, N_EXPERTS), D_MODEL ** -0.5),
        "b_expert": nrm(ks[20], (N_EXPERTS,), 0.01),
        "w_gate": nrm(ks[21], (N_EXPERTS, D_MODEL, D_EXPERT), D_MODEL ** -0.5),
        "w_up": nrm(ks[22], (N_EXPERTS, D_MODEL, D_EXPERT), D_MODEL ** -0.5),
        "w_down": nrm(ks[23], (N_EXPERTS, D_EXPERT, D_MODEL), D_EXPERT ** -0.5),
    }


def reference(x, c, positions, norm1_w, norm2_w, w_ada, b_ada, w_in, conv_w, conv_b,
              dt_bias, a_log, d_skip, ssd_norm_w, q_norm_w, k_norm_w, sinks, w_out,
              w_group, b_group, w_expert, b_expert, w_gate, w_up, w_down):
    mod = jax.nn.silu(c) @ w_ada + b_ada
    shift1, scale1, gate1, shift2, scale2, gate2 = [m[:, None, :] for m in jnp.split(mod, 6, axis=-1)]
    for _ in range(DEPTH):
        h = rms_norm(x, norm1_w) * (1.0 + scale1) + shift1
        x = x + gate1 * hybrid_mixer(h, positions, w_in, conv_w, conv_b, dt_bias, a_log, d_skip,
                                     ssd_norm_w, q_norm_w, k_norm_w, sinks, w_out)
        h = rms_norm(x, norm2_w) * (1.0 + scale2) + shift2
        x = x + gate2 * hierarchical_moe(h, w_group, b_group, w_expert, b_expert, w_gate, w_up, w_down)
    return x
```

```python
import contextlib
import numpy as np
import ml_dtypes
import concourse.bass as bass
import concourse.mybir as mybir
from concourse.bass_utils import run_bass_kernel_spmd

F32 = mybir.dt.float32
BF16 = mybir.dt.bfloat16
I32 = mybir.dt.int32
AF = mybir.ActivationFunctionType
ALU = mybir.AluOpType
AX = mybir.AxisListType

NCORES = 8
D = 1024
NSLOT = 64
NMAIN = 16
NPRE = NSLOT - NMAIN
HALO = NPRE - 1
INW = 2312
EPS = 1e-6
NEXP = 32
TWO_PI = 6.283185307179586
C1 = 6.28125
C2 = TWO_PI - C1

DEBUG_X2 = False
N_EXPERTS_RUN = NEXP
N_PREFIX_SKIP = 0
STOP_AFTER = 0
USE_POW = True
STAGE_LIMIT = 99
MAIN_PAIRS = True
HOP = 0.8
NO_CC = False
CC_TEST = False


class Buf:
    __slots__ = ("name", "w", "r", "psum")

    def __init__(self, name="", psum=False):
        self.name = name
        self.w = []
        self.r = []
        self.psum = psum


class Tile:
    def __init__(self, t, name=""):
        self.t = t
        self.b = Buf(name)

    def __getitem__(self, idx):
        return self.t[idx]


class View:
    def __init__(self, ap, buf):
        self.t = ap
        self.b = buf

    def __getitem__(self, idx):
        return self.t[idx]


class Op:
    __slots__ = ("id", "eng", "fn", "deps", "dma", "cost", "xfer", "tok", "succ", "cc")

    def __init__(self, id, eng, fn, deps, dma, cost, xfer):
        self.id = id; self.eng = eng; self.fn = fn; self.deps = deps; self.dma = dma
        self.cost = cost; self.xfer = xfer; self.tok = None; self.succ = False; self.cc = False


class Probe:
    def __init__(self):
        self.name = None; self.args = (); self.kw = {}

    def __getattr__(self, name):
        def f(*args, **kw):
            self.name = name; self.args = args; self.kw = kw
            return self
        return f


def _fsz(ap):
    v = ap.free_size
    return v() if callable(v) else v


def _nb(ap):
    v = ap.nbytes
    return v() if callable(v) else v


class Sched:
    ENGS = ("pe", "act", "dve", "pool", "sp")
    NDSEM = 12
    WINDOW = 320

    def __init__(self, nc):
        self.nc = nc
        self.all = []
        self.fence = []
        self.leaves = set()
        self.reorder = True

    @staticmethod
    def est(eng, n, fp32):
        if eng == "pe":
            c = 0.11 if n <= 128 else n / 2400.0 + 0.02
            return c * (4 if fp32 else 1)
        if eng == "act":
            return 0.25 + n / 1200.0
        if eng == "dve":
            return 0.16 + n / 960.0
        if eng == "pool":
            return 0.22 + n / 480.0
        return 0.1

    def op(self, eng, fn, reads=(), writes=(), dma=False, n=64, fp32=False, nbytes=0, cc=False):
        reads = [x.b if isinstance(x, (Tile, View)) else x for x in reads]
        writes = [x.b if isinstance(x, (Tile, View)) else x for x in writes]
        deps = set(self.fence)
        for b in reads:
            deps.update(b.w)
            if b.psum:
                deps.update(i for i in b.r if self.all[i].eng != eng)
        for b in writes:
            deps.update(b.w)
            deps.update(b.r)
        oid = len(self.all)
        pr = Probe()
        fn(pr)
        if cc:
            cost = 2.0; xfer = 30.0
        elif dma:
            nbytes = _nb(pr.kw["out"])
            cost = 1.2 if eng == "pool" else 0.15
            xfer = 2.0 + nbytes / 150e3
        else:
            if pr.name == "matmul":
                n = _fsz(pr.kw["rhs"]); fp32 = pr.kw["rhs"].dtype == F32
            elif pr.name == "transpose":
                n = 128
            else:
                o_ = pr.kw.get("out", pr.args[0] if pr.args else None)
                n = _fsz(o_) if o_ is not None else 64
                if eng == "dve" and pr.name in ("tensor_tensor", "scalar_tensor_tensor"):
                    a0 = pr.kw.get("in0"); a1 = pr.kw.get("in1")
                    if a0 is not None and a1 is not None and a0.dtype == F32 and a1.dtype == F32 \
                            and str(a0.space) == str(a1.space):
                        n = int(n * 1.0)
            cost = self.est(eng, n, fp32)
            if eng == "pool" and pr.kw.get("op", None) == ALU.pow:
                cost = 1.2
            xfer = 0.0
        o = Op(oid, eng, fn, deps, dma or cc, cost, xfer)
        o.cc = cc
        self.all.append(o)
        for d in deps:
            if not self.all[d].succ:
                self.all[d].succ = True
                self.leaves.discard(d)
        self.leaves.add(oid)
        for b in writes:
            b.w = [oid]
            b.r = []
        for b in reads:
            if b not in writes:
                b.r.append(oid)
        return oid

    def barrier(self, fn):
        deps = set(self.leaves) | set(self.fence)
        oid = len(self.all)
        o = Op(oid, "dve", fn, deps, False, 0.1, 0.0)
        self.all.append(o)
        for d in deps:
            self.all[d].succ = True
        self.leaves = {oid}
        self.fence = [oid]

    def schedule(self):
        ops = self.all
        per = {e: [o.id for o in ops if o.eng == e] for e in self.ENGS}
        if not self.reorder:
            return per
        head = {e: 0 for e in self.ENGS}
        done = [False] * len(ops)
        fin = [0.0] * len(ops)
        free = {e: 0.0 for e in self.ENGS}
        dma_free = 0.0
        order = {e: [] for e in self.ENGS}
        remaining = len(ops)
        INF = 1e30
        while remaining:
            best = None
            for e in self.ENGS:
                lst = per[e]
                h = head[e]
                while h < len(lst) and done[lst[h]]:
                    h += 1
                head[e] = h
                cnt = 0
                k = h
                while k < len(lst) and cnt < self.WINDOW:
                    oid = lst[k]
                    k += 1
                    if done[oid]:
                        continue
                    cnt += 1
                    o = ops[oid]
                    rdy = 0.0
                    ok = True
                    for d in o.deps:
                        if not done[d]:
                            ok = False
                            break
                        fd = fin[d] - (HOP if (ops[d].eng == e and not ops[d].dma) else 0.0)
                        if fd > rdy:
                            rdy = fd
                    if not ok:
                        continue
                    st = rdy if rdy > free[e] else free[e]
                    key = (st, oid)
                    if best is None or key < best[0]:
                        best = (key, e, oid)
                    if rdy <= free[e]:
                        break
            assert best is not None, "scheduler stuck (cyclic deps?)"
            (st, _), e, oid = best
            o = ops[oid]
            done[oid] = True
            remaining -= 1
            free[e] = st + o.cost
            if o.dma:
                t0 = max(st + o.cost, dma_free)
                dma_free = t0 + (o.xfer - 2.0)
                fin[oid] = t0 + o.xfer
            else:
                fin[oid] = st + o.cost + HOP
            order[e].append(oid)
        self.sim_makespan = max(fin) if fin else 0.0
        return order

    def run(self):
        nc = self.nc
        ops = self.all
        order = self.schedule()
        cnt = {}
        rr = {}
        prevdma = {}
        for e in self.ENGS:
            for oid in order[e]:
                o = ops[oid]
                if o.cc:
                    s = "cc%d" % oid
                    cnt[s] = 1
                    o.tok = (s, 1)
                    prevdma[oid] = (s, 0)
                    continue
                if o.dma:
                    k = rr.get(e, 0)
                    rr[e] = (k + 1) % self.NDSEM
                    s = "d_%s_%d" % (e, k)
                    prevdma[oid] = (s, cnt.get(s, 0))
                    cnt[s] = cnt.get(s, 0) + 16
                else:
                    s = e
                    cnt[s] = cnt.get(s, 0) + 1
                o.tok = (s, cnt[s])
        streams = {}
        for e in self.ENGS:
            waited = {}
            st = []
            for oid in order[e]:
                o = ops[oid]
                need = {}
                for d in o.deps:
                    s, v = ops[d].tok
                    if v > need.get(s, 0):
                        need[s] = v
                if o.dma and prevdma[oid][1]:
                    s, v = prevdma[oid]
                    need[s] = max(need.get(s, 0), v)
                waits = []
                for s, v in need.items():
                    if e == "pe" and s == "pe":
                        continue
                    if waited.get(s, 0) >= v:
                        continue
                    waited[s] = v
                    waits.append((s, v))
                st.append((waits, o))
            if e == "sp":
                waits = [(s, v) for s, v in cnt.items() if waited.get(s, 0) < v]
                st.append((waits, None))
            streams[e] = st
        self._check_deadlock(streams)
        with contextlib.ExitStack() as stck:
            sems = {n: stck.enter_context(nc.semaphore("s_" + n)) for n in sorted(cnt)}
            block = stck.enter_context(nc.Block())

            def play(engname):
                def body(eh):
                    for waits, o in streams[engname]:
                        for (s, v) in waits:
                            eh.wait_ge(sems[s], v)
                        if o is None:
                            continue
                        ins = o.fn(eh)
                        if o.cc:
                            ins.then_inc(sems[o.tok[0]])
                        else:
                            ins.then_inc(sems[o.tok[0]], 16 if o.dma else 1)
                return body

            block.tensor(play("pe"))
            block.scalar(play("act"))
            block.vector(play("dve"))
            block.gpsimd(play("pool"))
            block.sync(play("sp"))

    def _check_deadlock(self, streams):
        val = {}
        pos = {e: 0 for e in self.ENGS}
        progress = True
        while progress:
            progress = False
            for e in self.ENGS:
                st = streams[e]
                while pos[e] < len(st):
                    waits, o = st[pos[e]]
                    if any(val.get(s, 0) < v for s, v in waits):
                        break
                    if o is not None:
                        val[o.tok[0]] = val.get(o.tok[0], 0) + (1 if o.cc else (16 if o.dma else 1))
                    pos[e] += 1
                    progress = True
        for e in self.ENGS:
            assert pos[e] == len(streams[e]), "DEADLOCK in engine %s at %d/%d" % (e, pos[e], len(streams[e]))


def build_program():
    nc = bass.Bass("TRN2", target_bir_lowering=False, dynamic_dma_scratch_size=4096)

    def din(name, shape, dt=F32):
        return nc.dram_tensor(name, list(shape), dt, kind="ExternalInput").ap()

    xe = din("xe", [NSLOT * 128, D])
    flags_d = din("flags", [128, NSLOT])
    posi_d = din("posi", [128, NMAIN + 1], I32)
    cvec_d = din("cvec", [128, 8])

    cf32_d = din("cf32", [128, 3 * 128 + 8])
    cbf_d = din("cbf", [128, 3 * 128], BF16)
    norm1_w = din("norm1_w", [D]); norm2_w = din("norm2_w", [D])
    w_ada = din("w_ada", [D, 6 * D]); b_ada = din("b_ada", [6 * D])
    w_in = din("w_in", [D, INW])
    conv_w = din("conv_w", [4, D]); conv_b = din("conv_b", [D])
    dt_bias = din("dt_bias", [8]); a_log = din("a_log", [8]); d_skip = din("d_skip", [8])
    ssd_norm_w = din("ssd_norm_w", [512])
    q_norm_w = din("q_norm_w", [64]); k_norm_w = din("k_norm_w", [64])
    sinks = din("sinks", [8])
    w_out = din("w_out", [D, D])
    w_group = din("w_group", [D, 4]); b_group = din("b_group", [4])
    w_expert = din("w_expert", [D, 32]); b_expert = din("b_expert", [32])
    w_gate = din("w_gate", [N_EXPERTS_RUN, D, 256]); w_up = din("w_up", [N_EXPERTS_RUN, D, 256])
    w_down = din("w_down", [N_EXPERTS_RUN, 256, D])
    out_d = nc.dram_tensor("out", [NMAIN * 128, D], F32, kind="ExternalOutput").ap()
    if DEBUG_X2:
        dbg_d = nc.dram_tensor("dbg", [NMAIN * 128, D], F32, kind="ExternalOutput").ap()

    S = Sched(nc)
    op = S.op

    with contextlib.ExitStack() as root:
        def sb(stack, name, shape, dt=F32):
            return Tile(stack.enter_context(nc.sbuf_tensor("sb_" + name, list(shape), dt)), name)

        banks = [Tile(root.enter_context(nc.psum_tensor("bank%d" % i, [128, 512], F32)), "bank%d" % i)
                 for i in range(8)]
        for bk_ in banks:
            bk_.b.psum = True

        def bfv(bank):
            return bank.t[:].bitcast(BF16)

        xres = [None] * NMAIN
        xres_t = root.enter_context(nc.sbuf_tensor("sb_xres", [128, NMAIN, D], F32))
        xres_b = [Buf("xres%d" % j) for j in range(NMAIN)]
        cf32 = sb(root, "cf32", [128, 3 * 128 + 8])
        cbf = sb(root, "cbf", [128, 3 * 128], BF16)
        flags = sb(root, "flags", [128, NSLOT])
        neghalf = sb(root, "neghalf", [128, 32])
        TRI = cf32[:, 0:128]; SLM = cf32[:, 128:256]; ONES = cf32[:, 256:384]; INVF = cf32[:, 384:392]
        IDENT = cbf[:, 0:128]; MASKC = cbf[:, 128:256]; MASKP = cbf[:, 256:384]

        modscr = nc.dram_tensor("modscr", [128, 3 * D], F32, kind="Internal").ap()

        op("sp", lambda e: e.dma_start(out=cf32[:], in_=cf32_d), writes=[cf32], dma=True)
        op("sp", lambda e: e.dma_start(out=cbf[:], in_=cbf_d), writes=[cbf], dma=True)
        op("sp", lambda e: e.dma_start(out=flags[:], in_=flags_d), writes=[flags], dma=True)
        op("dve", lambda e: e.memset(neghalf[:, 0:16], -0.5), writes=[neghalf])

        p1 = root.enter_context(contextlib.ExitStack())
        mod1 = sb(p1, "mod1", [128, 2 * D])
        SHIFT1 = mod1[:, 0:1024]; G1 = mod1[:, 1024:2048]
        win = sb(p1, "win", [128, 8, INW], BF16)
        wout = sb(p1, "wout", [128, 8, D], BF16)
        convdiag = sb(p1, "convdiag", [128, 32, 128], BF16)
        cbrow = sb(p1, "cbrow", [1, D], BF16)
        onesrow = sb(p1, "onesrow", [1, 256], BF16)
        dI = sb(p1, "dI", [128, 8, 128], BF16)
        small = sb(p1, "small", [128, 8 * 6])
        DTB = small[:, 0:8]; ABC = small[:, 8:16]; DSK = small[:, 16:24]; SNK = small[:, 24:32]
        ESINK = small[:, 32:40]
        negc = sb(p1, "negc", [128, 1])
        snw = sb(p1, "snw", [128, 512])
        qkw = sb(p1, "qkw", [128, 2, 64])
        cossin = sb(p1, "cossin", [128, 2, NMAIN + 1, 8])
        maskp0 = sb(p1, "maskp0", [128, 128], BF16)
        negm = sb(p1, "negm", [128, 3, 4, 128], BF16)

        with contextlib.ExitStack() as p0:
            mod = sb(p0, "mod", [128, 6 * D])
            GATE1 = mod[:, 2048:3072]
            cvec = sb(p0, "cvec", [128, 8])
            screp = sb(p0, "screp", [128, 8, 128])
            wst = [sb(p0, "wst%d" % i, [128, 8, 256]) for i in range(2)]
            nbc = sb(p0, "nbc", [128, D])
            cwT = sb(p0, "cwT", [128, 4, 8])
            cbst = sb(p0, "cbst", [1, D])
            posi = sb(p0, "posi", [128, NMAIN + 1], I32)
            posf = sb(p0, "posf", [128, NMAIN + 1])
            ang = sb(p0, "ang", [128, NMAIN + 1, 8])
            kk = sb(p0, "kk", [128, NMAIN + 1, 8])
            kki = sb(p0, "kki", [128, NMAIN + 1, 8], I32)
            mx = sb(p0, "mx", [128, 2])

            op("sp", lambda e: e.dma_start(out=cvec[:], in_=cvec_d), writes=[cvec], dma=True)
            op("sp", lambda e: e.dma_start(out=mod[:], in_=b_ada.partition_broadcast(128)), writes=[mod], dma=True)
            win_src = w_in.rearrange("(p k) n -> p k n", k=8)
            for lo, hi in ((0, 1156), (1156, INW)):
                op("pool", lambda e, lo=lo, hi=hi: e.dma_start(out=win[:, :, lo:hi], in_=win_src[:, :, lo:hi]),
                   writes=[win], dma=True)
            op("pool", lambda e: e.dma_start(out=wout[:], in_=w_out.rearrange("(p k) n -> p k n", k=8)),
               writes=[wout], dma=True)

            op("act", lambda e: e.activation(out=cvec[:], in_=cvec[:], func=AF.Silu), reads=[cvec], writes=[cvec])
            op("dve", lambda e: e.tensor_copy(out=screp[:], in_=cvec[:, :, None].to_broadcast([128, 8, 128])),
               reads=[cvec], writes=[screp])
            wada_src = w_ada.rearrange("(p k) n -> p k n", k=8)
            NB = 24
            for nb in range(NB):
                ws = wst[nb % 2]
                bk = banks[nb % 2]
                op("sp", lambda e, ws=ws, nb=nb: e.dma_start(out=ws[:], in_=wada_src[:, :, nb * 256:(nb + 1) * 256]),
                   writes=[ws], dma=True)
                for kt in range(8):
                    op("pe", lambda e, ws=ws, bk=bk, kt=kt: e.matmul(bk[:, 0:256], lhsT=screp[:, kt, :], rhs=ws[:, kt, :],
                                                                     start=(kt == 0), stop=(kt == 7)),
                       reads=[screp, ws], writes=[bk])
                op("dve", lambda e, bk=bk, nb=nb: e.tensor_tensor(out=mod[:, nb * 256:(nb + 1) * 256], in0=bk[:, 0:256],
                                                                 in1=mod[:, nb * 256:(nb + 1) * 256], op=ALU.add),
                   reads=[bk], writes=[mod])
            op("sp", lambda e: e.dma_start(out=nbc[:], in_=norm1_w.partition_broadcast(128)), writes=[nbc], dma=True)
            op("dve", lambda e: e.scalar_tensor_tensor(out=G1, in0=mod[:, 1024:2048], scalar=1.0, in1=nbc[:], op0=ALU.add, op1=ALU.mult),
               reads=[nbc, mod], writes=[mod1])
            op("dve", lambda e: e.tensor_copy(out=SHIFT1, in_=mod[:, 0:1024]), reads=[mod], writes=[mod1])
            op("sp", lambda e: e.dma_start(out=nbc[:], in_=norm2_w.partition_broadcast(128)), writes=[nbc], dma=True)
            op("dve", lambda e: e.scalar_tensor_tensor(out=mod[:, 4096:5120], in0=mod[:, 4096:5120], scalar=1.0, in1=nbc[:], op0=ALU.add, op1=ALU.mult),
               reads=[nbc], writes=[mod])
            modscr_b = Buf("modscr")
            op("sp", lambda e: e.dma_start(out=modscr, in_=mod[:, 3072:6144]), reads=[mod], writes=[modscr_b], dma=True)
            for kt in range(8):
                op("dve", lambda e, kt=kt: e.tensor_tensor(out=wout[:, kt, :], in0=wout[:, kt, :], in1=GATE1, op=ALU.mult),
                   reads=[mod], writes=[wout])
                op("pool", lambda e, kt=kt: e.tensor_scalar(out=win[:, kt, 768:1280], in0=win[:, kt, 768:1280],
                                                            scalar1=0.5, scalar2=1.0, op0=ALU.mult, op1=ALU.mult),
                   writes=[win])
            op("sp", lambda e: e.dma_start(out=cwT[:], in_=conv_w.rearrange("k (c p) -> p k c", p=128),
                                           allow_slow_non_contiguous=True), writes=[cwT], dma=True)
            for k in range(4):
                for ct in range(8):
                    op("dve", lambda e, k=k, ct=ct: e.tensor_scalar(out=convdiag[:, k * 8 + ct, :], in0=IDENT,
                                                                   scalar1=cwT[:, k, ct:ct + 1], scalar2=0.5,
                                                                   op0=ALU.mult, op1=ALU.mult),
                       reads=[cbf, cwT], writes=[convdiag])
            op("sp", lambda e: e.dma_start(out=cbst[:], in_=conv_b.rearrange("(o n) -> o n", o=1)), writes=[cbst], dma=True)
            op("dve", lambda e: e.tensor_scalar(out=cbrow[:], in0=cbst[:], scalar1=0.5, scalar2=None, op0=ALU.mult),
               reads=[cbst], writes=[cbrow])
            op("dve", lambda e: e.memset(onesrow[:], 1.0), writes=[onesrow])
            for i, src in enumerate((dt_bias, a_log, d_skip, sinks)):
                op("sp", lambda e, i=i, src=src: e.dma_start(out=small[:, i * 8:(i + 1) * 8], in_=src.partition_broadcast(128)),
                   writes=[small], dma=True)
            op("act", lambda e: e.activation(out=ABC, in_=ABC, func=AF.Exp), reads=[small], writes=[small])
            op("dve", lambda e: e.tensor_scalar(out=ABC, in0=ABC, scalar1=-1.0, scalar2=None, op0=ALU.mult),
               reads=[small], writes=[small])
            for h in range(8):
                op("dve", lambda e, h=h: e.tensor_scalar(out=dI[:, h, :], in0=IDENT, scalar1=DSK[:, h:h + 1], scalar2=None,
                                                         op0=ALU.mult), reads=[cbf, small], writes=[dI])
            op("sp", lambda e: e.dma_start(out=snw[:], in_=ssd_norm_w.partition_broadcast(128)), writes=[snw], dma=True)
            op("sp", lambda e: e.dma_start(out=qkw[:, 0, :], in_=q_norm_w.partition_broadcast(128)), writes=[qkw], dma=True)
            op("sp", lambda e: e.dma_start(out=qkw[:, 1, :], in_=k_norm_w.partition_broadcast(128)), writes=[qkw], dma=True)
            op("dve", lambda e: e.tensor_reduce(out=mx[:, 0:1], in_=qkw[:, 0, :], axis=AX.X, op=ALU.max,
                                                apply_absolute_value=True), reads=[qkw], writes=[mx])
            op("dve", lambda e: e.tensor_reduce(out=mx[:, 1:2], in_=qkw[:, 1, :], axis=AX.X, op=ALU.max,
                                                apply_absolute_value=True), reads=[qkw], writes=[mx])
            op("dve", lambda e: e.tensor_tensor(out=negc[:], in0=mx[:, 0:1], in1=mx[:, 1:2], op=ALU.mult),
               reads=[mx], writes=[negc])
            op("dve", lambda e: e.tensor_scalar(out=negc[:], in0=negc[:], scalar1=-8.0, scalar2=None, op0=ALU.mult),
               reads=[negc], writes=[negc])
            op("act", lambda e: e.activation(out=ESINK, in_=SNK, func=AF.Exp, bias=negc[:]), reads=[small, negc],
               writes=[small])
            op("sp", lambda e: e.dma_start(out=posi[:], in_=posi_d), writes=[posi], dma=True)
            op("dve", lambda e: e.tensor_copy(out=posf[:], in_=posi[:]), reads=[posi], writes=[posf])
            op("dve", lambda e: e.tensor_tensor(out=ang[:], in0=posf[:, :, None].to_broadcast([128, NMAIN + 1, 8]),
                                                in1=INVF[:, None, :].to_broadcast([128, NMAIN + 1, 8]), op=ALU.mult),
               reads=[posf, cf32], writes=[ang])
            op("dve", lambda e: e.tensor_scalar(out=kk[:], in0=ang[:], scalar1=1.0 / TWO_PI, scalar2=None, op0=ALU.mult),
               reads=[ang], writes=[kk])
            op("dve", lambda e: e.tensor_copy(out=kki[:], in_=kk[:]), reads=[kk], writes=[kki])
            op("dve", lambda e: e.tensor_copy(out=kk[:], in_=kki[:]), reads=[kki], writes=[kk])
            op("dve", lambda e: e.scalar_tensor_tensor(out=ang[:], in0=kk[:], scalar=-C1, in1=ang[:], op0=ALU.mult, op1=ALU.add),
               reads=[kk, ang], writes=[ang])
            op("dve", lambda e: e.scalar_tensor_tensor(out=ang[:], in0=kk[:], scalar=-C2, in1=ang[:], op0=ALU.mult, op1=ALU.add),
               reads=[kk, ang], writes=[ang])
            for _ in range(2):
                for cmpop, sgn in ((ALU.is_gt, -1.0), (ALU.is_lt, 1.0)):
                    thr = np.pi if sgn < 0 else -np.pi
                    op("dve", lambda e, cmpop=cmpop, thr=thr, sgn=sgn: e.tensor_scalar(
                        out=kk[:], in0=ang[:], scalar1=float(thr), scalar2=float(sgn * TWO_PI), op0=cmpop, op1=ALU.mult),
                       reads=[ang], writes=[kk])
                    op("dve", lambda e: e.tensor_tensor(out=ang[:], in0=ang[:], in1=kk[:], op=ALU.add),
                       reads=[kk, ang], writes=[ang])
            op("act", lambda e: e.activation(out=cossin[:, 1, :, :], in_=ang[:], func=AF.Sin), reads=[ang], writes=[cossin])
            op("dve", lambda e: e.tensor_scalar(out=ang[:], in0=ang[:], scalar1=float(np.pi / 2), scalar2=None, op0=ALU.add),
               reads=[ang, cossin], writes=[ang])
            op("dve", lambda e: e.tensor_scalar(out=kk[:], in0=ang[:], scalar1=float(np.pi), scalar2=float(-TWO_PI),
                                                op0=ALU.is_gt, op1=ALU.mult), reads=[ang], writes=[kk])
            op("dve", lambda e: e.tensor_tensor(out=ang[:], in0=ang[:], in1=kk[:], op=ALU.add), reads=[kk, ang], writes=[ang])
            op("act", lambda e: e.activation(out=cossin[:, 0, :, :], in_=ang[:], func=AF.Sin), reads=[ang], writes=[cossin])
            op("dve", lambda e: e.tensor_scalar(out=maskp0[:], in0=MASKP, scalar1=flags[:, HALO:HALO + 1], scalar2=None,
                                                op0=ALU.mult), reads=[cbf, flags], writes=[maskp0])
            for mi, (msrc, mb_) in enumerate(((MASKP, cbf), (MASKC, cbf), (maskp0[:], maskp0))):
                for i4 in range(4):
                    op("dve", lambda e, mi=mi, i4=i4, msrc=msrc: e.tensor_scalar(out=negm[:, mi, i4, :], in0=msrc, scalar1=-1.0, scalar2=30000.0,
                                                                                op0=ALU.add, op1=ALU.mult), reads=[mb_], writes=[negm])
            if DEBUG_X2 and STOP_AFTER == 1:
                op("sp", lambda e: e.dma_start(out=dbg_d[0:128, :], in_=SHIFT1), reads=[mod1], dma=True)
                op("sp", lambda e: e.dma_start(out=dbg_d[128:256, :], in_=G1), reads=[mod1], dma=True)
                op("sp", lambda e: e.dma_start(out=dbg_d[256:384, 0:272], in_=cossin[:].rearrange("p a b c -> p (a b c)")), reads=[cossin], dma=True)
                op("sp", lambda e: e.dma_start(out=dbg_d[384:512, 0:48], in_=small[:]), reads=[small], dma=True)
                op("sp", lambda e: e.dma_start(out=dbg_d[640:768, :], in_=mod[:, 2048:3072]), reads=[mod], dma=True)
            S.barrier(lambda e: e.memset(neghalf[:, 16:32], -0.5))

        shr = sb(p1, "shr", [128, D])
        tmpf1 = shr
        xpf = [sb(p1, "xpf%d" % i, [128, D]) for i in range(2)]
        yo = sb(p1, "yo", [128, 512])
        ss = [sb(p1, "ss%d" % i, [128, 4]) for i in range(2)]
        hb = [sb(p1, "hb%d" % i, [128, D], BF16) for i in range(2)]
        hT2 = [sb(p1, "hT2_%d" % i, [128, 8, 256], BF16) for i in range(2)]
        hT = [View(hT2[i][:, :, 0:128], hT2[i].b) for i in range(2)]
        xpre2 = [sb(p1, "xpre2_%d" % i, [128, 8, 260], BF16) for i in range(2)]
        xpre = [View(xpre2[i][:, :, 0:132], xpre2[i].b) for i in range(2)]
        tcv2 = sb(p1, "tcv2", [128, 6, 256], BF16)
        tcv = View(tcv2[:].rearrange("p a b -> p (a b)")[:, 0:1024].rearrange("p (a b) -> p a b", b=128), tcv2.b)
        xc2 = sb(p1, "xc2", [128, 8, 256], BF16)
        xc_ = [View(xc2[:, :, i * 128:(i + 1) * 128], Buf("xc%d" % i)) for i in range(2)]
        dtw2 = sb(p1, "dtw2", [128, 12, 16])
        dtw_ = [View(dtw2[:, :, i * 8:(i + 1) * 8], Buf("dtw%d" % i)) for i in range(2)]
        xdt_ = [sb(p1, "xdt%d" % i, [128, 8, 64], BF16) for i in range(2)]
        xdte2 = sb(p1, "xdte2", [128, 2, 8, 64], BF16)
        xdte_ = [View(xdte2[:, i, :, :], Buf("xdte%d" % i)) for i in range(2)]
        xstok_ = [sb(p1, "xstok%d" % i, [128, 8, 64], BF16) for i in range(2)]
        btok2 = sb(p1, "btok2", [128, 2, 2, 128], BF16)
        btok_ = [View(btok2[:, i, :, :], Buf("btok%d" % i)) for i in range(2)]
        R = sb(p1, "R", [128, 8, 64])
        Rb = sb(p1, "Rb", [128, 8, 64], BF16)
        gtm = sb(p1, "gtm", [128, 2, 128], BF16)
        tz = sb(p1, "tz", [128, 512], BF16)
        sz_ = [sb(p1, "sz%d" % i, [128, 512], BF16) for i in range(2)]
        ssg = sb(p1, "ssg", [128, 4])
        sq = sb(p1, "sq", [128, 10, 64], BF16)
        junk = View(sq[:].rearrange("p a b -> p (a b)")[:, 0:512], sq.b)
        ssq = sb(p1, "ssq", [128, 16])
        qn = sb(p1, "qn", [128, 10, 64])
        Lm = View(qn[:].rearrange("p a b -> p (a b)")[:, 0:512].rearrange("p (a b) -> p a b", b=128), qn.b)
        rt = sb(p1, "rt", [128, 4, 10, 8])
        qr_ = [sb(p1, "qr%d" % i, [128, 10, 64], BF16) for i in range(2)]
        qT = sb(p1, "qT", [128, 4, 128], BF16)
        kT = [sb(p1, "kT%d" % i, [128, 128], BF16) for i in range(2)]
        vaug = [sb(p1, "vaug%d" % i, [128, 2, 65], BF16) for i in range(2)]
        pT = sb(p1, "pT", [128, 4, 512], BF16)
        decT = View(pT[:].rearrange("p a b -> p (a b)")[:, 0:1024].rearrange("p (a b) -> p a b", b=128), pT.b)
        den = sb(p1, "den", [128, 8])
        mix = sb(p1, "mix", [128, D], BF16)
        mixT = sb(p1, "mixT", [128, 8, 128], BF16)

        op("dve", lambda e: e.memset(R[:], 0.0), writes=[R])
        for i in range(2):
            op("dve", lambda e, i=i: e.memset(xpre[i][:], 0.0), writes=[xpre[i]])
            op("dve", lambda e, i=i: e.memset(vaug[i][:], 1.0), writes=[vaug[i]])
            op("dve", lambda e, i=i: e.memset(kT[i][:], 0.0), writes=[kT[i]])

        B = banks
        bank_rr = [0]

        def nb():
            bk = banks[bank_rr[0] % 8]
            bank_rr[0] += 1
            return bk

        def tview(bk):
            return bfv(bk).rearrange("p (a b) -> p a b", b=128)

        def rstd_from_ss(ssap, n, width, tile, reads):
            op("dve", lambda e: e.tensor_scalar(out=ssap, in0=ssap, scalar1=1.0 / n, scalar2=EPS, op0=ALU.mult, op1=ALU.add),
               reads=reads, writes=[tile])
            if USE_POW:
                op("pool", lambda e: e.tensor_tensor(out=ssap, in0=ssap, in1=neghalf[:, 0:width], op=ALU.pow),
                   reads=[tile, neghalf], writes=[tile])
            else:
                op("act", lambda e: e.activation(out=ssap, in_=ssap, func=AF.Sqrt), reads=[tile], writes=[tile])
                op("dve", lambda e: e.reciprocal(out=ssap, in_=ssap), reads=[tile], writes=[tile])

        NPAIR = 23

        def frontend(s):
            par = s % 2
            main = s >= NPRE
            if s < 2 * NPAIR:
                hdst = hT2[(s // 2) % 2]
                hdst_ap = hdst[:, :, (s % 2) * 128:(s % 2 + 1) * 128]
            elif main and MAIN_PAIRS:
                hdst = hT2[((s - NPRE) // 2) % 2]
                hdst_ap = hdst[:, :, ((s - NPRE) % 2) * 128:((s - NPRE) % 2 + 1) * 128]
            else:
                hdst = hT[par]
                hdst_ap = hdst[:]
            j = s - NPRE
            if main:
                xt = xres_t[:, j, :]; xb = xres_b[j]
            else:
                xt = xpf[par][:]; xb = xpf[par].b
            op("sp", lambda e: e.dma_start(out=xt, in_=xe[s * 128:(s + 1) * 128, :]), writes=[xb], dma=True)
            sst = ss[par]
            hbt = hb[par]
            op("act", lambda e: e.activation(out=hbt[:], in_=xt, func=AF.Square, accum_out=sst[:, 0:1]),
               reads=[xb], writes=[hbt, sst])
            rstd_from_ss(sst[:, 0:1], float(D), 1, sst, [sst])
            if main:
                tfa = tmpf1[:]; tfb = tmpf1.b
            else:
                tfa = xt; tfb = xb
            op("dve", lambda e: e.scalar_tensor_tensor(out=tfa, in0=xt, scalar=sst[:, 0:1], in1=G1, op0=ALU.mult, op1=ALU.mult),
               reads=[xb, sst, mod1], writes=[tfb])
            op("pool", lambda e: e.tensor_tensor(out=hbt[:], in0=tfa, in1=SHIFT1, op=ALU.add), reads=[tfb, mod1], writes=[hbt])
            TH = nb()
            tv = tview(TH)
            for kt in range(8):
                op("pe", lambda e, kt=kt: e.transpose(out=tv[:, kt, :], in_=hbt[:, kt::8], identity=IDENT),
                   reads=[hbt, cbf], writes=[TH])
            op("act", lambda e: e.activation(out=hdst_ap, in_=tv, func=AF.Copy), reads=[TH], writes=[hdst])

        def proj_tok(bank, ncols, col0, h):
            for kt in range(8):
                op("pe", lambda e, kt=kt: e.matmul(bank[:, 0:ncols], lhsT=h[:, kt, :], rhs=win[:, kt, col0:col0 + ncols],
                                                   start=(kt == 0), stop=(kt == 7)), reads=[h, win], writes=[bank])

        def backend2(s, pre_done=False):
            par = s % 2
            main = s >= NPRE
            halo = s == HALO
            j = s - NPRE
            h = hT[par]
            if pre_done:
                h = View(hT2[(j // 2) % 2][:, :, (j % 2) * 128:(j % 2 + 1) * 128], hT2[(j // 2) % 2].b)
            fl = flags[:, s:s + 1]
            nct = 8 if (main or halo) else 6
            xc = xc_[par]; dtw = dtw_[par]; xdt = xdt_[par]; xdte = xdte_[par]; xstok = xstok_[par]; btok = btok_[par]
            sz = sz_[par]; qr = qr_[par]
            V = dtw[:, 0, :]; U = dtw[:, 1, :]; W = dtw[:, 2, :]; Y = dtw[:, 3, :]; T1 = dtw[:, 4, :]
            DT = dtw[:, 5, :]; AA = dtw[:, 6, :]; ACS = dtw[:, 7, :]; EA = dtw[:, 8, :]; DTE = dtw[:, 9, :]
            CD = dtw[:, 10, :]; DTF = dtw[:, 11, :]
            qrf = qr[:].rearrange("p a b -> p (a b)")

            PK = nb()
            if main or halo:
                proj_tok(PK, 256, 512, h)
            for kt in range(0 if pre_done else 8):
                op("pe", lambda e, kt=kt: e.matmul(PK[:, 256:264], lhsT=h[:, kt, :], rhs=win[:, kt, 2304:2312],
                                                   start=(kt == 0), stop=(kt == 7)), reads=[h, win], writes=[PK])
            XB = [None, None] if pre_done else [nb(), nb()]
            for ct in range(0 if pre_done else nct):
                bk = XB[ct // 4]
                for kt in range(8):
                    op("pe", lambda e, kt=kt, ct=ct, bk=bk: e.matmul(
                        bk[:, (ct % 4) * 128:(ct % 4 + 1) * 128], lhsT=win[:, kt, 1280 + ct * 128:1280 + (ct + 1) * 128],
                        rhs=h[:, kt, :], start=(kt == 0), stop=(kt == 7)), reads=[h, win], writes=[bk])
            if main:
                PQ = nb(); PZ = nb()
                proj_tok(PQ, 512, 0, h)
                proj_tok(PZ, 512, 768, h)

            if not pre_done:
                xp = xpre[par]; xpn = xpre[1 - par]
                for hb2 in range(2):
                    n4 = min(4, nct - hb2 * 4)
                    op("act", lambda e, hb2=hb2, n4=n4: e.activation(
                        out=xp[:, hb2 * 4:hb2 * 4 + n4, 3:131],
                        in_=XB[hb2][:, 0:n4 * 128].rearrange("p (a b) -> p a b", b=128), func=AF.Copy, scale=fl),
                       reads=[XB[hb2], flags], writes=[xp])
                op("pool", lambda e: e.tensor_copy(out=xpn[:, :, 0:3], in_=xp[:, :, 128:131]), reads=[xp], writes=[xpn])

                op("dve", lambda e: e.tensor_tensor(out=V, in0=PK[:, 256:264], in1=DTB, op=ALU.add), reads=[PK, small], writes=[dtw])
                op("act", lambda e: e.activation(out=U, in_=V, func=AF.Abs), reads=[dtw], writes=[dtw])
                op("act", lambda e: e.activation(out=U, in_=U, func=AF.Exp, scale=-1.0), reads=[dtw], writes=[dtw])
                op("dve", lambda e: e.tensor_scalar(out=W, in0=U, scalar1=1.0, scalar2=None, op0=ALU.add), reads=[dtw], writes=[dtw])
                op("dve", lambda e: e.tensor_scalar(out=Y, in0=U, scalar1=-0.33, scalar2=0.99, op0=ALU.mult, op1=ALU.add),
                   reads=[dtw], writes=[dtw])
                op("dve", lambda e: e.tensor_tensor(out=Y, in0=Y, in1=U, op=ALU.mult), reads=[dtw], writes=[dtw])
                for _ in range(3):
                    op("act", lambda e: e.activation(out=T1, in_=Y, func=AF.Exp, scale=-1.0), reads=[dtw], writes=[dtw])
                    op("dve", lambda e: e.tensor_tensor(out=T1, in0=T1, in1=W, op=ALU.mult), reads=[dtw], writes=[dtw])
                    op("dve", lambda e: e.scalar_tensor_tensor(out=Y, in0=Y, scalar=-1.0, in1=T1, op0=ALU.add, op1=ALU.add),
                       reads=[dtw], writes=[dtw])
                op("dve", lambda e: e.scalar_tensor_tensor(out=DT, in0=V, scalar=0.0, in1=Y, op0=ALU.max, op1=ALU.add),
                   reads=[dtw], writes=[dtw])
                op("dve", lambda e: e.tensor_tensor(out=AA, in0=DT, in1=ABC, op=ALU.mult), reads=[dtw, small], writes=[dtw])
                op("dve", lambda e: e.tensor_scalar(out=DTF, in0=DT, scalar1=fl, scalar2=None, op0=ALU.mult), reads=[dtw, flags], writes=[dtw])

            if main or halo:
                vg = vaug[par]
                op("act", lambda e: e.activation(out=vg[:, :, 0:64], in_=PK[:, 128:256].rearrange("p (a b) -> p a b", b=64),
                                                 func=AF.Copy), reads=[PK], writes=[vg])
                op("act", lambda e: e.activation(out=sq[:, 8:10, :], in_=PK[:, 0:128].rearrange("p (a b) -> p a b", b=64),
                                                 func=AF.Square), reads=[PK], writes=[sq])
                h0 = 0 if main else 8
                if main:
                    op("act", lambda e: e.activation(out=sq[:, 0:8, :], in_=PQ[:, :].rearrange("p (a b) -> p a b", b=64),
                                                     func=AF.Square), reads=[PQ], writes=[sq])
                op("dve", lambda e: e.tensor_reduce(out=ssq[:, h0:10], in_=sq[:, h0:10, :], axis=AX.X, op=ALU.add),
                   reads=[sq], writes=[ssq])
                rstd_from_ss(ssq[:, h0:10], 64.0, 10 - h0, ssq, [ssq])
                if main:
                    op("dve", lambda e: e.tensor_tensor(
                        out=qn[:, 0:8, :].rearrange("p (i g) d -> p g i d", g=2),
                        in0=PQ[:, :].rearrange("p (g i d) -> p g i d", g=2, i=4),
                        in1=ssq[:, 0:8].rearrange("p (g i) -> p g i", g=2)[:, :, :, None].to_broadcast([128, 2, 4, 64]),
                        op=ALU.mult), reads=[PQ, ssq], writes=[qn])
                op("dve", lambda e: e.tensor_tensor(out=qn[:, 8:10, :], in0=PK[:, 0:128].rearrange("p (a b) -> p a b", b=64),
                                                    in1=ssq[:, 8:10, None].to_broadcast([128, 2, 64]), op=ALU.mult),
                   reads=[PK, ssq], writes=[qn])
                if main:
                    op("pool", lambda e: e.tensor_tensor(out=qn[:, 0:8, :], in0=qn[:, 0:8, :],
                                                         in1=qkw[:, 0, None, :].to_broadcast([128, 8, 64]), op=ALU.mult),
                       reads=[qkw], writes=[qn])
                op("pool", lambda e: e.tensor_tensor(out=qn[:, 8:10, :], in0=qn[:, 8:10, :],
                                                     in1=qkw[:, 1, None, :].to_broadcast([128, 2, 64]), op=ALU.mult),
                   reads=[qkw], writes=[qn])
                jj = s - HALO
                nhh = 10 - h0
                cosb = cossin[:, 0, jj, None, :].to_broadcast([128, nhh, 8])
                sinb = cossin[:, 1, jj, None, :].to_broadcast([128, nhh, 8])
                x1 = qn[:, h0:10, 0:8]; x2 = qn[:, h0:10, 8:16]
                op("pool", lambda e: e.tensor_tensor(out=rt[:, 0, h0:10, :], in0=x1, in1=cosb, op=ALU.mult), reads=[qn, cossin], writes=[rt])
                op("pool", lambda e: e.tensor_tensor(out=rt[:, 1, h0:10, :], in0=x2, in1=sinb, op=ALU.mult), reads=[qn, cossin], writes=[rt])
                op("pool", lambda e: e.tensor_tensor(out=rt[:, 2, h0:10, :], in0=x2, in1=cosb, op=ALU.mult), reads=[qn, cossin], writes=[rt])
                op("pool", lambda e: e.tensor_tensor(out=rt[:, 3, h0:10, :], in0=x1, in1=sinb, op=ALU.mult), reads=[qn, cossin], writes=[rt])
                op("pool", lambda e: e.tensor_tensor(out=qr[:, h0:10, 0:8], in0=rt[:, 0, h0:10, :], in1=rt[:, 1, h0:10, :], op=ALU.subtract),
                   reads=[rt], writes=[qr])
                op("pool", lambda e: e.tensor_tensor(out=qr[:, h0:10, 8:16], in0=rt[:, 2, h0:10, :], in1=rt[:, 3, h0:10, :], op=ALU.add),
                   reads=[rt], writes=[qr])
                op("pool", lambda e: e.tensor_copy(out=qr[:, h0:10, 16:64], in_=qn[:, h0:10, 16:64]), reads=[qn], writes=[qr])
            if main:
                op("act", lambda e: e.activation(out=tz[:], in_=PZ[:, :], func=AF.Tanh), reads=[PZ], writes=[tz])
                op("dve", lambda e: e.scalar_tensor_tensor(out=sz[:], in0=tz[:], scalar=1.0, in1=PZ[:, :], op0=ALU.add, op1=ALU.mult),
                   reads=[tz, PZ], writes=[sz])

            if not pre_done:
                CB = [nb(), nb()]
                for ct in range(nct):
                    bk = CB[ct // 4]
                    o = bk[:, (ct % 4) * 128:(ct % 4 + 1) * 128]
                    for k in range(4):
                        op("pe", lambda e, k=k, ct=ct, o=o: e.matmul(o, lhsT=convdiag[:, k * 8 + ct, :], rhs=xp[:, ct, k:k + 128],
                                                                    start=(k == 0), stop=False), reads=[convdiag, xp], writes=[bk])
                    op("pe", lambda e, ct=ct, o=o: e.matmul(o, lhsT=cbrow[0:1, ct * 128:(ct + 1) * 128], rhs=onesrow[0:1, 0:128],
                                                            start=False, stop=True), reads=[cbrow, onesrow], writes=[bk])
                for hb2 in range(2):
                    n4 = min(4, nct - hb2 * 4)
                    pv = CB[hb2][:, 0:n4 * 128].rearrange("p (a b) -> p a b", b=128)
                    op("act", lambda e, hb2=hb2, n4=n4, pv=pv: e.activation(out=tcv[:, hb2 * 4:hb2 * 4 + n4, :], in_=pv, func=AF.Tanh),
                       reads=[CB[hb2]], writes=[tcv])
                    op("dve", lambda e, hb2=hb2, n4=n4, pv=pv: e.scalar_tensor_tensor(
                        out=xc[:, hb2 * 4:hb2 * 4 + n4, :], in0=tcv[:, hb2 * 4:hb2 * 4 + n4, :], scalar=1.0, in1=pv,
                        op0=ALU.add, op1=ALU.mult), reads=[tcv, CB[hb2]], writes=[xc])

            DB = nb()
            if not pre_done:
                op("pe", lambda e: e.matmul(DB[:, 0:8], lhsT=TRI, rhs=AA, start=True, stop=True), reads=[cf32, dtw], writes=[DB])
                op("pe", lambda e: e.matmul(DB[:, 8:16], lhsT=ONES, rhs=AA, start=True, stop=True), reads=[cf32, dtw], writes=[DB])
            if main:
                for g in range(2):
                    op("pe", lambda e, g=g: e.matmul(DB[:, 128 + g * 128:256 + g * 128], lhsT=xc[:, 4 + g, :], rhs=xc[:, 6 + g, :],
                                                     start=True, stop=True), reads=[xc], writes=[DB])
            if not pre_done:
                op("act", lambda e: e.activation(out=ACS, in_=DB[:, 0:8], func=AF.Copy), reads=[DB], writes=[dtw])
                op("act", lambda e: e.activation(out=CD, in_=DB[:, 8:16], func=AF.Exp), reads=[DB], writes=[dtw])
                op("dve", lambda e: e.tensor_tensor(out=DTE, in0=DB[:, 8:16], in1=ACS, op=ALU.subtract), reads=[DB, dtw], writes=[dtw])
                op("act", lambda e: e.activation(out=DTE, in_=DTE, func=AF.Exp), reads=[dtw], writes=[dtw])
            if main:
                if not pre_done:
                    op("act", lambda e: e.activation(out=EA, in_=ACS, func=AF.Exp), reads=[dtw], writes=[dtw])
                op("dve", lambda e: e.tensor_tensor(out=gtm[:], in0=DB[:, 128:384].rearrange("p (a b) -> p a b", b=128),
                                                    in1=MASKC[:, None, :].to_broadcast([128, 2, 128]), op=ALU.mult),
                   reads=[DB, cbf], writes=[gtm])
            if not pre_done:
                op("dve", lambda e: e.tensor_tensor(out=DTE, in0=DTE, in1=DTF, op=ALU.mult), reads=[dtw], writes=[dtw])

            TB = nb()
            tv = tview(TB)
            for i in range(6):
                op("pe", lambda e, i=i: e.transpose(out=tv[:, i, :], in_=xc[:, i, :], identity=IDENT), reads=[xc, cbf], writes=[TB])
            xps = bfv(TB)[:, 0:512].rearrange("p (a b) -> p a b", b=64)
            op("dve", lambda e: e.tensor_tensor(out=xdte[:], in0=xps, in1=DTE[:, :, None].to_broadcast([128, 8, 64]), op=ALU.mult),
               reads=[TB, dtw], writes=[xdte])
            if main:
                op("dve", lambda e: e.tensor_tensor(out=xdt[:], in0=xps, in1=DTF[:, :, None].to_broadcast([128, 8, 64]), op=ALU.mult),
                   reads=[TB, dtw], writes=[xdt])
                op("act", lambda e: e.activation(out=xstok[:], in_=xps, func=AF.Copy), reads=[TB], writes=[xstok])
            op("act", lambda e: e.activation(out=btok[:], in_=tv[:, 4:6, :], func=AF.Copy), reads=[TB], writes=[btok])
            if main:
                op("act", lambda e: e.activation(out=Rb[:], in_=R[:], func=AF.Copy), reads=[R], writes=[Rb])

            SBK = nb()
            for g in range(2):
                op("pe", lambda e, g=g: e.matmul(SBK[:, g * 256:(g + 1) * 256], lhsT=btok[:, g, :],
                                                 rhs=xdte[:, g * 4:(g + 1) * 4, :], start=True, stop=True),
                   reads=[btok, xdte], writes=[SBK])
            op("dve", lambda e: e.tensor_tensor(out=R[:], in0=R[:], in1=CD[:, :, None].to_broadcast([128, 8, 64]), op=ALU.mult),
               reads=[dtw, Rb], writes=[R])
            op("dve", lambda e: e.tensor_tensor(out=R[:], in0=R[:], in1=SBK[:, :].rearrange("p (a b) -> p a b", b=64), op=ALU.add),
               reads=[SBK], writes=[R])

            if main or halo:
                TQ = nb()
                tq = tview(TQ)
                if main:
                    for i in range(4):
                        op("pe", lambda e, i=i: e.transpose(out=tq[:, i, :], in_=qrf[:, i * 128:(i + 1) * 128], identity=IDENT),
                           reads=[qr, cbf], writes=[TQ])
                op("pe", lambda e: e.transpose(out=tq[:, 4, :], in_=qrf[:, 512:640], identity=IDENT), reads=[qr, cbf], writes=[TQ])
                if main:
                    op("act", lambda e: e.activation(out=qT[:], in_=tq[:, 0:4, :], func=AF.Copy), reads=[TQ], writes=[qT])
                op("act", lambda e: e.activation(out=kT[par][:], in_=tq[:, 4, :], func=AF.Copy), reads=[TQ], writes=[kT[par]])
            if not main:
                return

            kprev = kT[1 - par]; vprev = vaug[1 - par]
            kcur = kT[par]; vcur = vaug[par]
            sbanks = [nb(), nb(), nb(), nb()]
            for g in range(2):
                for bi, kt_ in enumerate((kprev, kcur)):
                    bk = sbanks[g * 2 + bi]
                    op("pe", lambda e, g=g, kt_=kt_, bk=bk: e.matmul(
                        bk[:, :], lhsT=kt_[g * 64:(g + 1) * 64, :], rhs=qT[g * 64:(g + 1) * 64, :, :], start=True, stop=False),
                       reads=[kt_, qT], writes=[bk])
                    mi = (2 if j == 0 else 0) if bi == 0 else 1
                    op("pe", lambda e, bk=bk, mi=mi: e.matmul(bk[:, :], lhsT=IDENT, rhs=negm[:, mi, :, :], start=False, stop=True),
                       reads=[cbf, negm], writes=[bk])

            SEG = [nb(), nb()]
            for hf in range(2):
                op("pool", lambda e, hf=hf: e.tensor_tensor(out=Lm[:], in0=SLM[:, None, :].to_broadcast([128, 4, 128]),
                                                            in1=AA[:, hf * 4:(hf + 1) * 4, None].to_broadcast([128, 4, 128]), op=ALU.mult),
                   reads=[cf32, dtw], writes=[Lm])
                bk = SEG[hf]
                for h4 in range(4):
                    op("pe", lambda e, h4=h4, bk=bk: e.matmul(bk[:, h4 * 128:(h4 + 1) * 128], lhsT=Lm[:, h4, :], rhs=TRI,
                                                              start=True, stop=True), reads=[Lm, cf32], writes=[bk])
            for g in range(2):
                for bi in range(2):
                    bk = sbanks[g * 2 + bi]
                    idx = g * 2 + bi
                    op("act", lambda e, bk=bk, idx=idx: e.activation(out=pT[:, idx, :], in_=bk[:, :], func=AF.Exp, scale=0.125,
                                                                     bias=negc[:]), reads=[bk, negc], writes=[pT])
            obanks = [nb(), nb()]
            for g in range(2):
                ob = obanks[g]
                for i in range(4):
                    for bi, vt in enumerate((vprev, vcur)):
                        idx = g * 2 + bi
                        op("pe", lambda e, g=g, i=i, bi=bi, vt=vt, idx=idx, ob=ob: e.matmul(
                            ob[:, i * 65:(i + 1) * 65], lhsT=pT[:, idx, i * 128:(i + 1) * 128], rhs=vt[:, g, :],
                            start=(bi == 0), stop=(bi == 1)), reads=[pT, vt], writes=[ob])
                ov = ob[:, 0:260].rearrange("p (a b) -> p a b", b=65)
                op("dve", lambda e, g=g, ov=ov: e.tensor_tensor(out=den[:, g * 4:(g + 1) * 4], in0=ov[:, :, 64],
                                                               in1=ESINK[:, g * 4:(g + 1) * 4], op=ALU.add),
                   reads=[ob, small], writes=[den])
                op("dve", lambda e, g=g: e.reciprocal(out=den[:, g * 4:(g + 1) * 4], in_=den[:, g * 4:(g + 1) * 4]),
                   reads=[den], writes=[den])
                op("dve", lambda e, g=g, ov=ov: e.tensor_tensor(
                    out=mix[:, g * 256:(g + 1) * 256].rearrange("p (a b) -> p a b", b=64), in0=ov[:, :, 0:64],
                    in1=den[:, g * 4:(g + 1) * 4, None].to_broadcast([128, 4, 64]), op=ALU.mult), reads=[ob, den], writes=[mix])
            for hb2 in range(2):
                op("act", lambda e, hb2=hb2: e.activation(out=decT[:, hb2 * 4:(hb2 + 1) * 4, :],
                                                          in_=SEG[hb2][:, :].rearrange("p (a b) -> p a b", b=128), func=AF.Exp),
                   reads=[SEG[hb2]], writes=[decT])
            for g in range(2):
                op("dve", lambda e, g=g: e.tensor_tensor(out=decT[:, g * 4:(g + 1) * 4, :], in0=decT[:, g * 4:(g + 1) * 4, :],
                                                          in1=gtm[:, g, None, :].to_broadcast([128, 4, 128]), op=ALU.mult),
                   reads=[gtm], writes=[decT])
            YB = nb(); YOFF = nb()
            for hh in range(8):
                o = YB[:, hh * 64:(hh + 1) * 64]
                op("pe", lambda e, hh=hh, o=o: e.matmul(o, lhsT=decT[:, hh, :], rhs=xdt[:, hh, :], start=True, stop=False),
                   reads=[decT, xdt], writes=[YB])
                op("pe", lambda e, hh=hh, o=o: e.matmul(o, lhsT=dI[:, hh, :], rhs=xstok[:, hh, :], start=False, stop=True),
                   reads=[dI, xstok], writes=[YB])
            for g in range(2):
                op("pe", lambda e, g=g: e.matmul(YOFF[:, g * 256:(g + 1) * 256], lhsT=xc[:, 6 + g, :], rhs=Rb[:, g * 4:(g + 1) * 4, :],
                                                 start=True, stop=True), reads=[xc, Rb], writes=[YOFF])
            op("dve", lambda e: e.tensor_tensor(out=yo[:].rearrange("p (a b) -> p a b", b=64),
                                                in0=YOFF[:, :].rearrange("p (a b) -> p a b", b=64),
                                                in1=EA[:, :, None].to_broadcast([128, 8, 64]), op=ALU.mult),
               reads=[YOFF, dtw], writes=[yo])
            yy = yo
            op("dve", lambda e: e.tensor_tensor(out=yy[:], in0=YB[:, :], in1=yo[:], op=ALU.add), reads=[YB], writes=[yy])
            op("dve", lambda e: e.tensor_tensor(out=yy[:], in0=yy[:], in1=sz[:], op=ALU.mult), reads=[sz], writes=[yy])
            for g in range(2):
                op("act", lambda e, g=g: e.activation(out=junk[:, g * 256:(g + 1) * 256], in_=yy[:, g * 256:(g + 1) * 256],
                                                      func=AF.Square, accum_out=ssg[:, g:g + 1]), reads=[yy], writes=[junk, ssg])
            rstd_from_ss(ssg[:, 0:2], 256.0, 2, ssg, [ssg])
            for g in range(2):
                op("dve", lambda e, g=g: e.scalar_tensor_tensor(out=mix[:, 512 + g * 256:768 + g * 256], in0=yy[:, g * 256:(g + 1) * 256],
                                                                scalar=ssg[:, g:g + 1], in1=snw[:, g * 256:(g + 1) * 256],
                                                                op0=ALU.mult, op1=ALU.mult), reads=[yy, ssg, snw], writes=[mix])

            TM = nb()
            tm = tview(TM)
            for kt in range(8):
                op("pe", lambda e, kt=kt: e.transpose(out=tm[:, kt, :], in_=mix[:, kt::8], identity=IDENT), reads=[mix, cbf], writes=[TM])
            op("act", lambda e: e.activation(out=mixT[:], in_=tm, func=AF.Copy), reads=[TM], writes=[mixT])
            for half in range(2):
                bk = nb()
                for kt in range(8):
                    op("pe", lambda e, kt=kt, half=half, bk=bk: e.matmul(bk[:, :], lhsT=mixT[:, kt, :],
                                                                        rhs=wout[:, kt, half * 512:(half + 1) * 512],
                                                                        start=(kt == 0), stop=(kt == 7)), reads=[mixT, wout], writes=[bk])
                op("dve", lambda e, half=half, bk=bk: e.tensor_tensor(out=xres_t[:, j, half * 512:(half + 1) * 512],
                                                                      in0=bk[:, :], in1=xres_t[:, j, half * 512:(half + 1) * 512],
                                                                      op=ALU.add), reads=[bk], writes=[xres_b[j]])
            if DEBUG_X2:
                op("sp", lambda e: e.dma_start(out=dbg_d[j * 128:(j + 1) * 128, :], in_=xres_t[:, j, :]), reads=[xres_b[j]], dma=True)

        def prefix_pair(p):
            s0 = 2 * p
            pp = p % 2
            h2 = hT2[pp]
            fl = flags[:, s0:s0 + 1]
            xp = xpre2[pp]
            last = (p == NPAIR - 1)
            xpn = xpre2[0] if last else xpre2[1 - pp]
            XCB = [xc_[0].b, xc_[1].b]; DTB_ = [dtw_[0].b, dtw_[1].b]
            XDB = [xdte_[0].b, xdte_[1].b]; BTB = [btok_[0].b, btok_[1].b]
            D2 = lambda r: dtw2[:, r, :]
            V, U, W, Y, T1, DT, AA, ACS, DTE, CD, DTF = (D2(0), D2(1), D2(2), D2(3), D2(4), D2(5), D2(6), D2(7), D2(9), D2(10), D2(11))
            PK = nb()
            for hf in range(2):
                for kt in range(8):
                    op("pe", lambda e, kt=kt, hf=hf: e.matmul(PK[:, hf * 8:(hf + 1) * 8], lhsT=h2[:, kt, hf * 128:(hf + 1) * 128],
                                                              rhs=win[:, kt, 2304:2312], start=(kt == 0), stop=(kt == 7)),
                       reads=[h2, win], writes=[PK])
            XB = [nb(), nb(), nb()]
            for ct in range(6):
                bk = XB[ct // 2]
                for kt in range(8):
                    op("pe", lambda e, kt=kt, ct=ct, bk=bk: e.matmul(
                        bk[:, (ct % 2) * 256:(ct % 2 + 1) * 256], lhsT=win[:, kt, 1280 + ct * 128:1280 + (ct + 1) * 128],
                        rhs=h2[:, kt, :], start=(kt == 0), stop=(kt == 7)), reads=[h2, win], writes=[bk])
            for b3 in range(3):
                op("act", lambda e, b3=b3: e.activation(out=xp[:, 2 * b3:2 * b3 + 2, 3:259],
                                                        in_=XB[b3][:, :].rearrange("p (a b) -> p a b", b=256), func=AF.Copy, scale=fl),
                   reads=[XB[b3], flags], writes=[xp])
            op("pool", lambda e: e.tensor_copy(out=xpn[:, 0:6, 0:3], in_=xp[:, 0:6, 256:259]), reads=[xp], writes=[xpn])
            op("dve", lambda e: e.tensor_tensor(out=V.rearrange("p (a b) -> p a b", b=8), in0=PK[:, 0:16].rearrange("p (a b) -> p a b", b=8),
                                                in1=DTB[:, None, :].to_broadcast([128, 2, 8]), op=ALU.add), reads=[PK, small], writes=DTB_)
            op("act", lambda e: e.activation(out=U, in_=V, func=AF.Abs), reads=DTB_, writes=DTB_)
            op("act", lambda e: e.activation(out=U, in_=U, func=AF.Exp, scale=-1.0), reads=DTB_, writes=DTB_)
            op("dve", lambda e: e.tensor_scalar(out=W, in0=U, scalar1=1.0, scalar2=None, op0=ALU.add), reads=DTB_, writes=DTB_)
            op("dve", lambda e: e.tensor_scalar(out=Y, in0=U, scalar1=-0.33, scalar2=0.99, op0=ALU.mult, op1=ALU.add), reads=DTB_, writes=DTB_)
            op("dve", lambda e: e.tensor_tensor(out=Y, in0=Y, in1=U, op=ALU.mult), reads=DTB_, writes=DTB_)
            for _ in range(3):
                op("act", lambda e: e.activation(out=T1, in_=Y, func=AF.Exp, scale=-1.0), reads=DTB_, writes=DTB_)
                op("dve", lambda e: e.tensor_tensor(out=T1, in0=T1, in1=W, op=ALU.mult), reads=DTB_, writes=DTB_)
                op("dve", lambda e: e.scalar_tensor_tensor(out=Y, in0=Y, scalar=-1.0, in1=T1, op0=ALU.add, op1=ALU.add), reads=DTB_, writes=DTB_)
            op("dve", lambda e: e.scalar_tensor_tensor(out=DT, in0=V, scalar=0.0, in1=Y, op0=ALU.max, op1=ALU.add), reads=DTB_, writes=DTB_)
            op("dve", lambda e: e.tensor_tensor(out=AA.rearrange("p (a b) -> p a b", b=8), in0=DT.rearrange("p (a b) -> p a b", b=8),
                                                in1=ABC[:, None, :].to_broadcast([128, 2, 8]), op=ALU.mult), reads=DTB_ + [small.b], writes=DTB_)
            op("dve", lambda e: e.tensor_scalar(out=DTF, in0=DT, scalar1=fl, scalar2=None, op0=ALU.mult), reads=DTB_ + [flags.b], writes=DTB_)
            CB = [nb(), nb(), nb()]
            for ct in range(6):
                bk = CB[ct // 2]
                o = bk[:, (ct % 2) * 256:(ct % 2 + 1) * 256]
                for k in range(4):
                    op("pe", lambda e, k=k, ct=ct, o=o: e.matmul(o, lhsT=convdiag[:, k * 8 + ct, :], rhs=xp[:, ct, k:k + 256],
                                                                start=(k == 0), stop=False), reads=[convdiag, xp], writes=[bk])
                op("pe", lambda e, ct=ct, o=o: e.matmul(o, lhsT=cbrow[0:1, ct * 128:(ct + 1) * 128], rhs=onesrow[0:1, :],
                                                        start=False, stop=True), reads=[cbrow, onesrow], writes=[bk])
            for b3 in range(3):
                pv = CB[b3][:, :].rearrange("p (a b) -> p a b", b=256)
                op("act", lambda e, b3=b3, pv=pv: e.activation(out=tcv2[:, 2 * b3:2 * b3 + 2, :], in_=pv, func=AF.Tanh),
                   reads=[CB[b3]], writes=[tcv2])
                op("dve", lambda e, b3=b3, pv=pv: e.scalar_tensor_tensor(out=xc2[:, 2 * b3:2 * b3 + 2, :], in0=tcv2[:, 2 * b3:2 * b3 + 2, :],
                                                                         scalar=1.0, in1=pv, op0=ALU.add, op1=ALU.mult),
                   reads=[tcv2, CB[b3]], writes=XCB)
            DB = nb()
            op("pe", lambda e: e.matmul(DB[:, 0:16], lhsT=TRI, rhs=AA, start=True, stop=True), reads=[cf32] + DTB_, writes=[DB])
            op("pe", lambda e: e.matmul(DB[:, 16:32], lhsT=ONES, rhs=AA, start=True, stop=True), reads=[cf32] + DTB_, writes=[DB])
            op("act", lambda e: e.activation(out=ACS, in_=DB[:, 0:16], func=AF.Copy), reads=[DB], writes=DTB_)
            op("act", lambda e: e.activation(out=CD, in_=DB[:, 16:32], func=AF.Exp), reads=[DB], writes=DTB_)
            op("dve", lambda e: e.tensor_tensor(out=DTE, in0=DB[:, 16:32], in1=ACS, op=ALU.subtract), reads=[DB] + DTB_, writes=DTB_)
            op("act", lambda e: e.activation(out=DTE, in_=DTE, func=AF.Exp), reads=DTB_, writes=DTB_)
            op("dve", lambda e: e.tensor_tensor(out=DTE, in0=DTE, in1=DTF, op=ALU.mult), reads=DTB_, writes=DTB_)
            op("dve", lambda e: e.tensor_tensor(out=DTE[:, 0:8], in0=DTE[:, 0:8], in1=CD[:, 8:16], op=ALU.mult), reads=DTB_, writes=DTB_)
            op("dve", lambda e: e.tensor_tensor(out=CD[:, 0:8], in0=CD[:, 0:8], in1=CD[:, 8:16], op=ALU.mult), reads=DTB_, writes=DTB_)
            for hf in range(2):
                TB = nb()
                tv = tview(TB)
                for i in range(6):
                    op("pe", lambda e, i=i, hf=hf, tv=tv: e.transpose(out=tv[:, i, :], in_=xc2[:, i, hf * 128:(hf + 1) * 128], identity=IDENT),
                       reads=XCB + [cbf.b], writes=[TB])
                xps = bfv(TB)[:, 0:512].rearrange("p (a b) -> p a b", b=64)
                op("dve", lambda e, hf=hf, xps=xps: e.tensor_tensor(out=xdte2[:, hf, :, :], in0=xps,
                                                                   in1=DTE[:, hf * 8:(hf + 1) * 8, None].to_broadcast([128, 8, 64]), op=ALU.mult),
                   reads=[TB] + DTB_, writes=XDB)
                op("act", lambda e, hf=hf, tv=tv: e.activation(out=btok2[:, hf, :, :], in_=tv[:, 4:6, :], func=AF.Copy), reads=[TB], writes=BTB)
            SBK = nb()
            for g in range(2):
                for hf in range(2):
                    op("pe", lambda e, g=g, hf=hf: e.matmul(SBK[:, g * 256:(g + 1) * 256], lhsT=btok2[:, hf, g, :],
                                                            rhs=xdte2[:, hf, g * 4:(g + 1) * 4, :], start=(hf == 0), stop=(hf == 1)),
                       reads=BTB + XDB, writes=[SBK])
            op("dve", lambda e: e.tensor_tensor(out=R[:], in0=R[:], in1=CD[:, 0:8, None].to_broadcast([128, 8, 64]), op=ALU.mult),
               reads=DTB_, writes=[R])
            op("dve", lambda e: e.tensor_tensor(out=R[:], in0=R[:], in1=SBK[:, :].rearrange("p (a b) -> p a b", b=64), op=ALU.add),
               reads=[SBK], writes=[R])

        def main_pair_pre(pm):
            s0 = NPRE + 2 * pm
            pp = pm % 2
            h2 = hT2[pp]
            fl = flags[:, s0:s0 + 1]
            xp = xpre2[pp]
            xpn = xpre2[1 - pp]
            XCB = [xc_[0].b, xc_[1].b]; DTB_ = [dtw_[0].b, dtw_[1].b]
            tcvb = [Buf("tcvs%d" % i) for i in range(6)]
            for tb_ in tcvb:
                tb_.w = list(tcv2.b.w); tb_.r = list(tcv2.b.r)
            xcsl = [Buf("xcsl%d" % i) for i in range(8)]
            for xb_ in xcsl:
                xb_.w = list(xc_[0].b.w) + list(xc_[1].b.w); xb_.r = list(xc_[0].b.r) + list(xc_[1].b.r)
            D2 = lambda r: dtw2[:, r, :]
            V, U, W, Y, T1, DT, AA, ACS, EA, DTE, CD, DTF = (D2(0), D2(1), D2(2), D2(3), D2(4), D2(5), D2(6), D2(7), D2(8), D2(9), D2(10), D2(11))
            PK = nb()
            for hf in range(2):
                for kt in range(8):
                    op("pe", lambda e, kt=kt, hf=hf: e.matmul(PK[:, hf * 8:(hf + 1) * 8], lhsT=h2[:, kt, hf * 128:(hf + 1) * 128],
                                                              rhs=win[:, kt, 2304:2312], start=(kt == 0), stop=(kt == 7)),
                       reads=[h2, win], writes=[PK])
            XB = [nb(), nb(), nb(), nb()]
            for ct in range(8):
                bk = XB[ct // 2]
                for kt in range(8):
                    op("pe", lambda e, kt=kt, ct=ct, bk=bk: e.matmul(
                        bk[:, (ct % 2) * 256:(ct % 2 + 1) * 256], lhsT=win[:, kt, 1280 + ct * 128:1280 + (ct + 1) * 128],
                        rhs=h2[:, kt, :], start=(kt == 0), stop=(kt == 7)), reads=[h2, win], writes=[bk])
            for b3 in range(4):
                op("act", lambda e, b3=b3: e.activation(out=xp[:, 2 * b3:2 * b3 + 2, 3:259],
                                                        in_=XB[b3][:, :].rearrange("p (a b) -> p a b", b=256), func=AF.Copy, scale=fl),
                   reads=[XB[b3], flags], writes=[xp])
            op("pool", lambda e: e.tensor_copy(out=xpn[:, :, 0:3], in_=xp[:, :, 256:259]), reads=[xp], writes=[xpn])
            op("dve", lambda e: e.tensor_tensor(out=V.rearrange("p (a b) -> p a b", b=8), in0=PK[:, 0:16].rearrange("p (a b) -> p a b", b=8),
                                                in1=DTB[:, None, :].to_broadcast([128, 2, 8]), op=ALU.add), reads=[PK, small], writes=DTB_)
            op("act", lambda e: e.activation(out=U, in_=V, func=AF.Abs), reads=DTB_, writes=DTB_)
            op("act", lambda e: e.activation(out=U, in_=U, func=AF.Exp, scale=-1.0), reads=DTB_, writes=DTB_)
            op("dve", lambda e: e.tensor_scalar(out=W, in0=U, scalar1=1.0, scalar2=None, op0=ALU.add), reads=DTB_, writes=DTB_)
            op("dve", lambda e: e.tensor_scalar(out=Y, in0=U, scalar1=-0.33, scalar2=0.99, op0=ALU.mult, op1=ALU.add), reads=DTB_, writes=DTB_)
            op("dve", lambda e: e.tensor_tensor(out=Y, in0=Y, in1=U, op=ALU.mult), reads=DTB_, writes=DTB_)
            for _ in range(3):
                op("act", lambda e: e.activation(out=T1, in_=Y, func=AF.Exp, scale=-1.0), reads=DTB_, writes=DTB_)
                op("dve", lambda e: e.tensor_tensor(out=T1, in0=T1, in1=W, op=ALU.mult), reads=DTB_, writes=DTB_)
                op("dve", lambda e: e.scalar_tensor_tensor(out=Y, in0=Y, scalar=-1.0, in1=T1, op0=ALU.add, op1=ALU.add), reads=DTB_, writes=DTB_)
            op("dve", lambda e: e.scalar_tensor_tensor(out=DT, in0=V, scalar=0.0, in1=Y, op0=ALU.max, op1=ALU.add), reads=DTB_, writes=DTB_)
            op("dve", lambda e: e.tensor_tensor(out=AA.rearrange("p (a b) -> p a b", b=8), in0=DT.rearrange("p (a b) -> p a b", b=8),
                                                in1=ABC[:, None, :].to_broadcast([128, 2, 8]), op=ALU.mult), reads=DTB_ + [small.b], writes=DTB_)
            op("dve", lambda e: e.tensor_scalar(out=DTF, in0=DT, scalar1=fl, scalar2=None, op0=ALU.mult), reads=DTB_ + [flags.b], writes=DTB_)
            CB = [nb(), nb(), nb(), nb()]
            for ct in range(8):
                bk = CB[ct // 2]
                o = bk[:, (ct % 2) * 256:(ct % 2 + 1) * 256]
                for k in range(4):
                    op("pe", lambda e, k=k, ct=ct, o=o: e.matmul(o, lhsT=convdiag[:, k * 8 + ct, :], rhs=xp[:, ct, k:k + 256],
                                                                start=(k == 0), stop=False), reads=[convdiag, xp], writes=[bk])
                op("pe", lambda e, ct=ct, o=o: e.matmul(o, lhsT=cbrow[0:1, ct * 128:(ct + 1) * 128], rhs=onesrow[0:1, :],
                                                        start=False, stop=True), reads=[cbrow, onesrow], writes=[bk])
            for b3 in range(4):
                pv = CB[b3][:, :].rearrange("p (a b) -> p a b", b=256)
                for q2 in range(2):
                    pvh = pv[:, q2:q2 + 1, :]
                    sl = (2 * b3 + q2) % 6
                    op("act", lambda e, pvh=pvh, sl=sl: e.activation(out=tcv2[:, sl:sl + 1, :], in_=pvh, func=AF.Tanh),
                       reads=[CB[b3]], writes=[tcvb[sl]])
                    op("dve", lambda e, b3=b3, q2=q2, pvh=pvh, sl=sl: e.scalar_tensor_tensor(
                        out=xc2[:, 2 * b3 + q2:2 * b3 + q2 + 1, :], in0=tcv2[:, sl:sl + 1, :], scalar=1.0, in1=pvh, op0=ALU.add, op1=ALU.mult),
                       reads=[tcvb[sl], CB[b3]], writes=[xcsl[2 * b3 + q2]])
            allw = sorted(set(i for xb_ in xcsl for i in xb_.w))
            for hf_ in range(2):
                xc_[hf_].b.w = list(allw); xc_[hf_].b.r = []
            tcv2.b.w = sorted(set(i for tb_ in tcvb for i in tb_.w))
            tcv2.b.r = sorted(set(i for tb_ in tcvb for i in tb_.r))
            DB = nb()
            op("pe", lambda e: e.matmul(DB[:, 0:16], lhsT=TRI, rhs=AA, start=True, stop=True), reads=[cf32] + DTB_, writes=[DB])
            op("pe", lambda e: e.matmul(DB[:, 16:32], lhsT=ONES, rhs=AA, start=True, stop=True), reads=[cf32] + DTB_, writes=[DB])
            op("act", lambda e: e.activation(out=ACS, in_=DB[:, 0:16], func=AF.Copy), reads=[DB], writes=DTB_)
            op("act", lambda e: e.activation(out=CD, in_=DB[:, 16:32], func=AF.Exp), reads=[DB], writes=DTB_)
            op("dve", lambda e: e.tensor_tensor(out=DTE, in0=DB[:, 16:32], in1=ACS, op=ALU.subtract), reads=[DB] + DTB_, writes=DTB_)
            op("act", lambda e: e.activation(out=DTE, in_=DTE, func=AF.Exp), reads=DTB_, writes=DTB_)
            op("act", lambda e: e.activation(out=EA, in_=ACS, func=AF.Exp), reads=DTB_, writes=DTB_)
            op("dve", lambda e: e.tensor_tensor(out=DTE, in0=DTE, in1=DTF, op=ALU.mult), reads=DTB_, writes=DTB_)

        if STOP_AFTER != 1 and MAIN_PAIRS:
            frontend(0); frontend(1)
            for p in range(NPAIR):
                frontend(2 * p + 2)
                if p + 1 < NPAIR:
                    frontend(2 * p + 3)
                prefix_pair(p)
            frontend(2 * NPAIR + 1)
            backend2(2 * NPAIR)
            frontend(NPRE); frontend(NPRE + 1)
            backend2(2 * NPAIR + 1)
            for pm in range(NMAIN // 2):
                if pm + 1 < NMAIN // 2:
                    frontend(NPRE + 2 * pm + 2); frontend(NPRE + 2 * pm + 3)
                main_pair_pre(pm)
                backend2(NPRE + 2 * pm, pre_done=True)
                backend2(NPRE + 2 * pm + 1, pre_done=True)
        elif STOP_AFTER != 1:
            frontend(0); frontend(1)
            for p in range(NPAIR):
                frontend(2 * p + 2)
                if p + 1 < NPAIR:
                    frontend(2 * p + 3)
                prefix_pair(p)
            for s in range(2 * NPAIR, NSLOT):
                if s + 1 < NSLOT:
                    frontend(s + 1)
                backend2(s)

        S.barrier(lambda e: e.memset(neghalf[:, 16:32], -0.5))
        p1.close()

        p2 = root.enter_context(contextlib.ExitStack())
        mod2 = sb(p2, "mod2", [128, 3 * D])
        SHIFT2 = mod2[:, 0:1024]; G2 = mod2[:, 1024:2048]; GATE2 = mod2[:, 2048:3072]
        op("sp", lambda e: e.dma_start(out=mod2[:], in_=modscr), reads=[modscr_b], writes=[mod2], dma=True)
        h2T = sb(p2, "h2T", [128, 8, NMAIN * 128], BF16)
        comb = sb(p2, "comb", [128, NMAIN, 32])
        wr = sb(p2, "wr", [128, 8, 36], BF16)
        brbc = sb(p2, "brbc", [128, 36])
        wg = [sb(p2, "wg%d" % i, [128, 8, 512], BF16) for i in range(2)]
        wd = [sb(p2, "wd%d" % i, [128, 2, D], BF16) for i in range(2)]
        sg = [sb(p2, "sg%d" % i, [128, 2, 512], BF16) for i in range(2)]
        actT = [sb(p2, "actT%d" % i, [128, 2, 512], BF16) for i in range(2)]
        junk2 = sb(p2, "junk2", [128, D], BF16)
        ss2 = [sb(p2, "ss2_%d" % i, [128, 4]) for i in range(2)]
        tmp2 = [sb(p2, "tmp2_%d" % i, [128, D]) for i in range(2)]
        hb2t = [sb(p2, "hb2_%d" % i, [128, D], BF16) for i in range(2)]
        lgA = sb(p2, "lgA", [128, NMAIN, 36])
        rwa = sb(p2, "rwa", [128, 416])
        rwb = [sb(p2, "rwb%d" % i, [128, NMAIN, 32]) for i in range(2)]

        op("pool", lambda e: e.dma_start(out=wr[:, :, 0:4], in_=w_group.rearrange("(p k) n -> p k n", k=8)), writes=[wr], dma=True)
        op("pool", lambda e: e.dma_start(out=wr[:, :, 4:36], in_=w_expert.rearrange("(p k) n -> p k n", k=8)), writes=[wr], dma=True)
        op("sp", lambda e: e.dma_start(out=brbc[:, 0:4], in_=b_group.partition_broadcast(128)), writes=[brbc], dma=True)
        op("sp", lambda e: e.dma_start(out=brbc[:, 4:36], in_=b_expert.partition_broadcast(128)), writes=[brbc], dma=True)

        def load_expert(ei):
            slot = ei % 2
            op("pool", lambda e: e.dma_start(out=wg[slot][:, :, 0:256], in_=w_gate[ei].rearrange("(p k) f -> p k f", k=8)),
               writes=[wg[slot]], dma=True)
            op("pool", lambda e: e.dma_start(out=wg[slot][:, :, 256:512], in_=w_up[ei].rearrange("(p k) f -> p k f", k=8)),
               writes=[wg[slot]], dma=True)
            op("pool", lambda e: e.dma_start(out=wd[slot][:], in_=w_down[ei].rearrange("(j t) d -> j t d", t=2)),
               writes=[wd[slot]], dma=True)
            for t in range(2):
                op("pool", lambda e, t=t: e.tensor_tensor(out=wd[slot][:, t, :], in0=wd[slot][:, t, :], in1=GATE2, op=ALU.mult),
                   reads=[mod2], writes=[wd[slot]])

        if STOP_AFTER == 0:
            load_expert(0)
        BT = B[0]
        tv = bfv(BT).rearrange("p (a b) -> p a b", b=128)
        for j in range(NMAIN if STOP_AFTER == 0 else 0):
            par = j % 2
            xt = xres_t[:, j, :]; xb = xres_b[j]
            sst = ss2[par]
            op("act", lambda e, xt=xt, sst=sst: e.activation(out=junk2[:], in_=xt, func=AF.Square, accum_out=sst[:, 0:1]),
               reads=[xb], writes=[junk2, sst])
            rstd_from_ss(sst[:, 0:1], float(D), 1, sst, [sst])
            tf = tmp2[par]
            op("dve", lambda e, xt=xt, sst=sst, tf=tf: e.scalar_tensor_tensor(out=tf[:], in0=xt, scalar=sst[:, 0:1], in1=G2,
                                                                              op0=ALU.mult, op1=ALU.mult),
               reads=[xb, sst, mod2], writes=[tf])
            hbt = hb2t[par]
            op("pool", lambda e, tf=tf, hbt=hbt: e.tensor_tensor(out=hbt[:], in0=tf[:], in1=SHIFT2, op=ALU.add),
               reads=[tf, mod2], writes=[hbt])
            for kt in range(8):
                op("pe", lambda e, kt=kt, hbt=hbt: e.transpose(out=tv[:, kt, :], in_=hbt[:, kt::8], identity=IDENT),
                   reads=[hbt, cbf], writes=[BT])
            op("act", lambda e, j=j: e.activation(out=h2T[:, :, j * 128:(j + 1) * 128], in_=tv, func=AF.Copy), reads=[BT], writes=[h2T])
            for kt in range(8):
                op("pe", lambda e, kt=kt, j=j: e.matmul(B[1][:, 0:36], lhsT=h2T[:, kt, j * 128:(j + 1) * 128], rhs=wr[:, kt, :],
                                                        start=(kt == 0), stop=(kt == 7)), reads=[h2T, wr], writes=[B[1]])
            op("dve", lambda e, j=j: e.tensor_tensor(out=lgA[:, j, :], in0=B[1][:, 0:36], in1=brbc[:], op=ALU.add),
               reads=[B[1], brbc], writes=[lgA])

        if STOP_AFTER == 0:
            GL = lgA[:, :, 0:4]
            EL = lgA[:, :, 4:36]
            EL4 = EL.rearrange("p c (a b) -> p c a b", b=8)
            g1 = rwa[:, 0:16]; gs = rwa[:, 16:32]
            gd = rwa[:, 32:96].rearrange("p (c a) -> p c a", a=4)
            oh = rwa[:, 96:160].rearrange("p (c a) -> p c a", a=4)
            gw = rwa[:, 160:224].rearrange("p (c a) -> p c a", a=4)
            m1 = rwa[:, 224:288].rearrange("p (c a) -> p c a", a=4)
            m2 = rwa[:, 288:352].rearrange("p (c a) -> p c a", a=4)
            dn = rwa[:, 352:416].rearrange("p (c a) -> p c a", a=4)
            b4 = lambda t: t[:, :, :, None].to_broadcast([128, NMAIN, 4, 8])
            v4 = lambda t: t[:].rearrange("p c (a b) -> p c a b", b=8)
            RWA = [rwa, rwb[0], rwb[1]]
            op("dve", lambda e: e.tensor_reduce(out=g1, in_=GL, axis=AX.X, op=ALU.max), reads=[lgA], writes=RWA)
            op("dve", lambda e: e.tensor_tensor(out=gd, in0=GL, in1=g1[:, :, None].to_broadcast([128, NMAIN, 4]), op=ALU.subtract),
               reads=[lgA] + RWA, writes=RWA)
            op("act", lambda e: e.activation(out=gd, in_=gd, func=AF.Exp), reads=RWA, writes=RWA)
            op("dve", lambda e: e.tensor_reduce(out=gs, in_=gd, axis=AX.X, op=ALU.add), reads=RWA, writes=RWA)
            op("dve", lambda e: e.reciprocal(out=gs, in_=gs), reads=RWA, writes=RWA)
            op("dve", lambda e: e.tensor_tensor(out=oh, in0=GL, in1=g1[:, :, None].to_broadcast([128, NMAIN, 4]), op=ALU.is_equal),
               reads=[lgA] + RWA, writes=RWA)
            op("dve", lambda e: e.tensor_tensor(out=gw, in0=oh, in1=gs[:, :, None].to_broadcast([128, NMAIN, 4]), op=ALU.mult),
               reads=RWA, writes=RWA)
            op("dve", lambda e: e.tensor_reduce(out=m1, in_=EL4, axis=AX.X, op=ALU.max), reads=[lgA], writes=RWA)
            op("dve", lambda e: e.tensor_tensor(out=v4(rwb[0]), in0=EL4, in1=b4(m1), op=ALU.is_equal), reads=[lgA] + RWA, writes=RWA)
            op("dve", lambda e: e.scalar_tensor_tensor(out=rwb[1][:], in0=rwb[0][:], scalar=-1e30, in1=EL, op0=ALU.mult, op1=ALU.add),
               reads=[lgA] + RWA, writes=RWA)
            op("dve", lambda e: e.tensor_reduce(out=m2, in_=v4(rwb[1]), axis=AX.X, op=ALU.max), reads=RWA, writes=RWA)
            op("dve", lambda e: e.tensor_tensor(out=v4(rwb[0]), in0=EL4, in1=b4(m2), op=ALU.is_ge), reads=[lgA] + RWA, writes=RWA)
            op("dve", lambda e: e.tensor_tensor(out=v4(rwb[1]), in0=EL4, in1=b4(m1), op=ALU.subtract), reads=[lgA] + RWA, writes=RWA)
            op("act", lambda e: e.activation(out=rwb[1][:], in_=rwb[1][:], func=AF.Exp), reads=RWA, writes=RWA)
            op("dve", lambda e: e.tensor_tensor(out=rwb[1][:], in0=rwb[1][:], in1=rwb[0][:], op=ALU.mult), reads=RWA, writes=RWA)
            op("dve", lambda e: e.tensor_reduce(out=dn, in_=v4(rwb[1]), axis=AX.X, op=ALU.add), reads=RWA, writes=RWA)
            op("dve", lambda e: e.reciprocal(out=dn, in_=dn), reads=RWA, writes=RWA)
            op("dve", lambda e: e.tensor_tensor(out=dn, in0=dn, in1=gw, op=ALU.mult), reads=RWA, writes=RWA)
            op("dve", lambda e: e.tensor_tensor(out=v4(comb), in0=v4(rwb[1]), in1=b4(dn), op=ALU.mult), reads=RWA, writes=[comb])

        GU = [B[2], B[3], B[4], B[5]]
        YD = [[B[6], B[7]], [B[0], B[1]]]
        ydi = 0
        for ei in range(N_EXPERTS_RUN if STOP_AFTER == 0 else 0):
            slot = ei % 2
            if ei + 1 < N_EXPERTS_RUN:
                load_expert(ei + 1)
            wgt = wg[slot]; wdt = wd[slot]
            for G in range(4):
                sgt = sg[G % 2]; at = actT[G % 2]
                for part in range(4):
                    bk = GU[part]
                    c0 = (part // 2) * 256 + (part % 2)
                    for kt in range(8):
                        op("pe", lambda e, kt=kt, bk=bk, c0=c0, G=G, wgt=wgt: e.matmul(
                            bk[:, :], lhsT=wgt[:, kt, c0:c0 + 255:2], rhs=h2T[:, kt, G * 512:(G + 1) * 512],
                            start=(kt == 0), stop=(kt == 7)), reads=[wgt, h2T], writes=[bk])
                for ft in range(2):
                    op("act", lambda e, ft=ft, sgt=sgt: e.activation(out=sgt[:, ft, :], in_=GU[ft][:, :], func=AF.Silu),
                       reads=[GU[ft]], writes=[sgt])
                    op("dve", lambda e, ft=ft, sgt=sgt, at=at: e.tensor_tensor(out=at[:, ft, :], in0=GU[2 + ft][:, :], in1=sgt[:, ft, :],
                                                                              op=ALU.mult), reads=[GU[2 + ft], sgt], writes=[at])
                for tt in range(4):
                    jt = G * 4 + tt
                    yb = YD[ydi % 2]; ydi += 1
                    for half in range(2):
                        bk = yb[half]
                        for ft in range(2):
                            op("pe", lambda e, ft=ft, half=half, bk=bk, tt=tt, at=at, wdt=wdt: e.matmul(
                                bk[:, :], lhsT=at[:, ft, tt * 128:(tt + 1) * 128], rhs=wdt[:, ft, half * 512:(half + 1) * 512],
                                start=(ft == 0), stop=(ft == 1)), reads=[at, wdt], writes=[bk])
                        op("dve", lambda e, half=half, bk=bk, jt=jt, ei=ei: e.scalar_tensor_tensor(
                            out=xres_t[:, jt, half * 512:(half + 1) * 512], in0=bk[:, :], scalar=comb[:, jt, ei:ei + 1],
                            in1=xres_t[:, jt, half * 512:(half + 1) * 512], op0=ALU.mult, op1=ALU.add),
                           reads=[bk, comb], writes=[xres_b[jt]])
        for j in range(NMAIN):
            op("sp", lambda e, j=j: e.dma_start(out=out_d[j * 128:(j + 1) * 128, :], in_=xres_t[:, j, :]), reads=[xres_b[j]], dma=True)
        S.run()
        print('[sched] ops=%d sim_makespan=%.1f us' % (len(S.all), getattr(S, 'sim_makespan', 0.0)))
    return nc


_CONST = {}


def _consts():
    if not _CONST:
        i = np.arange(128)
        tri = (i[:, None] <= i[None, :]).astype(np.float32)
        slm = (i[:, None] > i[None, :]).astype(np.float32)
        ones = np.ones((128, 128), np.float32)
        invf = (500000.0 ** (-np.arange(8, dtype=np.float32) * 2.0 / 16.0)).astype(np.float32)
        cf32 = np.concatenate([tri, slm, ones, np.broadcast_to(invf, (128, 8))], axis=1).astype(np.float32)
        ident = np.eye(128, dtype=np.float32)
        cbf = np.concatenate([ident, tri, slm], axis=1).astype(ml_dtypes.bfloat16)
        _CONST["cf32"] = np.ascontiguousarray(cf32)
        _CONST["cbf"] = np.ascontiguousarray(cbf)
    return _CONST


_NC_CACHE = {}


def kernel(x, c, positions, norm1_w, norm2_w, w_ada, b_ada, w_in, conv_w, conv_b, dt_bias, a_log, d_skip,
           ssd_norm_w, q_norm_w, k_norm_w, sinks, w_out, w_group, b_group, w_expert, b_expert, w_gate, w_up, w_down):
    f32 = lambda a: np.ascontiguousarray(np.asarray(a, dtype=np.float32))
    x = f32(x); c = f32(c)
    positions = np.ascontiguousarray(np.asarray(positions, dtype=np.int32))
    cst = _consts()
    shared = {
        "cf32": cst["cf32"], "cbf": cst["cbf"],
        "norm1_w": f32(norm1_w), "norm2_w": f32(norm2_w), "w_ada": f32(w_ada), "b_ada": f32(b_ada),
        "w_in": f32(w_in), "conv_w": f32(conv_w), "conv_b": f32(conv_b), "dt_bias": f32(dt_bias),
        "a_log": f32(a_log), "d_skip": f32(d_skip), "ssd_norm_w": f32(ssd_norm_w), "q_norm_w": f32(q_norm_w),
        "k_norm_w": f32(k_norm_w), "sinks": f32(sinks), "w_out": f32(w_out), "w_group": f32(w_group),
        "b_group": f32(b_group), "w_expert": f32(w_expert), "b_expert": f32(b_expert),
        "w_gate": f32(w_gate[:N_EXPERTS_RUN]), "w_up": f32(w_up[:N_EXPERTS_RUN]), "w_down": f32(w_down[:N_EXPERTS_RUN]),
    }
    in_maps = []
    SEQ = x.shape[1]
    for core in range(NCORES):
        b, q = divmod(core, 4)
        t0 = q * NMAIN * 128 - NPRE * 128
        xe = np.zeros((NSLOT * 128, D), np.float32)
        flg = np.zeros((128, NSLOT), np.float32)
        posi = np.zeros((128, NMAIN + 1), np.int32)
        for s in range(NSLOT):
            ts = t0 + s * 128
            if ts >= 0:
                xe[s * 128:(s + 1) * 128] = x[b, ts:ts + 128]
                flg[:, s] = 1.0
                if s >= HALO:
                    posi[:, s - HALO] = positions[b, ts:ts + 128]
        m = dict(shared)
        m["xe"] = xe; m["flags"] = flg; m["posi"] = posi
        m["cvec"] = np.ascontiguousarray(c[b].reshape(128, 8))

        in_maps.append(m)
    if "nc" not in _NC_CACHE:
        _NC_CACHE["nc"] = build_program()
    nc = _NC_CACHE["nc"]
    res = run_bass_kernel_spmd(nc, in_maps, core_ids=list(range(NCORES)))
    out = np.empty((x.shape[0], SEQ, D), np.float32)
    for core in range(NCORES):
        b, q = divmod(core, 4)
        out[b, q * 2048:(q + 1) * 2048] = res.results[core]["out"]
    kernel.last_results = res
    return out
```

```python
import contextlib
import numpy as np
import ml_dtypes
import concourse.bass as bass
import concourse.mybir as mybir
from concourse.bass_utils import run_bass_kernel_spmd

F32 = mybir.dt.float32
BF16 = mybir.dt.bfloat16
I32 = mybir.dt.int32
AF = mybir.ActivationFunctionType
ALU = mybir.AluOpType
AX = mybir.AxisListType

NCORES = 8
D = 1024
NSLOT = 64
NMAIN = 16
NPRE = NSLOT - NMAIN
HALO = NPRE - 1
INW = 2312
EPS = 1e-6
NEXP = 32
TWO_PI = 6.283185307179586
C1 = 6.28125
C2 = TWO_PI - C1

DEBUG_X2 = False
N_EXPERTS_RUN = NEXP
N_PREFIX_SKIP = 0
STOP_AFTER = 0
USE_POW = True
STAGE_LIMIT = 99
MAIN_PAIRS = True
HOP = 0.8
NO_CC = False
CC_TEST = False


class Buf:
    __slots__ = ("name", "w", "r", "psum")

    def __init__(self, name="", psum=False):
        self.name = name
        self.w = []
        self.r = []
        self.psum = psum


class Tile:
    def __init__(self, t, name=""):
        self.t = t
        self.b = Buf(name)

    def __getitem__(self, idx):
        return self.t[idx]


class View:
    def __init__(self, ap, buf):
        self.t = ap
        self.b = buf

    def __getitem__(self, idx):
        return self.t[idx]


class Op:
    __slots__ = ("id", "eng", "fn", "deps", "dma", "cost", "xfer", "tok", "succ", "cc")

    def __init__(self, id, eng, fn, deps, dma, cost, xfer):
        self.id = id; self.eng = eng; self.fn = fn; self.deps = deps; self.dma = dma
        self.cost = cost; self.xfer = xfer; self.tok = None; self.succ = False; self.cc = False


class Probe:
    def __init__(self):
        self.name = None; self.args = (); self.kw = {}

    def __getattr__(self, name):
        def f(*args, **kw):
            self.name = name; self.args = args; self.kw = kw
            return self
        return f


def _fsz(ap):
    v = ap.free_size
    return v() if callable(v) else v


def _nb(ap):
    v = ap.nbytes
    return v() if callable(v) else v


class Sched:
    ENGS = ("pe", "act", "dve", "pool", "sp")
    NDSEM = 12
    WINDOW = 320

    def __init__(self, nc):
        self.nc = nc
        self.all = []
        self.fence = []
        self.leaves = set()
        self.reorder = True

    @staticmethod
    def est(eng, n, fp32):
        if eng == "pe":
            c = 0.11 if n <= 128 else n / 2400.0 + 0.02
            return c * (4 if fp32 else 1)
        if eng == "act":
            return 0.25 + n / 1200.0
        if eng == "dve":
            return 0.16 + n / 960.0
        if eng == "pool":
            return 0.45 + n / 450.0
        return 0.1

    def op(self, eng, fn, reads=(), writes=(), dma=False, n=64, fp32=False, nbytes=0, cc=False):
        reads = [x.b if isinstance(x, (Tile, View)) else x for x in reads]
        writes = [x.b if isinstance(x, (Tile, View)) else x for x in writes]
        deps = set(self.fence)
        for b in reads:
            deps.update(b.w)
            if b.psum:
                deps.update(i for i in b.r if self.all[i].eng != eng)
        for b in writes:
            deps.update(b.w)
            deps.update(b.r)
        oid = len(self.all)
        pr = Probe()
        fn(pr)
        if cc:
            cost = 2.0; xfer = 30.0
        elif dma:
            nbytes = _nb(pr.kw["out"])
            cost = 1.2 if eng == "pool" else 0.15
            xfer = 2.0 + nbytes / 150e3
        else:
            if pr.name == "matmul":
                n = _fsz(pr.kw["rhs"]); fp32 = pr.kw["rhs"].dtype == F32
            elif pr.name == "transpose":
                n = 128
            else:
                o_ = pr.kw.get("out", pr.args[0] if pr.args else None)
                n = _fsz(o_) if o_ is not None else 64
                if eng == "dve" and pr.name in ("tensor_tensor", "scalar_tensor_tensor"):
                    a0 = pr.kw.get("in0"); a1 = pr.kw.get("in1")
                    if a0 is not None and a1 is not None and a0.dtype == F32 and a1.dtype == F32 \
                            and str(a0.space) == str(a1.space):
                        n = int(n * 1.3)
            cost = self.est(eng, n, fp32)
            if eng == "pool" and pr.kw.get("op", None) == ALU.pow:
                cost = 1.6
            xfer = 0.0
        o = Op(oid, eng, fn, deps, dma or cc, cost, xfer)
        o.cc = cc
        self.all.append(o)
        for d in deps:
            if not self.all[d].succ:
                self.all[d].succ = True
                self.leaves.discard(d)
        self.leaves.add(oid)
        for b in writes:
            b.w = [oid]
            b.r = []
        for b in reads:
            if b not in writes:
                b.r.append(oid)
        return oid

    def barrier(self, fn):
        deps = set(self.leaves) | set(self.fence)
        oid = len(self.all)
        o = Op(oid, "dve", fn, deps, False, 0.1, 0.0)
        self.all.append(o)
        for d in deps:
            self.all[d].succ = True
        self.leaves = {oid}
        self.fence = [oid]

    def schedule(self):
        ops = self.all
        per = {e: [o.id for o in ops if o.eng == e] for e in self.ENGS}
        if not self.reorder:
            return per
        head = {e: 0 for e in self.ENGS}
        done = [False] * len(ops)
        fin = [0.0] * len(ops)
        free = {e: 0.0 for e in self.ENGS}
        dma_free = 0.0
        order = {e: [] for e in self.ENGS}
        remaining = len(ops)
        INF = 1e30
        while remaining:
            best = None
            for e in self.ENGS:
                lst = per[e]
                h = head[e]
                while h < len(lst) and done[lst[h]]:
                    h += 1
                head[e] = h
                cnt = 0
                k = h
                while k < len(lst) and cnt < self.WINDOW:
                    oid = lst[k]
                    k += 1
                    if done[oid]:
                        continue
                    cnt += 1
                    o = ops[oid]
                    rdy = 0.0
                    ok = True
                    for d in o.deps:
                        if not done[d]:
                            ok = False
                            break
                        fd = fin[d] - (HOP if (ops[d].eng == e and not ops[d].dma) else 0.0)
                        if fd > rdy:
                            rdy = fd
                    if not ok:
                        continue
                    st = rdy if rdy > free[e] else free[e]
                    key = (st, oid)
                    if best is None or key < best[0]:
                        best = (key, e, oid)
                    if rdy <= free[e]:
                        break
            assert best is not None, "scheduler stuck (cyclic deps?)"
            (st, _), e, oid = best
            o = ops[oid]
            done[oid] = True
            remaining -= 1
            free[e] = st + o.cost
            if o.dma:
                t0 = max(st + o.cost, dma_free)
                dma_free = t0 + (o.xfer - 2.0)
                fin[oid] = t0 + o.xfer
            else:
                fin[oid] = st + o.cost + HOP
            order[e].append(oid)
        self.sim_makespan = max(fin) if fin else 0.0
        return order

    def run(self):
        nc = self.nc
        ops = self.all
        order = self.schedule()
        cnt = {}
        rr = {}
        prevdma = {}
        for e in self.ENGS:
            for oid in order[e]:
                o = ops[oid]
                if o.cc:
                    s = "cc%d" % oid
                    cnt[s] = 1
                    o.tok = (s, 1)
                    prevdma[oid] = (s, 0)
                    continue
                if o.dma:
                    k = rr.get(e, 0)
                    rr[e] = (k + 1) % self.NDSEM
                    s = "d_%s_%d" % (e, k)
                    prevdma[oid] = (s, cnt.get(s, 0))
                    cnt[s] = cnt.get(s, 0) + 16
                else:
                    s = e
                    cnt[s] = cnt.get(s, 0) + 1
                o.tok = (s, cnt[s])
        streams = {}
        for e in self.ENGS:
            waited = {}
            st = []
            for oid in order[e]:
                o = ops[oid]
                need = {}
                for d in o.deps:
                    s, v = ops[d].tok
                    if v > need.get(s, 0):
                        need[s] = v
                if o.dma and prevdma[oid][1]:
                    s, v = prevdma[oid]
                    need[s] = max(need.get(s, 0), v)
                waits = []
                for s, v in need.items():
                    if e == "pe" and s == "pe":
                        continue
                    if waited.get(s, 0) >= v:
                        continue
                    waited[s] = v
                    waits.append((s, v))
                st.append((waits, o))
            if e == "sp":
                waits = [(s, v) for s, v in cnt.items() if waited.get(s, 0) < v]
                st.append((waits, None))
            streams[e] = st
        self._check_deadlock(streams)
        with contextlib.ExitStack() as stck:
            sems = {n: stck.enter_context(nc.semaphore("s_" + n)) for n in sorted(cnt)}
            block = stck.enter_context(nc.Block())

            def play(engname):
                def body(eh):
                    for waits, o in streams[engname]:
                        for (s, v) in waits:
                            eh.wait_ge(sems[s], v)
                        if o is None:
                            continue
                        ins = o.fn(eh)
                        if o.cc:
                            ins.then_inc(sems[o.tok[0]])
                        else:
                            ins.then_inc(sems[o.tok[0]], 16 if o.dma else 1)
                return body

            block.tensor(play("pe"))
            block.scalar(play("act"))
            block.vector(play("dve"))
            block.gpsimd(play("pool"))
            block.sync(play("sp"))

    def _check_deadlock(self, streams):
        val = {}
        pos = {e: 0 for e in self.ENGS}
        progress = True
        while progress:
            progress = False
            for e in self.ENGS:
                st = streams[e]
                while pos[e] < len(st):
                    waits, o = st[pos[e]]
                    if any(val.get(s, 0) < v for s, v in waits):
                        break
                    if o is not None:
                        val[o.tok[0]] = val.get(o.tok[0], 0) + (1 if o.cc else (16 if o.dma else 1))
                    pos[e] += 1
                    progress = True
        for e in self.ENGS:
            assert pos[e] == len(streams[e]), "DEADLOCK in engine %s at %d/%d" % (e, pos[e], len(streams[e]))


def build_program():
    nc = bass.Bass("TRN2", target_bir_lowering=False, dynamic_dma_scratch_size=4096)

    def din(name, shape, dt=F32):
        return nc.dram_tensor(name, list(shape), dt, kind="ExternalInput").ap()

    xe = din("xe", [NSLOT * 128, D])
    flags_d = din("flags", [128, NSLOT])
    posi_d = din("posi", [128, NMAIN + 1], I32)
    cvec_d = din("cvec", [128, 8])

    cf32_d = din("cf32", [128, 3 * 128 + 8])
    cbf_d = din("cbf", [128, 3 * 128], BF16)
    norm1_w = din("norm1_w", [D]); norm2_w = din("norm2_w", [D])
    w_ada = din("w_ada", [D, 6 * D]); b_ada = din("b_ada", [6 * D])
    w_in = din("w_in", [D, INW])
    conv_w = din("conv_w", [4, D]); conv_b = din("conv_b", [D])
    dt_bias = din("dt_bias", [8]); a_log = din("a_log", [8]); d_skip = din("d_skip", [8])
    ssd_norm_w = din("ssd_norm_w", [512])
    q_norm_w = din("q_norm_w", [64]); k_norm_w = din("k_norm_w", [64])
    sinks = din("sinks", [8])
    w_out = din("w_out", [D, D])
    w_group = din("w_group", [D, 4]); b_group = din("b_group", [4])
    w_expert = din("w_expert", [D, 32]); b_expert = din("b_expert", [32])
    w_gate = din("w_gate", [N_EXPERTS_RUN, D, 256]); w_up = din("w_up", [N_EXPERTS_RUN, D, 256])
    w_down = din("w_down", [N_EXPERTS_RUN, 256, D])
    out_d = nc.dram_tensor("out", [NMAIN * 128, D], F32, kind="ExternalOutput").ap()
    if DEBUG_X2:
        dbg_d = nc.dram_tensor("dbg", [NMAIN * 128, D], F32, kind="ExternalOutput").ap()

    S = Sched(nc)
    op = S.op

    with contextlib.ExitStack() as root:
        def sb(stack, name, shape, dt=F32):
            return Tile(stack.enter_context(nc.sbuf_tensor("sb_" + name, list(shape), dt)), name)

        banks = [Tile(root.enter_context(nc.psum_tensor("bank%d" % i, [128, 512], F32)), "bank%d" % i)
                 for i in range(8)]
        for bk_ in banks:
            bk_.b.psum = True

        def bfv(bank):
            return bank.t[:].bitcast(BF16)

        xres = [None] * NMAIN
        xres_t = root.enter_context(nc.sbuf_tensor("sb_xres", [128, NMAIN, D], F32))
        xres_b = [Buf("xres%d" % j) for j in range(NMAIN)]
        cf32 = sb(root, "cf32", [128, 3 * 128 + 8])
        cbf = sb(root, "cbf", [128, 3 * 128], BF16)
        flags = sb(root, "flags", [128, NSLOT])
        neghalf = sb(root, "neghalf", [128, 32])
        TRI = cf32[:, 0:128]; SLM = cf32[:, 128:256]; ONES = cf32[:, 256:384]; INVF = cf32[:, 384:392]
        IDENT = cbf[:, 0:128]; MASKC = cbf[:, 128:256]; MASKP = cbf[:, 256:384]

        modscr = nc.dram_tensor("modscr", [128, 3 * D], F32, kind="Internal").ap()

        op("sp", lambda e: e.dma_start(out=cf32[:], in_=cf32_d), writes=[cf32], dma=True)
        op("sp", lambda e: e.dma_start(out=cbf[:], in_=cbf_d), writes=[cbf], dma=True)
        op("sp", lambda e: e.dma_start(out=flags[:], in_=flags_d), writes=[flags], dma=True)
        op("dve", lambda e: e.memset(neghalf[:, 0:16], -0.5), writes=[neghalf])

        p1 = root.enter_context(contextlib.ExitStack())
        mod1 = sb(p1, "mod1", [128, 2 * D])
        SHIFT1 = mod1[:, 0:1024]; G1 = mod1[:, 1024:2048]
        win = sb(p1, "win", [128, 8, INW], BF16)
        wout = sb(p1, "wout", [128, 8, D], BF16)
        convdiag = sb(p1, "convdiag", [128, 32, 128], BF16)
        cbrow = sb(p1, "cbrow", [1, D], BF16)
        onesrow = sb(p1, "onesrow", [1, 256], BF16)
        dI = sb(p1, "dI", [128, 8, 128], BF16)
        small = sb(p1, "small", [128, 8 * 6])
        DTB = small[:, 0:8]; ABC = small[:, 8:16]; DSK = small[:, 16:24]; SNK = small[:, 24:32]
        ESINK = small[:, 32:40]
        negc = sb(p1, "negc", [128, 1])
        snw = sb(p1, "snw", [128, 512])
        qkw = sb(p1, "qkw", [128, 2, 64])
        cossin = sb(p1, "cossin", [128, 2, NMAIN + 1, 8])
        maskp0 = sb(p1, "maskp0", [128, 128], BF16)
        negm = sb(p1, "negm", [128, 3, 4, 128], BF16)

        with contextlib.ExitStack() as p0:
            mod = sb(p0, "mod", [128, 6 * D])
            GATE1 = mod[:, 2048:3072]
            cvec = sb(p0, "cvec", [128, 8])
            screp = sb(p0, "screp", [128, 8, 128])
            wst = [sb(p0, "wst%d" % i, [128, 8, 256]) for i in range(2)]
            nbc = sb(p0, "nbc", [128, D])
            cwT = sb(p0, "cwT", [128, 4, 8])
            cbst = sb(p0, "cbst", [1, D])
            posi = sb(p0, "posi", [128, NMAIN + 1], I32)
            posf = sb(p0, "posf", [128, NMAIN + 1])
            ang = sb(p0, "ang", [128, NMAIN + 1, 8])
            kk = sb(p0, "kk", [128, NMAIN + 1, 8])
            kki = sb(p0, "kki", [128, NMAIN + 1, 8], I32)
            mx = sb(p0, "mx", [128, 2])

            op("sp", lambda e: e.dma_start(out=cvec[:], in_=cvec_d), writes=[cvec], dma=True)
            op("sp", lambda e: e.dma_start(out=mod[:], in_=b_ada.partition_broadcast(128)), writes=[mod], dma=True)
            win_src = w_in.rearrange("(p k) n -> p k n", k=8)
            for lo, hi in ((0, 1156), (1156, INW)):
                op("pool", lambda e, lo=lo, hi=hi: e.dma_start(out=win[:, :, lo:hi], in_=win_src[:, :, lo:hi]),
                   writes=[win], dma=True)
            op("pool", lambda e: e.dma_start(out=wout[:], in_=w_out.rearrange("(p k) n -> p k n", k=8)),
               writes=[wout], dma=True)

            op("act", lambda e: e.activation(out=cvec[:], in_=cvec[:], func=AF.Silu), reads=[cvec], writes=[cvec])
            op("dve", lambda e: e.tensor_copy(out=screp[:], in_=cvec[:, :, None].to_broadcast([128, 8, 128])),
               reads=[cvec], writes=[screp])
            wada_src = w_ada.rearrange("(p k) n -> p k n", k=8)
            NB = 24
            for nb in range(NB):
                ws = wst[nb % 2]
                bk = banks[nb % 2]
                op("sp", lambda e, ws=ws, nb=nb: e.dma_start(out=ws[:], in_=wada_src[:, :, nb * 256:(nb + 1) * 256]),
                   writes=[ws], dma=True)
                for kt in range(8):
                    op("pe", lambda e, ws=ws, bk=bk, kt=kt: e.matmul(bk[:, 0:256], lhsT=screp[:, kt, :], rhs=ws[:, kt, :],
                                                                     start=(kt == 0), stop=(kt == 7)),
                       reads=[screp, ws], writes=[bk])
                op("dve", lambda e, bk=bk, nb=nb: e.tensor_tensor(out=mod[:, nb * 256:(nb + 1) * 256], in0=bk[:, 0:256],
                                                                 in1=mod[:, nb * 256:(nb + 1) * 256], op=ALU.add),
                   reads=[bk], writes=[mod])
            op("sp", lambda e: e.dma_start(out=nbc[:], in_=norm1_w.partition_broadcast(128)), writes=[nbc], dma=True)
            op("dve", lambda e: e.scalar_tensor_tensor(out=G1, in0=mod[:, 1024:2048], scalar=1.0, in1=nbc[:], op0=ALU.add, op1=ALU.mult),
               reads=[nbc, mod], writes=[mod1])
            op("dve", lambda e: e.tensor_copy(out=SHIFT1, in_=mod[:, 0:1024]), reads=[mod], writes=[mod1])
            op("sp", lambda e: e.dma_start(out=nbc[:], in_=norm2_w.partition_broadcast(128)), writes=[nbc], dma=True)
            op("dve", lambda e: e.scalar_tensor_tensor(out=mod[:, 4096:5120], in0=mod[:, 4096:5120], scalar=1.0, in1=nbc[:], op0=ALU.add, op1=ALU.mult),
               reads=[nbc], writes=[mod])
            modscr_b = Buf("modscr")
            op("sp", lambda e: e.dma_start(out=modscr, in_=mod[:, 3072:6144]), reads=[mod], writes=[modscr_b], dma=True)
            for kt in range(8):
                op("dve", lambda e, kt=kt: e.tensor_tensor(out=wout[:, kt, :], in0=wout[:, kt, :], in1=GATE1, op=ALU.mult),
                   reads=[mod], writes=[wout])
                op("pool", lambda e, kt=kt: e.tensor_scalar(out=win[:, kt, 768:1280], in0=win[:, kt, 768:1280],
                                                            scalar1=0.5, scalar2=1.0, op0=ALU.mult, op1=ALU.mult),
                   writes=[win])
            op("sp", lambda e: e.dma_start(out=cwT[:], in_=conv_w.rearrange("k (c p) -> p k c", p=128),
                                           allow_slow_non_contiguous=True), writes=[cwT], dma=True)
            for k in range(4):
                for ct in range(8):
                    op("dve", lambda e, k=k, ct=ct: e.tensor_scalar(out=convdiag[:, k * 8 + ct, :], in0=IDENT,
                                                                   scalar1=cwT[:, k, ct:ct + 1], scalar2=0.5,
                                                                   op0=ALU.mult, op1=ALU.mult),
                       reads=[cbf, cwT], writes=[convdiag])
            op("sp", lambda e: e.dma_start(out=cbst[:], in_=conv_b.rearrange("(o n) -> o n", o=1)), writes=[cbst], dma=True)
            op("dve", lambda e: e.tensor_scalar(out=cbrow[:], in0=cbst[:], scalar1=0.5, scalar2=None, op0=ALU.mult),
               reads=[cbst], writes=[cbrow])
            op("dve", lambda e: e.memset(onesrow[:], 1.0), writes=[onesrow])
            for i, src in enumerate((dt_bias, a_log, d_skip, sinks)):
                op("sp", lambda e, i=i, src=src: e.dma_start(out=small[:, i * 8:(i + 1) * 8], in_=src.partition_broadcast(128)),
                   writes=[small], dma=True)
            op("act", lambda e: e.activation(out=ABC, in_=ABC, func=AF.Exp), reads=[small], writes=[small])
            op("dve", lambda e: e.tensor_scalar(out=ABC, in0=ABC, scalar1=-1.0, scalar2=None, op0=ALU.mult),
               reads=[small], writes=[small])
            for h in range(8):
                op("dve", lambda e, h=h: e.tensor_scalar(out=dI[:, h, :], in0=IDENT, scalar1=DSK[:, h:h + 1], scalar2=None,
                                                         op0=ALU.mult), reads=[cbf, small], writes=[dI])
            op("sp", lambda e: e.dma_start(out=snw[:], in_=ssd_norm_w.partition_broadcast(128)), writes=[snw], dma=True)
            op("sp", lambda e: e.dma_start(out=qkw[:, 0, :], in_=q_norm_w.partition_broadcast(128)), writes=[qkw], dma=True)
            op("sp", lambda e: e.dma_start(out=qkw[:, 1, :], in_=k_norm_w.partition_broadcast(128)), writes=[qkw], dma=True)
            op("dve", lambda e: e.tensor_reduce(out=mx[:, 0:1], in_=qkw[:, 0, :], axis=AX.X, op=ALU.max,
                                                apply_absolute_value=True), reads=[qkw], writes=[mx])
            op("dve", lambda e: e.tensor_reduce(out=mx[:, 1:2], in_=qkw[:, 1, :], axis=AX.X, op=ALU.max,
                                                apply_absolute_value=True), reads=[qkw], writes=[mx])
            op("dve", lambda e: e.tensor_tensor(out=negc[:], in0=mx[:, 0:1], in1=mx[:, 1:2], op=ALU.mult),
               reads=[mx], writes=[negc])
            op("dve", lambda e: e.tensor_scalar(out=negc[:], in0=negc[:], scalar1=-8.0, scalar2=None, op0=ALU.mult),
               reads=[negc], writes=[negc])
            op("act", lambda e: e.activation(out=ESINK, in_=SNK, func=AF.Exp, bias=negc[:]), reads=[small, negc],
               writes=[small])
            op("sp", lambda e: e.dma_start(out=posi[:], in_=posi_d), writes=[posi], dma=True)
            op("dve", lambda e: e.tensor_copy(out=posf[:], in_=posi[:]), reads=[posi], writes=[posf])
            op("dve", lambda e: e.tensor_tensor(out=ang[:], in0=posf[:, :, None].to_broadcast([128, NMAIN + 1, 8]),
                                                in1=INVF[:, None, :].to_broadcast([128, NMAIN + 1, 8]), op=ALU.mult),
               reads=[posf, cf32], writes=[ang])
            op("dve", lambda e: e.tensor_scalar(out=kk[:], in0=ang[:], scalar1=1.0 / TWO_PI, scalar2=None, op0=ALU.mult),
               reads=[ang], writes=[kk])
            op("dve", lambda e: e.tensor_copy(out=kki[:], in_=kk[:]), reads=[kk], writes=[kki])
            op("dve", lambda e: e.tensor_copy(out=kk[:], in_=kki[:]), reads=[kki], writes=[kk])
            op("dve", lambda e: e.scalar_tensor_tensor(out=ang[:], in0=kk[:], scalar=-C1, in1=ang[:], op0=ALU.mult, op1=ALU.add),
               reads=[kk, ang], writes=[ang])
            op("dve", lambda e: e.scalar_tensor_tensor(out=ang[:], in0=kk[:], scalar=-C2, in1=ang[:], op0=ALU.mult, op1=ALU.add),
               reads=[kk, ang], writes=[ang])
            for _ in range(2):
                for cmpop, sgn in ((ALU.is_gt, -1.0), (ALU.is_lt, 1.0)):
                    thr = np.pi if sgn < 0 else -np.pi
                    op("dve", lambda e, cmpop=cmpop, thr=thr, sgn=sgn: e.tensor_scalar(
                        out=kk[:], in0=ang[:], scalar1=float(thr), scalar2=float(sgn * TWO_PI), op0=cmpop, op1=ALU.mult),
                       reads=[ang], writes=[kk])
                    op("dve", lambda e: e.tensor_tensor(out=ang[:], in0=ang[:], in1=kk[:], op=ALU.add),
                       reads=[kk, ang], writes=[ang])
            op("act", lambda e: e.activation(out=cossin[:, 1, :, :], in_=ang[:], func=AF.Sin), reads=[ang], writes=[cossin])
            op("dve", lambda e: e.tensor_scalar(out=ang[:], in0=ang[:], scalar1=float(np.pi / 2), scalar2=None, op0=ALU.add),
               reads=[ang, cossin], writes=[ang])
            op("dve", lambda e: e.tensor_scalar(out=kk[:], in0=ang[:], scalar1=float(np.pi), scalar2=float(-TWO_PI),
                                                op0=ALU.is_gt, op1=ALU.mult), reads=[ang], writes=[kk])
            op("dve", lambda e: e.tensor_tensor(out=ang[:], in0=ang[:], in1=kk[:], op=ALU.add), reads=[kk, ang], writes=[ang])
            op("act", lambda e: e.activation(out=cossin[:, 0, :, :], in_=ang[:], func=AF.Sin), reads=[ang], writes=[cossin])
            op("dve", lambda e: e.tensor_scalar(out=maskp0[:], in0=MASKP, scalar1=flags[:, HALO:HALO + 1], scalar2=None,
                                                op0=ALU.mult), reads=[cbf, flags], writes=[maskp0])
            for mi, (msrc, mb_) in enumerate(((MASKP, cbf), (MASKC, cbf), (maskp0[:], maskp0))):
                for i4 in range(4):
                    op("dve", lambda e, mi=mi, i4=i4, msrc=msrc: e.tensor_scalar(out=negm[:, mi, i4, :], in0=msrc, scalar1=-1.0, scalar2=30000.0,
                                                                                op0=ALU.add, op1=ALU.mult), reads=[mb_], writes=[negm])
            if DEBUG_X2 and STOP_AFTER == 1:
                op("sp", lambda e: e.dma_start(out=dbg_d[0:128, :], in_=SHIFT1), reads=[mod1], dma=True)
                op("sp", lambda e: e.dma_start(out=dbg_d[128:256, :], in_=G1), reads=[mod1], dma=True)
                op("sp", lambda e: e.dma_start(out=dbg_d[256:384, 0:272], in_=cossin[:].rearrange("p a b c -> p (a b c)")), reads=[cossin], dma=True)
                op("sp", lambda e: e.dma_start(out=dbg_d[384:512, 0:48], in_=small[:]), reads=[small], dma=True)
                op("sp", lambda e: e.dma_start(out=dbg_d[640:768, :], in_=mod[:, 2048:3072]), reads=[mod], dma=True)
            S.barrier(lambda e: e.memset(neghalf[:, 16:32], -0.5))

        shr = sb(p1, "shr", [128, D])
        tmpf1 = shr
        xpf = [sb(p1, "xpf%d" % i, [128, D]) for i in range(2)]
        yo = sb(p1, "yo", [128, 512])
        ss = [sb(p1, "ss%d" % i, [128, 4]) for i in range(2)]
        hb = [sb(p1, "hb%d" % i, [128, D], BF16) for i in range(2)]
        hT2 = [sb(p1, "hT2_%d" % i, [128, 8, 256], BF16) for i in range(2)]
        hT = [View(hT2[i][:, :, 0:128], hT2[i].b) for i in range(2)]
        xpre2 = [sb(p1, "xpre2_%d" % i, [128, 8, 260], BF16) for i in range(2)]
        xpre = [View(xpre2[i][:, :, 0:132], xpre2[i].b) for i in range(2)]
        tcv2 = sb(p1, "tcv2", [128, 6, 256], BF16)
        tcv = View(tcv2[:].rearrange("p a b -> p (a b)")[:, 0:1024].rearrange("p (a b) -> p a b", b=128), tcv2.b)
        xc2 = sb(p1, "xc2", [128, 8, 256], BF16)
        xc_ = [View(xc2[:, :, i * 128:(i + 1) * 128], Buf("xc%d" % i)) for i in range(2)]
        dtw2 = sb(p1, "dtw2", [128, 12, 16])
        dtw_ = [View(dtw2[:, :, i * 8:(i + 1) * 8], Buf("dtw%d" % i)) for i in range(2)]
        xdt_ = [sb(p1, "xdt%d" % i, [128, 8, 64], BF16) for i in range(2)]
        xdte2 = sb(p1, "xdte2", [128, 2, 8, 64], BF16)
        xdte_ = [View(xdte2[:, i, :, :], Buf("xdte%d" % i)) for i in range(2)]
        xstok_ = [sb(p1, "xstok%d" % i, [128, 8, 64], BF16) for i in range(2)]
        btok2 = sb(p1, "btok2", [128, 2, 2, 128], BF16)
        btok_ = [View(btok2[:, i, :, :], Buf("btok%d" % i)) for i in range(2)]
        R = sb(p1, "R", [128, 8, 64])
        Rb = sb(p1, "Rb", [128, 8, 64], BF16)
        gtm = sb(p1, "gtm", [128, 2, 128], BF16)
        tz = sb(p1, "tz", [128, 512], BF16)
        sz_ = [sb(p1, "sz%d" % i, [128, 512], BF16) for i in range(2)]
        ssg = sb(p1, "ssg", [128, 4])
        sq = sb(p1, "sq", [128, 10, 64], BF16)
        junk = View(sq[:].rearrange("p a b -> p (a b)")[:, 0:512], sq.b)
        ssq = sb(p1, "ssq", [128, 16])
        qn = sb(p1, "qn", [128, 10, 64])
        Lm = View(qn[:].rearrange("p a b -> p (a b)")[:, 0:512].rearrange("p (a b) -> p a b", b=128), qn.b)
        rt = sb(p1, "rt", [128, 4, 10, 8])
        qr_ = [sb(p1, "qr%d" % i, [128, 10, 64], BF16) for i in range(2)]
        qT = sb(p1, "qT", [128, 4, 128], BF16)
        kT = [sb(p1, "kT%d" % i, [128, 128], BF16) for i in range(2)]
        vaug = [sb(p1, "vaug%d" % i, [128, 2, 65], BF16) for i in range(2)]
        pT = sb(p1, "pT", [128, 4, 512], BF16)
        decT = View(pT[:].rearrange("p a b -> p (a b)")[:, 0:1024].rearrange("p (a b) -> p a b", b=128), pT.b)
        den = sb(p1, "den", [128, 8])
        mix = sb(p1, "mix", [128, D], BF16)
        mixT = sb(p1, "mixT", [128, 8, 128], BF16)

        op("dve", lambda e: e.memset(R[:], 0.0), writes=[R])
        for i in range(2):
            op("dve", lambda e, i=i: e.memset(xpre[i][:], 0.0), writes=[xpre[i]])
            op("dve", lambda e, i=i: e.memset(vaug[i][:], 1.0), writes=[vaug[i]])
            op("dve", lambda e, i=i: e.memset(kT[i][:], 0.0), writes=[kT[i]])

        B = banks
        bank_rr = [0]

        def nb():
            bk = banks[bank_rr[0] % 8]
            bank_rr[0] += 1
            return bk

        def tview(bk):
            return bfv(bk).rearrange("p (a b) -> p a b", b=128)

        def rstd_from_ss(ssap, n, width, tile, reads):
            op("dve", lambda e: e.tensor_scalar(out=ssap, in0=ssap, scalar1=1.0 / n, scalar2=EPS, op0=ALU.mult, op1=ALU.add),
               reads=reads, writes=[tile])
            if USE_POW:
                op("pool", lambda e: e.tensor_tensor(out=ssap, in0=ssap, in1=neghalf[:, 0:width], op=ALU.pow),
                   reads=[tile, neghalf], writes=[tile])
            else:
                op("act", lambda e: e.activation(out=ssap, in_=ssap, func=AF.Sqrt), reads=[tile], writes=[tile])
                op("dve", lambda e: e.reciprocal(out=ssap, in_=ssap), reads=[tile], writes=[tile])

        NPAIR = 23

        def frontend(s):
            par = s % 2
            main = s >= NPRE
            if s < 2 * NPAIR:
                hdst = hT2[(s // 2) % 2]
                hdst_ap = hdst[:, :, (s % 2) * 128:(s % 2 + 1) * 128]
            elif main and MAIN_PAIRS:
                hdst = hT2[((s - NPRE) // 2) % 2]
                hdst_ap = hdst[:, :, ((s - NPRE) % 2) * 128:((s - NPRE) % 2 + 1) * 128]
            else:
                hdst = hT[par]
                hdst_ap = hdst[:]
            j = s - NPRE
            if main:
                xt = xres_t[:, j, :]; xb = xres_b[j]
            else:
                xt = xpf[par][:]; xb = xpf[par].b
            op("sp", lambda e: e.dma_start(out=xt, in_=xe[s * 128:(s + 1) * 128, :]), writes=[xb], dma=True)
            sst = ss[par]
            hbt = hb[par]
            op("act", lambda e: e.activation(out=hbt[:], in_=xt, func=AF.Square, accum_out=sst[:, 0:1]),
               reads=[xb], writes=[hbt, sst])
            rstd_from_ss(sst[:, 0:1], float(D), 1, sst, [sst])
            if main:
                tfa = tmpf1[:]; tfb = tmpf1.b
            else:
                tfa = xt; tfb = xb
            op("dve", lambda e: e.scalar_tensor_tensor(out=tfa, in0=xt, scalar=sst[:, 0:1], in1=G1, op0=ALU.mult, op1=ALU.mult),
               reads=[xb, sst, mod1], writes=[tfb])
            op("pool", lambda e: e.tensor_tensor(out=hbt[:], in0=tfa, in1=SHIFT1, op=ALU.add), reads=[tfb, mod1], writes=[hbt])
            TH = nb()
            tv = tview(TH)
            for kt in range(8):
                op("pe", lambda e, kt=kt: e.transpose(out=tv[:, kt, :], in_=hbt[:, kt::8], identity=IDENT),
                   reads=[hbt, cbf], writes=[TH])
            op("act", lambda e: e.activation(out=hdst_ap, in_=tv, func=AF.Copy), reads=[TH], writes=[hdst])

        def proj_tok(bank, ncols, col0, h):
            for kt in range(8):
                op("pe", lambda e, kt=kt: e.matmul(bank[:, 0:ncols], lhsT=h[:, kt, :], rhs=win[:, kt, col0:col0 + ncols],
                                                   start=(kt == 0), stop=(kt == 7)), reads=[h, win], writes=[bank])

        def backend2(s, pre_done=False):
            par = s % 2
            main = s >= NPRE
            halo = s == HALO
            j = s - NPRE
            h = hT[par]
            if pre_done:
                h = View(hT2[(j // 2) % 2][:, :, (j % 2) * 128:(j % 2 + 1) * 128], hT2[(j // 2) % 2].b)
            fl = flags[:, s:s + 1]
            nct = 8 if (main or halo) else 6
            xc = xc_[par]; dtw = dtw_[par]; xdt = xdt_[par]; xdte = xdte_[par]; xstok = xstok_[par]; btok = btok_[par]
            sz = sz_[par]; qr = qr_[par]
            V = dtw[:, 0, :]; U = dtw[:, 1, :]; W = dtw[:, 2, :]; Y = dtw[:, 3, :]; T1 = dtw[:, 4, :]
            DT = dtw[:, 5, :]; AA = dtw[:, 6, :]; ACS = dtw[:, 7, :]; EA = dtw[:, 8, :]; DTE = dtw[:, 9, :]
            CD = dtw[:, 10, :]; DTF = dtw[:, 11, :]
            qrf = qr[:].rearrange("p a b -> p (a b)")

            PK = nb()
            if main or halo:
                proj_tok(PK, 256, 512, h)
            for kt in range(0 if pre_done else 8):
                op("pe", lambda e, kt=kt: e.matmul(PK[:, 256:264], lhsT=h[:, kt, :], rhs=win[:, kt, 2304:2312],
                                                   start=(kt == 0), stop=(kt == 7)), reads=[h, win], writes=[PK])
            XB = [None, None] if pre_done else [nb(), nb()]
            for ct in range(0 if pre_done else nct):
                bk = XB[ct // 4]
                for kt in range(8):
                    op("pe", lambda e, kt=kt, ct=ct, bk=bk: e.matmul(
                        bk[:, (ct % 4) * 128:(ct % 4 + 1) * 128], lhsT=win[:, kt, 1280 + ct * 128:1280 + (ct + 1) * 128],
                        rhs=h[:, kt, :], start=(kt == 0), stop=(kt == 7)), reads=[h, win], writes=[bk])
            if main:
                PQ = nb(); PZ = nb()
                proj_tok(PQ, 512, 0, h)
                proj_tok(PZ, 512, 768, h)

            if not pre_done:
                xp = xpre[par]; xpn = xpre[1 - par]
                for hb2 in range(2):
                    n4 = min(4, nct - hb2 * 4)
                    op("act", lambda e, hb2=hb2, n4=n4: e.activation(
                        out=xp[:, hb2 * 4:hb2 * 4 + n4, 3:131],
                        in_=XB[hb2][:, 0:n4 * 128].rearrange("p (a b) -> p a b", b=128), func=AF.Copy, scale=fl),
                       reads=[XB[hb2], flags], writes=[xp])
                op("pool", lambda e: e.tensor_copy(out=xpn[:, :, 0:3], in_=xp[:, :, 128:131]), reads=[xp], writes=[xpn])

                op("dve", lambda e: e.tensor_tensor(out=V, in0=PK[:, 256:264], in1=DTB, op=ALU.add), reads=[PK, small], writes=[dtw])
                op("act", lambda e: e.activation(out=U, in_=V, func=AF.Abs), reads=[dtw], writes=[dtw])
                op("act", lambda e: e.activation(out=U, in_=U, func=AF.Exp, scale=-1.0), reads=[dtw], writes=[dtw])
                op("dve", lambda e: e.tensor_scalar(out=W, in0=U, scalar1=1.0, scalar2=None, op0=ALU.add), reads=[dtw], writes=[dtw])
                op("dve", lambda e: e.tensor_scalar(out=Y, in0=U, scalar1=-0.33, scalar2=0.99, op0=ALU.mult, op1=ALU.add),
                   reads=[dtw], writes=[dtw])
                op("dve", lambda e: e.tensor_tensor(out=Y, in0=Y, in1=U, op=ALU.mult), reads=[dtw], writes=[dtw])
                for _ in range(3):
                    op("act", lambda e: e.activation(out=T1, in_=Y, func=AF.Exp, scale=-1.0), reads=[dtw], writes=[dtw])
                    op("dve", lambda e: e.tensor_tensor(out=T1, in0=T1, in1=W, op=ALU.mult), reads=[dtw], writes=[dtw])
                    op("dve", lambda e: e.scalar_tensor_tensor(out=Y, in0=Y, scalar=-1.0, in1=T1, op0=ALU.add, op1=ALU.add),
                       reads=[dtw], writes=[dtw])
                op("dve", lambda e: e.scalar_tensor_tensor(out=DT, in0=V, scalar=0.0, in1=Y, op0=ALU.max, op1=ALU.add),
                   reads=[dtw], writes=[dtw])
                op("dve", lambda e: e.tensor_tensor(out=AA, in0=DT, in1=ABC, op=ALU.mult), reads=[dtw, small], writes=[dtw])
                op("dve", lambda e: e.tensor_scalar(out=DTF, in0=DT, scalar1=fl, scalar2=None, op0=ALU.mult), reads=[dtw, flags], writes=[dtw])

            if main or halo:
                vg = vaug[par]
                op("act", lambda e: e.activation(out=vg[:, :, 0:64], in_=PK[:, 128:256].rearrange("p (a b) -> p a b", b=64),
                                                 func=AF.Copy), reads=[PK], writes=[vg])
                op("act", lambda e: e.activation(out=sq[:, 8:10, :], in_=PK[:, 0:128].rearrange("p (a b) -> p a b", b=64),
                                                 func=AF.Square), reads=[PK], writes=[sq])
                h0 = 0 if main else 8
                if main:
                    op("act", lambda e: e.activation(out=sq[:, 0:8, :], in_=PQ[:, :].rearrange("p (a b) -> p a b", b=64),
                                                     func=AF.Square), reads=[PQ], writes=[sq])
                op("dve", lambda e: e.tensor_reduce(out=ssq[:, h0:10], in_=sq[:, h0:10, :], axis=AX.X, op=ALU.add),
                   reads=[sq], writes=[ssq])
                rstd_from_ss(ssq[:, h0:10], 64.0, 10 - h0, ssq, [ssq])
                if main:
                    op("dve", lambda e: e.tensor_tensor(
                        out=qn[:, 0:8, :].rearrange("p (i g) d -> p g i d", g=2),
                        in0=PQ[:, :].rearrange("p (g i d) -> p g i d", g=2, i=4),
                        in1=ssq[:, 0:8].rearrange("p (g i) -> p g i", g=2)[:, :, :, None].to_broadcast([128, 2, 4, 64]),
                        op=ALU.mult), reads=[PQ, ssq], writes=[qn])
                op("dve", lambda e: e.tensor_tensor(out=qn[:, 8:10, :], in0=PK[:, 0:128].rearrange("p (a b) -> p a b", b=64),
                                                    in1=ssq[:, 8:10, None].to_broadcast([128, 2, 64]), op=ALU.mult),
                   reads=[PK, ssq], writes=[qn])
                if main:
                    op("pool", lambda e: e.tensor_tensor(out=qn[:, 0:8, :], in0=qn[:, 0:8, :],
                                                         in1=qkw[:, 0, None, :].to_broadcast([128, 8, 64]), op=ALU.mult),
                       reads=[qkw], writes=[qn])
                op("pool", lambda e: e.tensor_tensor(out=qn[:, 8:10, :], in0=qn[:, 8:10, :],
                                                     in1=qkw[:, 1, None, :].to_broadcast([128, 2, 64]), op=ALU.mult),
                   reads=[qkw], writes=[qn])
                jj = s - HALO
                nhh = 10 - h0
                cosb = cossin[:, 0, jj, None, :].to_broadcast([128, nhh, 8])
                sinb = cossin[:, 1, jj, None, :].to_broadcast([128, nhh, 8])
                x1 = qn[:, h0:10, 0:8]; x2 = qn[:, h0:10, 8:16]
                op("pool", lambda e: e.tensor_tensor(out=rt[:, 0, h0:10, :], in0=x1, in1=cosb, op=ALU.mult), reads=[qn, cossin], writes=[rt])
                op("pool", lambda e: e.tensor_tensor(out=rt[:, 1, h0:10, :], in0=x2, in1=sinb, op=ALU.mult), reads=[qn, cossin], writes=[rt])
                op("pool", lambda e: e.tensor_tensor(out=rt[:, 2, h0:10, :], in0=x2, in1=cosb, op=ALU.mult), reads=[qn, cossin], writes=[rt])
                op("pool", lambda e: e.tensor_tensor(out=rt[:, 3, h0:10, :], in0=x1, in1=sinb, op=ALU.mult), reads=[qn, cossin], writes=[rt])
                op("pool", lambda e: e.tensor_tensor(out=qr[:, h0:10, 0:8], in0=rt[:, 0, h0:10, :], in1=rt[:, 1, h0:10, :], op=ALU.subtract),
                   reads=[rt], writes=[qr])
                op("pool", lambda e: e.tensor_tensor(out=qr[:, h0:10, 8:16], in0=rt[:, 2, h0:10, :], in1=rt[:, 3, h0:10, :], op=ALU.add),
                   reads=[rt], writes=[qr])
                op("pool", lambda e: e.tensor_copy(out=qr[:, h0:10, 16:64], in_=qn[:, h0:10, 16:64]), reads=[qn], writes=[qr])
            if main:
                op("act", lambda e: e.activation(out=tz[:], in_=PZ[:, :], func=AF.Tanh), reads=[PZ], writes=[tz])
                op("dve", lambda e: e.scalar_tensor_tensor(out=sz[:], in0=tz[:], scalar=1.0, in1=PZ[:, :], op0=ALU.add, op1=ALU.mult),
                   reads=[tz, PZ], writes=[sz])

            if not pre_done:
                CB = [nb(), nb()]
                for ct in range(nct):
                    bk = CB[ct // 4]
                    o = bk[:, (ct % 4) * 128:(ct % 4 + 1) * 128]
                    for k in range(4):
                        op("pe", lambda e, k=k, ct=ct, o=o: e.matmul(o, lhsT=convdiag[:, k * 8 + ct, :], rhs=xp[:, ct, k:k + 128],
                                                                    start=(k == 0), stop=False), reads=[convdiag, xp], writes=[bk])
                    op("pe", lambda e, ct=ct, o=o: e.matmul(o, lhsT=cbrow[0:1, ct * 128:(ct + 1) * 128], rhs=onesrow[0:1, 0:128],
                                                            start=False, stop=True), reads=[cbrow, onesrow], writes=[bk])
                for hb2 in range(2):
                    n4 = min(4, nct - hb2 * 4)
                    pv = CB[hb2][:, 0:n4 * 128].rearrange("p (a b) -> p a b", b=128)
                    op("act", lambda e, hb2=hb2, n4=n4, pv=pv: e.activation(out=tcv[:, hb2 * 4:hb2 * 4 + n4, :], in_=pv, func=AF.Tanh),
                       reads=[CB[hb2]], writes=[tcv])
                    op("dve", lambda e, hb2=hb2, n4=n4, pv=pv: e.scalar_tensor_tensor(
                        out=xc[:, hb2 * 4:hb2 * 4 + n4, :], in0=tcv[:, hb2 * 4:hb2 * 4 + n4, :], scalar=1.0, in1=pv,
                        op0=ALU.add, op1=ALU.mult), reads=[tcv, CB[hb2]], writes=[xc])

            DB = nb()
            if not pre_done:
                op("pe", lambda e: e.matmul(DB[:, 0:8], lhsT=TRI, rhs=AA, start=True, stop=True), reads=[cf32, dtw], writes=[DB])
                op("pe", lambda e: e.matmul(DB[:, 8:16], lhsT=ONES, rhs=AA, start=True, stop=True), reads=[cf32, dtw], writes=[DB])
            if main:
                for g in range(2):
                    op("pe", lambda e, g=g: e.matmul(DB[:, 128 + g * 128:256 + g * 128], lhsT=xc[:, 4 + g, :], rhs=xc[:, 6 + g, :],
                                                     start=True, stop=True), reads=[xc], writes=[DB])
            if not pre_done:
                op("act", lambda e: e.activation(out=ACS, in_=DB[:, 0:8], func=AF.Copy), reads=[DB], writes=[dtw])
                op("act", lambda e: e.activation(out=CD, in_=DB[:, 8:16], func=AF.Exp), reads=[DB], writes=[dtw])
                op("dve", lambda e: e.tensor_tensor(out=DTE, in0=DB[:, 8:16], in1=ACS, op=ALU.subtract), reads=[DB, dtw], writes=[dtw])
                op("act", lambda e: e.activation(out=DTE, in_=DTE, func=AF.Exp), reads=[dtw], writes=[dtw])
            if main:
                if not pre_done:
                    op("act", lambda e: e.activation(out=EA, in_=ACS, func=AF.Exp), reads=[dtw], writes=[dtw])
                op("dve", lambda e: e.tensor_tensor(out=gtm[:], in0=DB[:, 128:384].rearrange("p (a b) -> p a b", b=128),
                                                    in1=MASKC[:, None, :].to_broadcast([128, 2, 128]), op=ALU.mult),
                   reads=[DB, cbf], writes=[gtm])
            if not pre_done:
                op("dve", lambda e: e.tensor_tensor(out=DTE, in0=DTE, in1=DTF, op=ALU.mult), reads=[dtw], writes=[dtw])

            TB = nb()
            tv = tview(TB)
            for i in range(6):
                op("pe", lambda e, i=i: e.transpose(out=tv[:, i, :], in_=xc[:, i, :], identity=IDENT), reads=[xc, cbf], writes=[TB])
            xps = bfv(TB)[:, 0:512].rearrange("p (a b) -> p a b", b=64)
            op("dve", lambda e: e.tensor_tensor(out=xdte[:], in0=xps, in1=DTE[:, :, None].to_broadcast([128, 8, 64]), op=ALU.mult),
               reads=[TB, dtw], writes=[xdte])
            if main:
                op("dve", lambda e: e.tensor_tensor(out=xdt[:], in0=xps, in1=DTF[:, :, None].to_broadcast([128, 8, 64]), op=ALU.mult),
                   reads=[TB, dtw], writes=[xdt])
                op("act", lambda e: e.activation(out=xstok[:], in_=xps, func=AF.Copy), reads=[TB], writes=[xstok])
            op("act", lambda e: e.activation(out=btok[:], in_=tv[:, 4:6, :], func=AF.Copy), reads=[TB], writes=[btok])
            if main:
                op("act", lambda e: e.activation(out=Rb[:], in_=R[:], func=AF.Copy), reads=[R], writes=[Rb])

            SBK = nb()
            for g in range(2):
                op("pe", lambda e, g=g: e.matmul(SBK[:, g * 256:(g + 1) * 256], lhsT=btok[:, g, :],
                                                 rhs=xdte[:, g * 4:(g + 1) * 4, :], start=True, stop=True),
                   reads=[btok, xdte], writes=[SBK])
            op("dve", lambda e: e.tensor_tensor(out=R[:], in0=R[:], in1=CD[:, :, None].to_broadcast([128, 8, 64]), op=ALU.mult),
               reads=[dtw, Rb], writes=[R])
            op("dve", lambda e: e.tensor_tensor(out=R[:], in0=R[:], in1=SBK[:, :].rearrange("p (a b) -> p a b", b=64), op=ALU.add),
               reads=[SBK], writes=[R])

            if main or halo:
                TQ = nb()
                tq = tview(TQ)
                if main:
                    for i in range(4):
                        op("pe", lambda e, i=i: e.transpose(out=tq[:, i, :], in_=qrf[:, i * 128:(i + 1) * 128], identity=IDENT),
                           reads=[qr, cbf], writes=[TQ])
                op("pe", lambda e: e.transpose(out=tq[:, 4, :], in_=qrf[:, 512:640], identity=IDENT), reads=[qr, cbf], writes=[TQ])
                if main:
                    op("act", lambda e: e.activation(out=qT[:], in_=tq[:, 0:4, :], func=AF.Copy), reads=[TQ], writes=[qT])
                op("act", lambda e: e.activation(out=kT[par][:], in_=tq[:, 4, :], func=AF.Copy), reads=[TQ], writes=[kT[par]])
            if not main:
                return

            kprev = kT[1 - par]; vprev = vaug[1 - par]
            kcur = kT[par]; vcur = vaug[par]
            sbanks = [nb(), nb(), nb(), nb()]
            for g in range(2):
                for bi, kt_ in enumerate((kprev, kcur)):
                    bk = sbanks[g * 2 + bi]
                    op("pe", lambda e, g=g, kt_=kt_, bk=bk: e.matmul(
                        bk[:, :], lhsT=kt_[g * 64:(g + 1) * 64, :], rhs=qT[g * 64:(g + 1) * 64, :, :], start=True, stop=False),
                       reads=[kt_, qT], writes=[bk])
                    mi = (2 if j == 0 else 0) if bi == 0 else 1
                    op("pe", lambda e, bk=bk, mi=mi: e.matmul(bk[:, :], lhsT=IDENT, rhs=negm[:, mi, :, :], start=False, stop=True),
                       reads=[cbf, negm], writes=[bk])

            SEG = [nb(), nb()]
            for hf in range(2):
                op("pool", lambda e, hf=hf: e.tensor_tensor(out=Lm[:], in0=SLM[:, None, :].to_broadcast([128, 4, 128]),
                                                            in1=AA[:, hf * 4:(hf + 1) * 4, None].to_broadcast([128, 4, 128]), op=ALU.mult),
                   reads=[cf32, dtw], writes=[Lm])
                bk = SEG[hf]
                for h4 in range(4):
                    op("pe", lambda e, h4=h4, bk=bk: e.matmul(bk[:, h4 * 128:(h4 + 1) * 128], lhsT=Lm[:, h4, :], rhs=TRI,
                                                              start=True, stop=True), reads=[Lm, cf32], writes=[bk])
            for g in range(2):
                for bi in range(2):
                    bk = sbanks[g * 2 + bi]
                    idx = g * 2 + bi
                    op("act", lambda e, bk=bk, idx=idx: e.activation(out=pT[:, idx, :], in_=bk[:, :], func=AF.Exp, scale=0.125,
                                                                     bias=negc[:]), reads=[bk, negc], writes=[pT])
            obanks = [nb(), nb()]
            for g in range(2):
                ob = obanks[g]
                for i in range(4):
                    for bi, vt in enumerate((vprev, vcur)):
                        idx = g * 2 + bi
                        op("pe", lambda e, g=g, i=i, bi=bi, vt=vt, idx=idx, ob=ob: e.matmul(
                            ob[:, i * 65:(i + 1) * 65], lhsT=pT[:, idx, i * 128:(i + 1) * 128], rhs=vt[:, g, :],
                            start=(bi == 0), stop=(bi == 1)), reads=[pT, vt], writes=[ob])
                ov = ob[:, 0:260].rearrange("p (a b) -> p a b", b=65)
                op("dve", lambda e, g=g, ov=ov: e.tensor_tensor(out=den[:, g * 4:(g + 1) * 4], in0=ov[:, :, 64],
                                                               in1=ESINK[:, g * 4:(g + 1) * 4], op=ALU.add),
                   reads=[ob, small], writes=[den])
                op("dve", lambda e, g=g: e.reciprocal(out=den[:, g * 4:(g + 1) * 4], in_=den[:, g * 4:(g + 1) * 4]),
                   reads=[den], writes=[den])
                op("dve", lambda e, g=g, ov=ov: e.tensor_tensor(
                    out=mix[:, g * 256:(g + 1) * 256].rearrange("p (a b) -> p a b", b=64), in0=ov[:, :, 0:64],
                    in1=den[:, g * 4:(g + 1) * 4, None].to_broadcast([128, 4, 64]), op=ALU.mult), reads=[ob, den], writes=[mix])
            for hb2 in range(2):
                op("act", lambda e, hb2=hb2: e.activation(out=decT[:, hb2 * 4:(hb2 + 1) * 4, :],
                                                          in_=SEG[hb2][:, :].rearrange("p (a b) -> p a b", b=128), func=AF.Exp),
                   reads=[SEG[hb2]], writes=[decT])
            for g in range(2):
                op("dve", lambda e, g=g: e.tensor_tensor(out=decT[:, g * 4:(g + 1) * 4, :], in0=decT[:, g * 4:(g + 1) * 4, :],
                                                          in1=gtm[:, g, None, :].to_broadcast([128, 4, 128]), op=ALU.mult),
                   reads=[gtm], writes=[decT])
            YB = nb(); YOFF = nb()
            for hh in range(8):
                o = YB[:, hh * 64:(hh + 1) * 64]
                op("pe", lambda e, hh=hh, o=o: e.matmul(o, lhsT=decT[:, hh, :], rhs=xdt[:, hh, :], start=True, stop=False),
                   reads=[decT, xdt], writes=[YB])
                op("pe", lambda e, hh=hh, o=o: e.matmul(o, lhsT=dI[:, hh, :], rhs=xstok[:, hh, :], start=False, stop=True),
                   reads=[dI, xstok], writes=[YB])
            for g in range(2):
                op("pe", lambda e, g=g: e.matmul(YOFF[:, g * 256:(g + 1) * 256], lhsT=xc[:, 6 + g, :], rhs=Rb[:, g * 4:(g + 1) * 4, :],
                                                 start=True, stop=True), reads=[xc, Rb], writes=[YOFF])
            op("dve", lambda e: e.tensor_tensor(out=yo[:].rearrange("p (a b) -> p a b", b=64),
                                                in0=YOFF[:, :].rearrange("p (a b) -> p a b", b=64),
                                                in1=EA[:, :, None].to_broadcast([128, 8, 64]), op=ALU.mult),
               reads=[YOFF, dtw], writes=[yo])
            yy = yo
            op("dve", lambda e: e.tensor_tensor(out=yy[:], in0=YB[:, :], in1=yo[:], op=ALU.add), reads=[YB], writes=[yy])
            op("dve", lambda e: e.tensor_tensor(out=yy[:], in0=yy[:], in1=sz[:], op=ALU.mult), reads=[sz], writes=[yy])
            for g in range(2):
                op("act", lambda e, g=g: e.activation(out=junk[:, g * 256:(g + 1) * 256], in_=yy[:, g * 256:(g + 1) * 256],
                                                      func=AF.Square, accum_out=ssg[:, g:g + 1]), reads=[yy], writes=[junk, ssg])
            rstd_from_ss(ssg[:, 0:2], 256.0, 2, ssg, [ssg])
            for g in range(2):
                op("dve", lambda e, g=g: e.scalar_tensor_tensor(out=mix[:, 512 + g * 256:768 + g * 256], in0=yy[:, g * 256:(g + 1) * 256],
                                                                scalar=ssg[:, g:g + 1], in1=snw[:, g * 256:(g + 1) * 256],
                                                                op0=ALU.mult, op1=ALU.mult), reads=[yy, ssg, snw], writes=[mix])

            TM = nb()
            tm = tview(TM)
            for kt in range(8):
                op("pe", lambda e, kt=kt: e.transpose(out=tm[:, kt, :], in_=mix[:, kt::8], identity=IDENT), reads=[mix, cbf], writes=[TM])
            op("act", lambda e: e.activation(out=mixT[:], in_=tm, func=AF.Copy), reads=[TM], writes=[mixT])
            for half in range(2):
                bk = nb()
                for kt in range(8):
                    op("pe", lambda e, kt=kt, half=half, bk=bk: e.matmul(bk[:, :], lhsT=mixT[:, kt, :],
                                                                        rhs=wout[:, kt, half * 512:(half + 1) * 512],
                                                                        start=(kt == 0), stop=(kt == 7)), reads=[mixT, wout], writes=[bk])
                op("dve", lambda e, half=half, bk=bk: e.tensor_tensor(out=xres_t[:, j, half * 512:(half + 1) * 512],
                                                                      in0=bk[:, :], in1=xres_t[:, j, half * 512:(half + 1) * 512],
                                                                      op=ALU.add), reads=[bk], writes=[xres_b[j]])
            if DEBUG_X2:
                op("sp", lambda e: e.dma_start(out=dbg_d[j * 128:(j + 1) * 128, :], in_=xres_t[:, j, :]), reads=[xres_b[j]], dma=True)

        def prefix_pair(p):
            s0 = 2 * p
            pp = p % 2
            h2 = hT2[pp]
            fl = flags[:, s0:s0 + 1]
            xp = xpre2[pp]
            last = (p == NPAIR - 1)
            xpn = xpre2[0] if last else xpre2[1 - pp]
            XCB = [xc_[0].b, xc_[1].b]; DTB_ = [dtw_[0].b, dtw_[1].b]
            XDB = [xdte_[0].b, xdte_[1].b]; BTB = [btok_[0].b, btok_[1].b]
            D2 = lambda r: dtw2[:, r, :]
            V, U, W, Y, T1, DT, AA, ACS, DTE, CD, DTF = (D2(0), D2(1), D2(2), D2(3), D2(4), D2(5), D2(6), D2(7), D2(9), D2(10), D2(11))
            PK = nb()
            for hf in range(2):
                for kt in range(8):
                    op("pe", lambda e, kt=kt, hf=hf: e.matmul(PK[:, hf * 8:(hf + 1) * 8], lhsT=h2[:, kt, hf * 128:(hf + 1) * 128],
                                                              rhs=win[:, kt, 2304:2312], start=(kt == 0), stop=(kt == 7)),
                       reads=[h2, win], writes=[PK])
            XB = [nb(), nb(), nb()]
            for ct in range(6):
                bk = XB[ct // 2]
                for kt in range(8):
                    op("pe", lambda e, kt=kt, ct=ct, bk=bk: e.matmul(
                        bk[:, (ct % 2) * 256:(ct % 2 + 1) * 256], lhsT=win[:, kt, 1280 + ct * 128:1280 + (ct + 1) * 128],
                        rhs=h2[:, kt, :], start=(kt == 0), stop=(kt == 7)), reads=[h2, win], writes=[bk])
            for b3 in range(3):
                op("act", lambda e, b3=b3: e.activation(out=xp[:, 2 * b3:2 * b3 + 2, 3:259],
                                                        in_=XB[b3][:, :].rearrange("p (a b) -> p a b", b=256), func=AF.Copy, scale=fl),
                   reads=[XB[b3], flags], writes=[xp])
            op("pool", lambda e: e.tensor_copy(out=xpn[:, 0:6, 0:3], in_=xp[:, 0:6, 256:259]), reads=[xp], writes=[xpn])
            op("dve", lambda e: e.tensor_tensor(out=V.rearrange("p (a b) -> p a b", b=8), in0=PK[:, 0:16].rearrange("p (a b) -> p a b", b=8),
                                                in1=DTB[:, None, :].to_broadcast([128, 2, 8]), op=ALU.add), reads=[PK, small], writes=DTB_)
            op("act", lambda e: e.activation(out=U, in_=V, func=AF.Abs), reads=DTB_, writes=DTB_)
            op("act", lambda e: e.activation(out=U, in_=U, func=AF.Exp, scale=-1.0), reads=DTB_, writes=DTB_)
            op("dve", lambda e: e.tensor_scalar(out=W, in0=U, scalar1=1.0, scalar2=None, op0=ALU.add), reads=DTB_, writes=DTB_)
            op("dve", lambda e: e.tensor_scalar(out=Y, in0=U, scalar1=-0.33, scalar2=0.99, op0=ALU.mult, op1=ALU.add), reads=DTB_, writes=DTB_)
            op("dve", lambda e: e.tensor_tensor(out=Y, in0=Y, in1=U, op=ALU.mult), reads=DTB_, writes=DTB_)
            for _ in range(3):
                op("act", lambda e: e.activation(out=T1, in_=Y, func=AF.Exp, scale=-1.0), reads=DTB_, writes=DTB_)
                op("dve", lambda e: e.tensor_tensor(out=T1, in0=T1, in1=W, op=ALU.mult), reads=DTB_, writes=DTB_)
                op("dve", lambda e: e.scalar_tensor_tensor(out=Y, in0=Y, scalar=-1.0, in1=T1, op0=ALU.add, op1=ALU.add), reads=DTB_, writes=DTB_)
            op("dve", lambda e: e.scalar_tensor_tensor(out=DT, in0=V, scalar=0.0, in1=Y, op0=ALU.max, op1=ALU.add), reads=DTB_, writes=DTB_)
            op("dve", lambda e: e.tensor_tensor(out=AA.rearrange("p (a b) -> p a b", b=8), in0=DT.rearrange("p (a b) -> p a b", b=8),
                                                in1=ABC[:, None, :].to_broadcast([128, 2, 8]), op=ALU.mult), reads=DTB_ + [small.b], writes=DTB_)
            op("dve", lambda e: e.tensor_scalar(out=DTF, in0=DT, scalar1=fl, scalar2=None, op0=ALU.mult), reads=DTB_ + [flags.b], writes=DTB_)
            CB = [nb(), nb(), nb()]
            for ct in range(6):
                bk = CB[ct // 2]
                o = bk[:, (ct % 2) * 256:(ct % 2 + 1) * 256]
                for k in range(4):
                    op("pe", lambda e, k=k, ct=ct, o=o: e.matmul(o, lhsT=convdiag[:, k * 8 + ct, :], rhs=xp[:, ct, k:k + 256],
                                                                start=(k == 0), stop=False), reads=[convdiag, xp], writes=[bk])
                op("pe", lambda e, ct=ct, o=o: e.matmul(o, lhsT=cbrow[0:1, ct * 128:(ct + 1) * 128], rhs=onesrow[0:1, :],
                                                        start=False, stop=True), reads=[cbrow, onesrow], writes=[bk])
            for b3 in range(3):
                pv = CB[b3][:, :].rearrange("p (a b) -> p a b", b=256)
                op("act", lambda e, b3=b3, pv=pv: e.activation(out=tcv2[:, 2 * b3:2 * b3 + 2, :], in_=pv, func=AF.Tanh),
                   reads=[CB[b3]], writes=[tcv2])
                op("dve", lambda e, b3=b3, pv=pv: e.scalar_tensor_tensor(out=xc2[:, 2 * b3:2 * b3 + 2, :], in0=tcv2[:, 2 * b3:2 * b3 + 2, :],
                                                                         scalar=1.0, in1=pv, op0=ALU.add, op1=ALU.mult),
                   reads=[tcv2, CB[b3]], writes=XCB)
            DB = nb()
            op("pe", lambda e: e.matmul(DB[:, 0:16], lhsT=TRI, rhs=AA, start=True, stop=True), reads=[cf32] + DTB_, writes=[DB])
            op("pe", lambda e: e.matmul(DB[:, 16:32], lhsT=ONES, rhs=AA, start=True, stop=True), reads=[cf32] + DTB_, writes=[DB])
            op("act", lambda e: e.activation(out=ACS, in_=DB[:, 0:16], func=AF.Copy), reads=[DB], writes=DTB_)
            op("act", lambda e: e.activation(out=CD, in_=DB[:, 16:32], func=AF.Exp), reads=[DB], writes=DTB_)
            op("dve", lambda e: e.tensor_tensor(out=DTE, in0=DB[:, 16:32], in1=ACS, op=ALU.subtract), reads=[DB] + DTB_, writes=DTB_)
            op("act", lambda e: e.activation(out=DTE, in_=DTE, func=AF.Exp), reads=DTB_, writes=DTB_)
            op("dve", lambda e: e.tensor_tensor(out=DTE, in0=DTE, in1=DTF, op=ALU.mult), reads=DTB_, writes=DTB_)
            op("dve", lambda e: e.tensor_tensor(out=DTE[:, 0:8], in0=DTE[:, 0:8], in1=CD[:, 8:16], op=ALU.mult), reads=DTB_, writes=DTB_)
            op("dve", lambda e: e.tensor_tensor(out=CD[:, 0:8], in0=CD[:, 0:8], in1=CD[:, 8:16], op=ALU.mult), reads=DTB_, writes=DTB_)
            for hf in range(2):
                TB = nb()
                tv = tview(TB)
                for i in range(6):
                    op("pe", lambda e, i=i, hf=hf, tv=tv: e.transpose(out=tv[:, i, :], in_=xc2[:, i, hf * 128:(hf + 1) * 128], identity=IDENT),
                       reads=XCB + [cbf.b], writes=[TB])
                xps = bfv(TB)[:, 0:512].rearrange("p (a b) -> p a b", b=64)
                op("dve", lambda e, hf=hf, xps=xps: e.tensor_tensor(out=xdte2[:, hf, :, :], in0=xps,
                                                                   in1=DTE[:, hf * 8:(hf + 1) * 8, None].to_broadcast([128, 8, 64]), op=ALU.mult),
                   reads=[TB] + DTB_, writes=XDB)
                op("act", lambda e, hf=hf, tv=tv: e.activation(out=btok2[:, hf, :, :], in_=tv[:, 4:6, :], func=AF.Copy), reads=[TB], writes=BTB)
            SBK = nb()
            for g in range(2):
                for hf in range(2):
                    op("pe", lambda e, g=g, hf=hf: e.matmul(SBK[:, g * 256:(g + 1) * 256], lhsT=btok2[:, hf, g, :],
                                                            rhs=xdte2[:, hf, g * 4:(g + 1) * 4, :], start=(hf == 0), stop=(hf == 1)),
                       reads=BTB + XDB, writes=[SBK])
            op("dve", lambda e: e.tensor_tensor(out=R[:], in0=R[:], in1=CD[:, 0:8, None].to_broadcast([128, 8, 64]), op=ALU.mult),
               reads=DTB_, writes=[R])
            op("dve", lambda e: e.tensor_tensor(out=R[:], in0=R[:], in1=SBK[:, :].rearrange("p (a b) -> p a b", b=64), op=ALU.add),
               reads=[SBK], writes=[R])

        def main_pair_pre(pm):
            s0 = NPRE + 2 * pm
            pp = pm % 2
            h2 = hT2[pp]
            fl = flags[:, s0:s0 + 1]
            xp = xpre2[pp]
            xpn = xpre2[1 - pp]
            XCB = [xc_[0].b, xc_[1].b]; DTB_ = [dtw_[0].b, dtw_[1].b]
            tcvb = [Buf("tcvs%d" % i) for i in range(6)]
            for tb_ in tcvb:
                tb_.w = list(tcv2.b.w); tb_.r = list(tcv2.b.r)
            xcsl = [Buf("xcsl%d" % i) for i in range(8)]
            for xb_ in xcsl:
                xb_.w = list(xc_[0].b.w) + list(xc_[1].b.w); xb_.r = list(xc_[0].b.r) + list(xc_[1].b.r)
            D2 = lambda r: dtw2[:, r, :]
            V, U, W, Y, T1, DT, AA, ACS, EA, DTE, CD, DTF = (D2(0), D2(1), D2(2), D2(3), D2(4), D2(5), D2(6), D2(7), D2(8), D2(9), D2(10), D2(11))
            PK = nb()
            for hf in range(2):
                for kt in range(8):
                    op("pe", lambda e, kt=kt, hf=hf: e.matmul(PK[:, hf * 8:(hf + 1) * 8], lhsT=h2[:, kt, hf * 128:(hf + 1) * 128],
                                                              rhs=win[:, kt, 2304:2312], start=(kt == 0), stop=(kt == 7)),
                       reads=[h2, win], writes=[PK])
            XB = [nb(), nb(), nb(), nb()]
            for ct in range(8):
                bk = XB[ct // 2]
                for kt in range(8):
                    op("pe", lambda e, kt=kt, ct=ct, bk=bk: e.matmul(
                        bk[:, (ct % 2) * 256:(ct % 2 + 1) * 256], lhsT=win[:, kt, 1280 + ct * 128:1280 + (ct + 1) * 128],
                        rhs=h2[:, kt, :], start=(kt == 0), stop=(kt == 7)), reads=[h2, win], writes=[bk])
            for b3 in range(4):
                op("act", lambda e, b3=b3: e.activation(out=xp[:, 2 * b3:2 * b3 + 2, 3:259],
                                                        in_=XB[b3][:, :].rearrange("p (a b) -> p a b", b=256), func=AF.Copy, scale=fl),
                   reads=[XB[b3], flags], writes=[xp])
            op("pool", lambda e: e.tensor_copy(out=xpn[:, :, 0:3], in_=xp[:, :, 256:259]), reads=[xp], writes=[xpn])
            op("dve", lambda e: e.tensor_tensor(out=V.rearrange("p (a b) -> p a b", b=8), in0=PK[:, 0:16].rearrange("p (a b) -> p a b", b=8),
                                                in1=DTB[:, None, :].to_broadcast([128, 2, 8]), op=ALU.add), reads=[PK, small], writes=DTB_)
            op("act", lambda e: e.activation(out=U, in_=V, func=AF.Abs), reads=DTB_, writes=DTB_)
            op("act", lambda e: e.activation(out=U, in_=U, func=AF.Exp, scale=-1.0), reads=DTB_, writes=DTB_)
            op("dve", lambda e: e.tensor_scalar(out=W, in0=U, scalar1=1.0, scalar2=None, op0=ALU.add), reads=DTB_, writes=DTB_)
            op("dve", lambda e: e.tensor_scalar(out=Y, in0=U, scalar1=-0.33, scalar2=0.99, op0=ALU.mult, op1=ALU.add), reads=DTB_, writes=DTB_)
            op("dve", lambda e: e.tensor_tensor(out=Y, in0=Y, in1=U, op=ALU.mult), reads=DTB_, writes=DTB_)
            for _ in range(3):
                op("act", lambda e: e.activation(out=T1, in_=Y, func=AF.Exp, scale=-1.0), reads=DTB_, writes=DTB_)
                op("dve", lambda e: e.tensor_tensor(out=T1, in0=T1, in1=W, op=ALU.mult), reads=DTB_, writes=DTB_)
                op("dve", lambda e: e.scalar_tensor_tensor(out=Y, in0=Y, scalar=-1.0, in1=T1, op0=ALU.add, op1=ALU.add), reads=DTB_, writes=DTB_)
            op("dve", lambda e: e.scalar_tensor_tensor(out=DT, in0=V, scalar=0.0, in1=Y, op0=ALU.max, op1=ALU.add), reads=DTB_, writes=DTB_)
            op("dve", lambda e: e.tensor_tensor(out=AA.rearrange("p (a b) -> p a b", b=8), in0=DT.rearrange("p (a b) -> p a b", b=8),
                                                in1=ABC[:, None, :].to_broadcast([128, 2, 8]), op=ALU.mult), reads=DTB_ + [small.b], writes=DTB_)
            op("dve", lambda e: e.tensor_scalar(out=DTF, in0=DT, scalar1=fl, scalar2=None, op0=ALU.mult), reads=DTB_ + [flags.b], writes=DTB_)
            CB = [nb(), nb(), nb(), nb()]
            for ct in range(8):
                bk = CB[ct // 2]
                o = bk[:, (ct % 2) * 256:(ct % 2 + 1) * 256]
                for k in range(4):
                    op("pe", lambda e, k=k, ct=ct, o=o: e.matmul(o, lhsT=convdiag[:, k * 8 + ct, :], rhs=xp[:, ct, k:k + 256],
                                                                start=(k == 0), stop=False), reads=[convdiag, xp], writes=[bk])
                op("pe", lambda e, ct=ct, o=o: e.matmul(o, lhsT=cbrow[0:1, ct * 128:(ct + 1) * 128], rhs=onesrow[0:1, :],
                                                        start=False, stop=True), reads=[cbrow, onesrow], writes=[bk])
            for b3 in range(4):
                pv = CB[b3][:, :].rearrange("p (a b) -> p a b", b=256)
                for q2 in range(2):
                    pvh = pv[:, q2:q2 + 1, :]
                    sl = (2 * b3 + q2) % 6
                    op("act", lambda e, pvh=pvh, sl=sl: e.activation(out=tcv2[:, sl:sl + 1, :], in_=pvh, func=AF.Tanh),
                       reads=[CB[b3]], writes=[tcvb[sl]])
                    op("dve", lambda e, b3=b3, q2=q2, pvh=pvh, sl=sl: e.scalar_tensor_tensor(
                        out=xc2[:, 2 * b3 + q2:2 * b3 + q2 + 1, :], in0=tcv2[:, sl:sl + 1, :], scalar=1.0, in1=pvh, op0=ALU.add, op1=ALU.mult),
                       reads=[tcvb[sl], CB[b3]], writes=[xcsl[2 * b3 + q2]])
            allw = sorted(set(i for xb_ in xcsl for i in xb_.w))
            for hf_ in range(2):
                xc_[hf_].b.w = list(allw); xc_[hf_].b.r = []
            tcv2.b.w = sorted(set(i for tb_ in tcvb for i in tb_.w))
            tcv2.b.r = sorted(set(i for tb_ in tcvb for i in tb_.r))
            DB = nb()
            op("pe", lambda e: e.matmul(DB[:, 0:16], lhsT=TRI, rhs=AA, start=True, stop=True), reads=[cf32] + DTB_, writes=[DB])
            op("pe", lambda e: e.matmul(DB[:, 16:32], lhsT=ONES, rhs=AA, start=True, stop=True), reads=[cf32] + DTB_, writes=[DB])
            op("act", lambda e: e.activation(out=ACS, in_=DB[:, 0:16], func=AF.Copy), reads=[DB], writes=DTB_)
            op("act", lambda e: e.activation(out=CD, in_=DB[:, 16:32], func=AF.Exp), reads=[DB], writes=DTB_)
            op("dve", lambda e: e.tensor_tensor(out=DTE, in0=DB[:, 16:32], in1=ACS, op=ALU.subtract), reads=[DB] + DTB_, writes=DTB_)
            op("act", lambda e: e.activation(out=DTE, in_=DTE, func=AF.Exp), reads=DTB_, writes=DTB_)
            op("act", lambda e: e.activation(out=EA, in_=ACS, func=AF.Exp), reads=DTB_, writes=DTB_)
            op("dve", lambda e: e.tensor_tensor(out=DTE, in0=DTE, in1=DTF, op=ALU.mult), reads=DTB_, writes=DTB_)

        if STOP_AFTER != 1 and MAIN_PAIRS:
            frontend(0); frontend(1)
            for p in range(NPAIR):
                frontend(2 * p + 2)
                if p + 1 < NPAIR:
                    frontend(2 * p + 3)
                prefix_pair(p)
            frontend(2 * NPAIR + 1)
            backend2(2 * NPAIR)
            frontend(NPRE); frontend(NPRE + 1)
            backend2(2 * NPAIR + 1)
            for pm in range(NMAIN // 2):
                if pm + 1 < NMAIN // 2:
                    frontend(NPRE + 2 * pm + 2); frontend(NPRE + 2 * pm + 3)
                main_pair_pre(pm)
                backend2(NPRE + 2 * pm, pre_done=True)
                backend2(NPRE + 2 * pm + 1, pre_done=True)
        elif STOP_AFTER != 1:
            frontend(0); frontend(1)
            for p in range(NPAIR):
                frontend(2 * p + 2)
                if p + 1 < NPAIR:
                    frontend(2 * p + 3)
                prefix_pair(p)
            for s in range(2 * NPAIR, NSLOT):
                if s + 1 < NSLOT:
                    frontend(s + 1)
                backend2(s)

        S.barrier(lambda e: e.memset(neghalf[:, 16:32], -0.5))
        p1.close()

        p2 = root.enter_context(contextlib.ExitStack())
        mod2 = sb(p2, "mod2", [128, 3 * D])
        SHIFT2 = mod2[:, 0:1024]; G2 = mod2[:, 1024:2048]; GATE2 = mod2[:, 2048:3072]
        op("sp", lambda e: e.dma_start(out=mod2[:], in_=modscr), reads=[modscr_b], writes=[mod2], dma=True)
        h2T = sb(p2, "h2T", [128, 8, NMAIN * 128], BF16)
        comb = sb(p2, "comb", [128, NMAIN, 32])
        wr = sb(p2, "wr", [128, 8, 36], BF16)
        brbc = sb(p2, "brbc", [128, 36])
        wg = [sb(p2, "wg%d" % i, [128, 8, 512], BF16) for i in range(2)]
        wd = [sb(p2, "wd%d" % i, [128, 2, D], BF16) for i in range(2)]
        sg = [sb(p2, "sg%d" % i, [128, 2, 512], BF16) for i in range(2)]
        actT = [sb(p2, "actT%d" % i, [128, 2, 512], BF16) for i in range(2)]
        junk2 = sb(p2, "junk2", [128, D], BF16)
        ss2 = [sb(p2, "ss2_%d" % i, [128, 4]) for i in range(2)]
        tmp2 = [sb(p2, "tmp2_%d" % i, [128, D]) for i in range(2)]
        hb2t = [sb(p2, "hb2_%d" % i, [128, D], BF16) for i in range(2)]
        lgA = sb(p2, "lgA", [128, NMAIN, 36])
        rwa = sb(p2, "rwa", [128, 416])
        rwb = [sb(p2, "rwb%d" % i, [128, NMAIN, 32]) for i in range(2)]

        op("pool", lambda e: e.dma_start(out=wr[:, :, 0:4], in_=w_group.rearrange("(p k) n -> p k n", k=8)), writes=[wr], dma=True)
        op("pool", lambda e: e.dma_start(out=wr[:, :, 4:36], in_=w_expert.rearrange("(p k) n -> p k n", k=8)), writes=[wr], dma=True)
        op("sp", lambda e: e.dma_start(out=brbc[:, 0:4], in_=b_group.partition_broadcast(128)), writes=[brbc], dma=True)
        op("sp", lambda e: e.dma_start(out=brbc[:, 4:36], in_=b_expert.partition_broadcast(128)), writes=[brbc], dma=True)

        def load_expert(ei):
            slot = ei % 2
            op("pool", lambda e: e.dma_start(out=wg[slot][:, :, 0:256], in_=w_gate[ei].rearrange("(p k) f -> p k f", k=8)),
               writes=[wg[slot]], dma=True)
            op("pool", lambda e: e.dma_start(out=wg[slot][:, :, 256:512], in_=w_up[ei].rearrange("(p k) f -> p k f", k=8)),
               writes=[wg[slot]], dma=True)
            op("pool", lambda e: e.dma_start(out=wd[slot][:], in_=w_down[ei].rearrange("(j t) d -> j t d", t=2)),
               writes=[wd[slot]], dma=True)
            for t in range(2):
                op("pool", lambda e, t=t: e.tensor_tensor(out=wd[slot][:, t, :], in0=wd[slot][:, t, :], in1=GATE2, op=ALU.mult),
                   reads=[mod2], writes=[wd[slot]])

        if STOP_AFTER == 0:
            load_expert(0)
        BT = B[0]
        tv = bfv(BT).rearrange("p (a b) -> p a b", b=128)
        for j in range(NMAIN if STOP_AFTER == 0 else 0):
            par = j % 2
            xt = xres_t[:, j, :]; xb = xres_b[j]
            sst = ss2[par]
            op("act", lambda e, xt=xt, sst=sst: e.activation(out=junk2[:], in_=xt, func=AF.Square, accum_out=sst[:, 0:1]),
               reads=[xb], writes=[junk2, sst])
            rstd_from_ss(sst[:, 0:1], float(D), 1, sst, [sst])
            tf = tmp2[par]
            op("dve", lambda e, xt=xt, sst=sst, tf=tf: e.scalar_tensor_tensor(out=tf[:], in0=xt, scalar=sst[:, 0:1], in1=G2,
                                                                              op0=ALU.mult, op1=ALU.mult),
               reads=[xb, sst, mod2], writes=[tf])
            hbt = hb2t[par]
            op("pool", lambda e, tf=tf, hbt=hbt: e.tensor_tensor(out=hbt[:], in0=tf[:], in1=SHIFT2, op=ALU.add),
               reads=[tf, mod2], writes=[hbt])
            for kt in range(8):
                op("pe", lambda e, kt=kt, hbt=hbt: e.transpose(out=tv[:, kt, :], in_=hbt[:, kt::8], identity=IDENT),
                   reads=[hbt, cbf], writes=[BT])
            op("act", lambda e, j=j: e.activation(out=h2T[:, :, j * 128:(j + 1) * 128], in_=tv, func=AF.Copy), reads=[BT], writes=[h2T])
            for kt in range(8):
                op("pe", lambda e, kt=kt, j=j: e.matmul(B[1][:, 0:36], lhsT=h2T[:, kt, j * 128:(j + 1) * 128], rhs=wr[:, kt, :],
                                                        start=(kt == 0), stop=(kt == 7)), reads=[h2T, wr], writes=[B[1]])
            op("dve", lambda e, j=j: e.tensor_tensor(out=lgA[:, j, :], in0=B[1][:, 0:36], in1=brbc[:], op=ALU.add),
               reads=[B[1], brbc], writes=[lgA])

        if STOP_AFTER == 0:
            GL = lgA[:, :, 0:4]
            EL = lgA[:, :, 4:36]
            EL4 = EL.rearrange("p c (a b) -> p c a b", b=8)
            g1 = rwa[:, 0:16]; gs = rwa[:, 16:32]
            gd = rwa[:, 32:96].rearrange("p (c a) -> p c a", a=4)
            oh = rwa[:, 96:160].rearrange("p (c a) -> p c a", a=4)
            gw = rwa[:, 160:224].rearrange("p (c a) -> p c a", a=4)
            m1 = rwa[:, 224:288].rearrange("p (c a) -> p c a", a=4)
            m2 = rwa[:, 288:352].rearrange("p (c a) -> p c a", a=4)
            dn = rwa[:, 352:416].rearrange("p (c a) -> p c a", a=4)
            b4 = lambda t: t[:, :, :, None].to_broadcast([128, NMAIN, 4, 8])
            v4 = lambda t: t[:].rearrange("p c (a b) -> p c a b", b=8)
            RWA = [rwa, rwb[0], rwb[1]]
            op("dve", lambda e: e.tensor_reduce(out=g1, in_=GL, axis=AX.X, op=ALU.max), reads=[lgA], writes=RWA)
            op("dve", lambda e: e.tensor_tensor(out=gd, in0=GL, in1=g1[:, :, None].to_broadcast([128, NMAIN, 4]), op=ALU.subtract),
               reads=[lgA] + RWA, writes=RWA)
            op("act", lambda e: e.activation(out=gd, in_=gd, func=AF.Exp), reads=RWA, writes=RWA)
            op("dve", lambda e: e.tensor_reduce(out=gs, in_=gd, axis=AX.X, op=ALU.add), reads=RWA, writes=RWA)
            op("dve", lambda e: e.reciprocal(out=gs, in_=gs), reads=RWA, writes=RWA)
            op("dve", lambda e: e.tensor_tensor(out=oh, in0=GL, in1=g1[:, :, None].to_broadcast([128, NMAIN, 4]), op=ALU.is_equal),
               reads=[lgA] + RWA, writes=RWA)
            op("dve", lambda e: e.tensor_tensor(out=gw, in0=oh, in1=gs[:, :, None].to_broadcast([128, NMAIN, 4]), op=ALU.mult),
               reads=RWA, writes=RWA)
            op("dve", lambda e: e.tensor_reduce(out=m1, in_=EL4, axis=AX.X, op=ALU.max), reads=[lgA], writes=RWA)
            op("dve", lambda e: e.tensor_tensor(out=v4(rwb[0]), in0=EL4, in1=b4(m1), op=ALU.is_equal), reads=[lgA] + RWA, writes=RWA)
            op("dve", lambda e: e.scalar_tensor_tensor(out=rwb[1][:], in0=rwb[0][:], scalar=-1e30, in1=EL, op0=ALU.mult, op1=ALU.add),
               reads=[lgA] + RWA, writes=RWA)
            op("dve", lambda e: e.tensor_reduce(out=m2, in_=v4(rwb[1]), axis=AX.X, op=ALU.max), reads=RWA, writes=RWA)
            op("dve", lambda e: e.tensor_tensor(out=v4(rwb[0]), in0=EL4, in1=b4(m2), op=ALU.is_ge), reads=[lgA] + RWA, writes=RWA)
            op("dve", lambda e: e.tensor_tensor(out=v4(rwb[1]), in0=EL4, in1=b4(m1), op=ALU.subtract), reads=[lgA] + RWA, writes=RWA)
            op("act", lambda e: e.activation(out=rwb[1][:], in_=rwb[1][:], func=AF.Exp), reads=RWA, writes=RWA)
            op("dve", lambda e: e.tensor_tensor(out=rwb[1][:], in0=rwb[1][:], in1=rwb[0][:], op=ALU.mult), reads=RWA, writes=RWA)
            op("dve", lambda e: e.tensor_reduce(out=dn, in_=v4(rwb[1]), axis=AX.X, op=ALU.add), reads=RWA, writes=RWA)
            op("dve", lambda e: e.reciprocal(out=dn, in_=dn), reads=RWA, writes=RWA)
            op("dve", lambda e: e.tensor_tensor(out=dn, in0=dn, in1=gw, op=ALU.mult), reads=RWA, writes=RWA)
            op("dve", lambda e: e.tensor_tensor(out=v4(comb), in0=v4(rwb[1]), in1=b4(dn), op=ALU.mult), reads=RWA, writes=[comb])

        GU = [B[2], B[3], B[4], B[5]]
        YD = [[B[6], B[7]], [B[0], B[1]]]
        ydi = 0
        for ei in range(N_EXPERTS_RUN if STOP_AFTER == 0 else 0):
            slot = ei % 2
            if ei + 1 < N_EXPERTS_RUN:
                load_expert(ei + 1)
            wgt = wg[slot]; wdt = wd[slot]
            for G in range(4):
                sgt = sg[G % 2]; at = actT[G % 2]
                for part in range(4):
                    bk = GU[part]
                    c0 = (part // 2) * 256 + (part % 2)
                    for kt in range(8):
                        op("pe", lambda e, kt=kt, bk=bk, c0=c0, G=G, wgt=wgt: e.matmul(
                            bk[:, :], lhsT=wgt[:, kt, c0:c0 + 255:2], rhs=h2T[:, kt, G * 512:(G + 1) * 512],
                            start=(kt == 0), stop=(kt == 7)), reads=[wgt, h2T], writes=[bk])
                for ft in range(2):
                    op("act", lambda e, ft=ft, sgt=sgt: e.activation(out=sgt[:, ft, :], in_=GU[ft][:, :], func=AF.Silu),
                       reads=[GU[ft]], writes=[sgt])
                    op("dve", lambda e, ft=ft, sgt=sgt, at=at: e.tensor_tensor(out=at[:, ft, :], in0=GU[2 + ft][:, :], in1=sgt[:, ft, :],
                                                                              op=ALU.mult), reads=[GU[2 + ft], sgt], writes=[at])
                for tt in range(4):
                    jt = G * 4 + tt
                    yb = YD[ydi % 2]; ydi += 1
                    for half in range(2):
                        bk = yb[half]
                        for ft in range(2):
                            op("pe", lambda e, ft=ft, half=half, bk=bk, tt=tt, at=at, wdt=wdt: e.matmul(
                                bk[:, :], lhsT=at[:, ft, tt * 128:(tt + 1) * 128], rhs=wdt[:, ft, half * 512:(half + 1) * 512],
                                start=(ft == 0), stop=(ft == 1)), reads=[at, wdt], writes=[bk])
                        op("dve", lambda e, half=half, bk=bk, jt=jt, ei=ei: e.scalar_tensor_tensor(
                            out=xres_t[:, jt, half * 512:(half + 1) * 512], in0=bk[:, :], scalar=comb[:, jt, ei:ei + 1],
                            in1=xres_t[:, jt, half * 512:(half + 1) * 512], op0=ALU.mult, op1=ALU.add),
                           reads=[bk, comb], writes=[xres_b[jt]])
        for j in range(NMAIN):
            op("sp", lambda e, j=j: e.dma_start(out=out_d[j * 128:(j + 1) * 128, :], in_=xres_t[:, j, :]), reads=[xres_b[j]], dma=True)
        S.run()
        print('[sched] ops=%d sim_makespan=%.1f us' % (len(S.all), getattr(S, 'sim_makespan', 0.0)))
    return nc


_CONST = {}


def _consts():
    if not _CONST:
        i = np.arange(128)
        tri = (i[:, None] <= i[None, :]).astype(np.float32)
        slm = (i[:, None] > i[None, :]).astype(np.float32)
        ones = np.ones((128, 128), np.float32)
        invf = (500000.0 ** (-np.arange(8, dtype=np.float32) * 2.0 / 16.0)).astype(np.float32)
        cf32 = np.concatenate([tri, slm, ones, np.broadcast_to(invf, (128, 8))], axis=1).astype(np.float32)
        ident = np.eye(128, dtype=np.float32)
        cbf = np.concatenate([ident, tri, slm], axis=1).astype(ml_dtypes.bfloat16)
        _CONST["cf32"] = np.ascontiguousarray(cf32)
        _CONST["cbf"] = np.ascontiguousarray(cbf)
    return _CONST


_NC_CACHE = {}


def kernel(x, c, positions, norm1_w, norm2_w, w_ada, b_ada, w_in, conv_w, conv_b, dt_bias, a_log, d_skip,
           ssd_norm_w, q_norm_w, k_norm_w, sinks, w_out, w_group, b_group, w_expert, b_expert, w_gate, w_up, w_down):
    f32 = lambda a: np.ascontiguousarray(np.asarray(a, dtype=np.float32))
    x = f32(x); c = f32(c)
    positions = np.ascontiguousarray(np.asarray(positions, dtype=np.int32))
    cst = _consts()
    shared = {
        "cf32": cst["cf32"], "cbf": cst["cbf"],
        "norm1_w": f32(norm1_w), "norm2_w": f32(norm2_w), "w_ada": f32(w_ada), "b_ada": f32(b_ada),
        "w_in": f32(w_in), "conv_w": f32(conv_w), "conv_b": f32(conv_b), "dt_bias": f32(dt_bias),
        "a_log": f32(a_log), "d_skip": f32(d_skip), "ssd_norm_w": f32(ssd_norm_w), "q_norm_w": f32(q_norm_w),
        "k_norm_w": f32(k_norm_w), "sinks": f32(sinks), "w_out": f32(w_out), "w_group": f32(w_group),
        "b_group": f32(b_group), "w_expert": f32(w_expert), "b_expert": f32(b_expert),
        "w_gate": f32(w_gate[:N_EXPERTS_RUN]), "w_up": f32(w_up[:N_EXPERTS_RUN]), "w_down": f32(w_down[:N_EXPERTS_RUN]),
    }
    in_maps = []
    SEQ = x.shape[1]
    for core in range(NCORES):
        b, q = divmod(core, 4)
        t0 = q * NMAIN * 128 - NPRE * 128
        xe = np.zeros((NSLOT * 128, D), np.float32)
        flg = np.zeros((128, NSLOT), np.float32)
        posi = np.zeros((128, NMAIN + 1), np.int32)
        for s in range(NSLOT):
            ts = t0 + s * 128
            if ts >= 0:
                xe[s * 128:(s + 1) * 128] = x[b, ts:ts + 128]
                flg[:, s] = 1.0
                if s >= HALO:
                    posi[:, s - HALO] = positions[b, ts:ts + 128]
        m = dict(shared)
        m["xe"] = xe; m["flags"] = flg; m["posi"] = posi
        m["cvec"] = np.ascontiguousarray(c[b].reshape(128, 8))

        in_maps.append(m)
    if "nc" not in _NC_CACHE:
        _NC_CACHE["nc"] = build_program()
    nc = _NC_CACHE["nc"]
    res = run_bass_kernel_spmd(nc, in_maps, core_ids=list(range(NCORES)))
    out = np.empty((x.shape[0], SEQ, D), np.float32)
    for core in range(NCORES):
        b, q = divmod(core, 4)
        out[b, q * 2048:(q + 1) * 2048] = res.results[core]["out"]
    kernel.last_results = res
    return out
```

```python
import contextlib
import numpy as np
import ml_dtypes
import concourse.bass as bass
import concourse.mybir as mybir
from concourse.bass_utils import run_bass_kernel_spmd

F32 = mybir.dt.float32
BF16 = mybir.dt.bfloat16
I32 = mybir.dt.int32
AF = mybir.ActivationFunctionType
ALU = mybir.AluOpType
AX = mybir.AxisListType

NCORES = 8
D = 1024
NSLOT = 64
NMAIN = 16
NPRE = NSLOT - NMAIN
HALO = NPRE - 1
INW = 2312
EPS = 1e-6
NEXP = 32
TWO_PI = 6.283185307179586
C1 = 6.28125
C2 = TWO_PI - C1

DEBUG_X2 = False
N_EXPERTS_RUN = NEXP
N_PREFIX_SKIP = 0
STOP_AFTER = 0
USE_POW = True
STAGE_LIMIT = 99
MAIN_PAIRS = True
HOP = 0.8
NO_CC = False
CC_TEST = False


class Buf:
    __slots__ = ("name", "w", "r", "psum")

    def __init__(self, name="", psum=False):
        self.name = name
        self.w = []
        self.r = []
        self.psum = psum


class Tile:
    def __init__(self, t, name=""):
        self.t = t
        self.b = Buf(name)

    def __getitem__(self, idx):
        return self.t[idx]


class View:
    def __init__(self, ap, buf):
        self.t = ap
        self.b = buf

    def __getitem__(self, idx):
        return self.t[idx]


class Op:
    __slots__ = ("id", "eng", "fn", "deps", "dma", "cost", "xfer", "tok", "succ", "cc")

    def __init__(self, id, eng, fn, deps, dma, cost, xfer):
        self.id = id; self.eng = eng; self.fn = fn; self.deps = deps; self.dma = dma
        self.cost = cost; self.xfer = xfer; self.tok = None; self.succ = False; self.cc = False


class Probe:
    def __init__(self):
        self.name = None; self.args = (); self.kw = {}

    def __getattr__(self, name):
        def f(*args, **kw):
            self.name = name; self.args = args; self.kw = kw
            return self
        return f


def _fsz(ap):
    v = ap.free_size
    return v() if callable(v) else v


def _nb(ap):
    v = ap.nbytes
    return v() if callable(v) else v


class Sched:
    ENGS = ("pe", "act", "dve", "pool", "sp")
    NDSEM = 12
    WINDOW = 320

    def __init__(self, nc):
        self.nc = nc
        self.all = []
        self.fence = []
        self.leaves = set()
        self.reorder = True

    @staticmethod
    def est(eng, n, fp32):
        if eng == "pe":
            c = 0.11 if n <= 128 else n / 2400.0 + 0.02
            return c * (4 if fp32 else 1)
        if eng == "act":
            return 0.32 + n / 1200.0
        if eng == "dve":
            return 0.2 + n / 960.0
        if eng == "pool":
            return 0.45 + n / 450.0
        return 0.1

    def op(self, eng, fn, reads=(), writes=(), dma=False, n=64, fp32=False, nbytes=0, cc=False):
        reads = [x.b if isinstance(x, (Tile, View)) else x for x in reads]
        writes = [x.b if isinstance(x, (Tile, View)) else x for x in writes]
        deps = set(self.fence)
        for b in reads:
            deps.update(b.w)
            if b.psum:
                deps.update(i for i in b.r if self.all[i].eng != eng)
        for b in writes:
            deps.update(b.w)
            deps.update(b.r)
        oid = len(self.all)
        pr = Probe()
        fn(pr)
        if cc:
            cost = 2.0; xfer = 30.0
        elif dma:
            nbytes = _nb(pr.kw["out"])
            cost = 1.2 if eng == "pool" else 0.15
            xfer = 2.0 + nbytes / 150e3
        else:
            if pr.name == "matmul":
                n = _fsz(pr.kw["rhs"]); fp32 = pr.kw["rhs"].dtype == F32
            elif pr.name == "transpose":
                n = 128
            else:
                o_ = pr.kw.get("out", pr.args[0] if pr.args else None)
                n = _fsz(o_) if o_ is not None else 64
                if eng == "dve" and pr.name in ("tensor_tensor", "scalar_tensor_tensor"):
                    a0 = pr.kw.get("in0"); a1 = pr.kw.get("in1")
                    if a0 is not None and a1 is not None and a0.dtype == F32 and a1.dtype == F32 \
                            and str(a0.space) == str(a1.space):
                        n = int(n * 1.3)
            cost = self.est(eng, n, fp32)
            if eng == "pool" and pr.kw.get("op", None) == ALU.pow:
                cost = 1.6
            xfer = 0.0
        o = Op(oid, eng, fn, deps, dma or cc, cost, xfer)
        o.cc = cc
        self.all.append(o)
        for d in deps:
            if not self.all[d].succ:
                self.all[d].succ = True
                self.leaves.discard(d)
        self.leaves.add(oid)
        for b in writes:
            b.w = [oid]
            b.r = []
        for b in reads:
            if b not in writes:
                b.r.append(oid)
        return oid

    def barrier(self, fn):
        deps = set(self.leaves) | set(self.fence)
        oid = len(self.all)
        o = Op(oid, "dve", fn, deps, False, 0.1, 0.0)
        self.all.append(o)
        for d in deps:
            self.all[d].succ = True
        self.leaves = {oid}
        self.fence = [oid]

    def schedule(self):
        ops = self.all
        per = {e: [o.id for o in ops if o.eng == e] for e in self.ENGS}
        if not self.reorder:
            return per
        head = {e: 0 for e in self.ENGS}
        done = [False] * len(ops)
        fin = [0.0] * len(ops)
        free = {e: 0.0 for e in self.ENGS}
        dma_free = 0.0
        order = {e: [] for e in self.ENGS}
        remaining = len(ops)
        INF = 1e30
        while remaining:
            best = None
            for e in self.ENGS:
                lst = per[e]
                h = head[e]
                while h < len(lst) and done[lst[h]]:
                    h += 1
                head[e] = h
                cnt = 0
                k = h
                while k < len(lst) and cnt < self.WINDOW:
                    oid = lst[k]
                    k += 1
                    if done[oid]:
                        continue
                    cnt += 1
                    o = ops[oid]
                    rdy = 0.0
                    ok = True
                    for d in o.deps:
                        if not done[d]:
                            ok = False
                            break
                        fd = fin[d] - (HOP if (ops[d].eng == e and not ops[d].dma) else 0.0)
                        if fd > rdy:
                            rdy = fd
                    if not ok:
                        continue
                    st = rdy if rdy > free[e] else free[e]
                    key = (st, oid)
                    if best is None or key < best[0]:
                        best = (key, e, oid)
                    if rdy <= free[e]:
                        break
            assert best is not None, "scheduler stuck (cyclic deps?)"
            (st, _), e, oid = best
            o = ops[oid]
            done[oid] = True
            remaining -= 1
            free[e] = st + o.cost
            if o.dma:
                t0 = max(st + o.cost, dma_free)
                dma_free = t0 + (o.xfer - 2.0)
                fin[oid] = t0 + o.xfer
            else:
                fin[oid] = st + o.cost + HOP
            order[e].append(oid)
        self.sim_makespan = max(fin) if fin else 0.0
        return order

    def run(self):
        nc = self.nc
        ops = self.all
        order = self.schedule()
        cnt = {}
        rr = {}
        prevdma = {}
        for e in self.ENGS:
            for oid in order[e]:
                o = ops[oid]
                if o.cc:
                    s = "cc%d" % oid
                    cnt[s] = 1
                    o.tok = (s, 1)
                    prevdma[oid] = (s, 0)
                    continue
                if o.dma:
                    k = rr.get(e, 0)
                    rr[e] = (k + 1) % self.NDSEM
                    s = "d_%s_%d" % (e, k)
                    prevdma[oid] = (s, cnt.get(s, 0))
                    cnt[s] = cnt.get(s, 0) + 16
                else:
                    s = e
                    cnt[s] = cnt.get(s, 0) + 1
                o.tok = (s, cnt[s])
        streams = {}
        for e in self.ENGS:
            waited = {}
            st = []
            for oid in order[e]:
                o = ops[oid]
                need = {}
                for d in o.deps:
                    s, v = ops[d].tok
                    if v > need.get(s, 0):
                        need[s] = v
                if o.dma and prevdma[oid][1]:
                    s, v = prevdma[oid]
                    need[s] = max(need.get(s, 0), v)
                waits = []
                for s, v in need.items():
                    if e == "pe" and s == "pe":
                        continue
                    if waited.get(s, 0) >= v:
                        continue
                    waited[s] = v
                    waits.append((s, v))
                st.append((waits, o))
            if e == "sp":
                waits = [(s, v) for s, v in cnt.items() if waited.get(s, 0) < v]
                st.append((waits, None))
            streams[e] = st
        self._check_deadlock(streams)
        with contextlib.ExitStack() as stck:
            sems = {n: stck.enter_context(nc.semaphore("s_" + n)) for n in sorted(cnt)}
            block = stck.enter_context(nc.Block())

            def play(engname):
                def body(eh):
                    for waits, o in streams[engname]:
                        for (s, v) in waits:
                            eh.wait_ge(sems[s], v)
                        if o is None:
                            continue
                        ins = o.fn(eh)
                        if o.cc:
                            ins.then_inc(sems[o.tok[0]])
                        else:
                            ins.then_inc(sems[o.tok[0]], 16 if o.dma else 1)
                return body

            block.tensor(play("pe"))
            block.scalar(play("act"))
            block.vector(play("dve"))
            block.gpsimd(play("pool"))
            block.sync(play("sp"))

    def _check_deadlock(self, streams):
        val = {}
        pos = {e: 0 for e in self.ENGS}
        progress = True
        while progress:
            progress = False
            for e in self.ENGS:
                st = streams[e]
                while pos[e] < len(st):
                    waits, o = st[pos[e]]
                    if any(val.get(s, 0) < v for s, v in waits):
                        break
                    if o is not None:
                        val[o.tok[0]] = val.get(o.tok[0], 0) + (1 if o.cc else (16 if o.dma else 1))
                    pos[e] += 1
                    progress = True
        for e in self.ENGS:
            assert pos[e] == len(streams[e]), "DEADLOCK in engine %s at %d/%d" % (e, pos[e], len(streams[e]))


def build_program():
    nc = bass.Bass("TRN2", target_bir_lowering=False, dynamic_dma_scratch_size=4096)

    def din(name, shape, dt=F32):
        return nc.dram_tensor(name, list(shape), dt, kind="ExternalInput").ap()

    xe = din("xe", [NSLOT * 128, D])
    flags_d = din("flags", [128, NSLOT])
    posi_d = din("posi", [128, NMAIN + 1], I32)
    cvec_d = din("cvec", [128, 8])

    cf32_d = din("cf32", [128, 3 * 128 + 8])
    cbf_d = din("cbf", [128, 3 * 128], BF16)
    norm1_w = din("norm1_w", [D]); norm2_w = din("norm2_w", [D])
    w_ada = din("w_ada", [D, 6 * D]); b_ada = din("b_ada", [6 * D])
    w_in = din("w_in", [D, INW])
    conv_w = din("conv_w", [4, D]); conv_b = din("conv_b", [D])
    dt_bias = din("dt_bias", [8]); a_log = din("a_log", [8]); d_skip = din("d_skip", [8])
    ssd_norm_w = din("ssd_norm_w", [512])
    q_norm_w = din("q_norm_w", [64]); k_norm_w = din("k_norm_w", [64])
    sinks = din("sinks", [8])
    w_out = din("w_out", [D, D])
    w_group = din("w_group", [D, 4]); b_group = din("b_group", [4])
    w_expert = din("w_expert", [D, 32]); b_expert = din("b_expert", [32])
    w_gate = din("w_gate", [N_EXPERTS_RUN, D, 256]); w_up = din("w_up", [N_EXPERTS_RUN, D, 256])
    w_down = din("w_down", [N_EXPERTS_RUN, 256, D])
    out_d = nc.dram_tensor("out", [NMAIN * 128, D], F32, kind="ExternalOutput").ap()
    if DEBUG_X2:
        dbg_d = nc.dram_tensor("dbg", [NMAIN * 128, D], F32, kind="ExternalOutput").ap()

    S = Sched(nc)
    op = S.op

    with contextlib.ExitStack() as root:
        def sb(stack, name, shape, dt=F32):
            return Tile(stack.enter_context(nc.sbuf_tensor("sb_" + name, list(shape), dt)), name)

        banks = [Tile(root.enter_context(nc.psum_tensor("bank%d" % i, [128, 512], F32)), "bank%d" % i)
                 for i in range(8)]
        for bk_ in banks:
            bk_.b.psum = True

        def bfv(bank):
            return bank.t[:].bitcast(BF16)

        xres = [None] * NMAIN
        xres_t = root.enter_context(nc.sbuf_tensor("sb_xres", [128, NMAIN, D], F32))
        xres_b = [Buf("xres%d" % j) for j in range(NMAIN)]
        cf32 = sb(root, "cf32", [128, 3 * 128 + 8])
        cbf = sb(root, "cbf", [128, 3 * 128], BF16)
        flags = sb(root, "flags", [128, NSLOT])
        neghalf = sb(root, "neghalf", [128, 32])
        TRI = cf32[:, 0:128]; SLM = cf32[:, 128:256]; ONES = cf32[:, 256:384]; INVF = cf32[:, 384:392]
        IDENT = cbf[:, 0:128]; MASKC = cbf[:, 128:256]; MASKP = cbf[:, 256:384]

        modscr = nc.dram_tensor("modscr", [128, 3 * D], F32, kind="Internal").ap()

        op("sp", lambda e: e.dma_start(out=cf32[:], in_=cf32_d), writes=[cf32], dma=True)
        op("sp", lambda e: e.dma_start(out=cbf[:], in_=cbf_d), writes=[cbf], dma=True)
        op("sp", lambda e: e.dma_start(out=flags[:], in_=flags_d), writes=[flags], dma=True)
        op("dve", lambda e: e.memset(neghalf[:, 0:16], -0.5), writes=[neghalf])

        p1 = root.enter_context(contextlib.ExitStack())
        mod1 = sb(p1, "mod1", [128, 2 * D])
        SHIFT1 = mod1[:, 0:1024]; G1 = mod1[:, 1024:2048]
        win = sb(p1, "win", [128, 8, INW], BF16)
        wout = sb(p1, "wout", [128, 8, D], BF16)
        convdiag = sb(p1, "convdiag", [128, 32, 128], BF16)
        cbrow = sb(p1, "cbrow", [1, D], BF16)
        onesrow = sb(p1, "onesrow", [1, 256], BF16)
        dI = sb(p1, "dI", [128, 8, 128], BF16)
        small = sb(p1, "small", [128, 8 * 6])
        DTB = small[:, 0:8]; ABC = small[:, 8:16]; DSK = small[:, 16:24]; SNK = small[:, 24:32]
        ESINK = small[:, 32:40]
        negc = sb(p1, "negc", [128, 1])
        snw = sb(p1, "snw", [128, 512])
        qkw = sb(p1, "qkw", [128, 2, 64])
        cossin = sb(p1, "cossin", [128, 2, NMAIN + 1, 8])
        maskp0 = sb(p1, "maskp0", [128, 128], BF16)
        negm = sb(p1, "negm", [128, 3, 4, 128], BF16)

        with contextlib.ExitStack() as p0:
            mod = sb(p0, "mod", [128, 6 * D])
            GATE1 = mod[:, 2048:3072]
            cvec = sb(p0, "cvec", [128, 8])
            screp = sb(p0, "screp", [128, 8, 128])
            wst = [sb(p0, "wst%d" % i, [128, 8, 256]) for i in range(2)]
            nbc = sb(p0, "nbc", [128, D])
            cwT = sb(p0, "cwT", [128, 4, 8])
            cbst = sb(p0, "cbst", [1, D])
            posi = sb(p0, "posi", [128, NMAIN + 1], I32)
            posf = sb(p0, "posf", [128, NMAIN + 1])
            ang = sb(p0, "ang", [128, NMAIN + 1, 8])
            kk = sb(p0, "kk", [128, NMAIN + 1, 8])
            kki = sb(p0, "kki", [128, NMAIN + 1, 8], I32)
            mx = sb(p0, "mx", [128, 2])

            op("sp", lambda e: e.dma_start(out=cvec[:], in_=cvec_d), writes=[cvec], dma=True)
            op("sp", lambda e: e.dma_start(out=mod[:], in_=b_ada.partition_broadcast(128)), writes=[mod], dma=True)
            win_src = w_in.rearrange("(p k) n -> p k n", k=8)
            for lo, hi in ((0, 1156), (1156, INW)):
                op("pool", lambda e, lo=lo, hi=hi: e.dma_start(out=win[:, :, lo:hi], in_=win_src[:, :, lo:hi]),
                   writes=[win], dma=True)
            op("pool", lambda e: e.dma_start(out=wout[:], in_=w_out.rearrange("(p k) n -> p k n", k=8)),
               writes=[wout], dma=True)

            op("act", lambda e: e.activation(out=cvec[:], in_=cvec[:], func=AF.Silu), reads=[cvec], writes=[cvec])
            op("dve", lambda e: e.tensor_copy(out=screp[:], in_=cvec[:, :, None].to_broadcast([128, 8, 128])),
               reads=[cvec], writes=[screp])
            wada_src = w_ada.rearrange("(p k) n -> p k n", k=8)
            NB = 24
            for nb in range(NB):
                ws = wst[nb % 2]
                bk = banks[nb % 2]
                op("sp", lambda e, ws=ws, nb=nb: e.dma_start(out=ws[:], in_=wada_src[:, :, nb * 256:(nb + 1) * 256]),
                   writes=[ws], dma=True)
                for kt in range(8):
                    op("pe", lambda e, ws=ws, bk=bk, kt=kt: e.matmul(bk[:, 0:256], lhsT=screp[:, kt, :], rhs=ws[:, kt, :],
                                                                     start=(kt == 0), stop=(kt == 7)),
                       reads=[screp, ws], writes=[bk])
                op("dve", lambda e, bk=bk, nb=nb: e.tensor_tensor(out=mod[:, nb * 256:(nb + 1) * 256], in0=bk[:, 0:256],
                                                                 in1=mod[:, nb * 256:(nb + 1) * 256], op=ALU.add),
                   reads=[bk], writes=[mod])
            op("sp", lambda e: e.dma_start(out=nbc[:], in_=norm1_w.partition_broadcast(128)), writes=[nbc], dma=True)
            op("dve", lambda e: e.scalar_tensor_tensor(out=G1, in0=mod[:, 1024:2048], scalar=1.0, in1=nbc[:], op0=ALU.add, op1=ALU.mult),
               reads=[nbc, mod], writes=[mod1])
            op("dve", lambda e: e.tensor_copy(out=SHIFT1, in_=mod[:, 0:1024]), reads=[mod], writes=[mod1])
            op("sp", lambda e: e.dma_start(out=nbc[:], in_=norm2_w.partition_broadcast(128)), writes=[nbc], dma=True)
            op("dve", lambda e: e.scalar_tensor_tensor(out=mod[:, 4096:5120], in0=mod[:, 4096:5120], scalar=1.0, in1=nbc[:], op0=ALU.add, op1=ALU.mult),
               reads=[nbc], writes=[mod])
            modscr_b = Buf("modscr")
            op("sp", lambda e: e.dma_start(out=modscr, in_=mod[:, 3072:6144]), reads=[mod], writes=[modscr_b], dma=True)
            for kt in range(8):
                op("dve", lambda e, kt=kt: e.tensor_tensor(out=wout[:, kt, :], in0=wout[:, kt, :], in1=GATE1, op=ALU.mult),
                   reads=[mod], writes=[wout])
                op("pool", lambda e, kt=kt: e.tensor_scalar(out=win[:, kt, 768:1280], in0=win[:, kt, 768:1280],
                                                            scalar1=0.5, scalar2=1.0, op0=ALU.mult, op1=ALU.mult),
                   writes=[win])
            op("sp", lambda e: e.dma_start(out=cwT[:], in_=conv_w.rearrange("k (c p) -> p k c", p=128),
                                           allow_slow_non_contiguous=True), writes=[cwT], dma=True)
            for k in range(4):
                for ct in range(8):
                    op("dve", lambda e, k=k, ct=ct: e.tensor_scalar(out=convdiag[:, k * 8 + ct, :], in0=IDENT,
                                                                   scalar1=cwT[:, k, ct:ct + 1], scalar2=0.5,
                                                                   op0=ALU.mult, op1=ALU.mult),
                       reads=[cbf, cwT], writes=[convdiag])
            op("sp", lambda e: e.dma_start(out=cbst[:], in_=conv_b.rearrange("(o n) -> o n", o=1)), writes=[cbst], dma=True)
            op("dve", lambda e: e.tensor_scalar(out=cbrow[:], in0=cbst[:], scalar1=0.5, scalar2=None, op0=ALU.mult),
               reads=[cbst], writes=[cbrow])
            op("dve", lambda e: e.memset(onesrow[:], 1.0), writes=[onesrow])
            for i, src in enumerate((dt_bias, a_log, d_skip, sinks)):
                op("sp", lambda e, i=i, src=src: e.dma_start(out=small[:, i * 8:(i + 1) * 8], in_=src.partition_broadcast(128)),
                   writes=[small], dma=True)
            op("act", lambda e: e.activation(out=ABC, in_=ABC, func=AF.Exp), reads=[small], writes=[small])
            op("dve", lambda e: e.tensor_scalar(out=ABC, in0=ABC, scalar1=-1.0, scalar2=None, op0=ALU.mult),
               reads=[small], writes=[small])
            for h in range(8):
                op("dve", lambda e, h=h: e.tensor_scalar(out=dI[:, h, :], in0=IDENT, scalar1=DSK[:, h:h + 1], scalar2=None,
                                                         op0=ALU.mult), reads=[cbf, small], writes=[dI])
            op("sp", lambda e: e.dma_start(out=snw[:], in_=ssd_norm_w.partition_broadcast(128)), writes=[snw], dma=True)
            op("sp", lambda e: e.dma_start(out=qkw[:, 0, :], in_=q_norm_w.partition_broadcast(128)), writes=[qkw], dma=True)
            op("sp", lambda e: e.dma_start(out=qkw[:, 1, :], in_=k_norm_w.partition_broadcast(128)), writes=[qkw], dma=True)
            op("dve", lambda e: e.tensor_reduce(out=mx[:, 0:1], in_=qkw[:, 0, :], axis=AX.X, op=ALU.max,
                                                apply_absolute_value=True), reads=[qkw], writes=[mx])
            op("dve", lambda e: e.tensor_reduce(out=mx[:, 1:2], in_=qkw[:, 1, :], axis=AX.X, op=ALU.max,
                                                apply_absolute_value=True), reads=[qkw], writes=[mx])
            op("dve", lambda e: e.tensor_tensor(out=negc[:], in0=mx[:, 0:1], in1=mx[:, 1:2], op=ALU.mult),
               reads=[mx], writes=[negc])
            op("dve", lambda e: e.tensor_scalar(out=negc[:], in0=negc[:], scalar1=-8.0, scalar2=None, op0=ALU.mult),
               reads=[negc], writes=[negc])
            op("act", lambda e: e.activation(out=ESINK, in_=SNK, func=AF.Exp, bias=negc[:]), reads=[small, negc],
               writes=[small])
            op("sp", lambda e: e.dma_start(out=posi[:], in_=posi_d), writes=[posi], dma=True)
            op("dve", lambda e: e.tensor_copy(out=posf[:], in_=posi[:]), reads=[posi], writes=[posf])
            op("dve", lambda e: e.tensor_tensor(out=ang[:], in0=posf[:, :, None].to_broadcast([128, NMAIN + 1, 8]),
                                                in1=INVF[:, None, :].to_broadcast([128, NMAIN + 1, 8]), op=ALU.mult),
               reads=[posf, cf32], writes=[ang])
            op("dve", lambda e: e.tensor_scalar(out=kk[:], in0=ang[:], scalar1=1.0 / TWO_PI, scalar2=None, op0=ALU.mult),
               reads=[ang], writes=[kk])
            op("dve", lambda e: e.tensor_copy(out=kki[:], in_=kk[:]), reads=[kk], writes=[kki])
            op("dve", lambda e: e.tensor_copy(out=kk[:], in_=kki[:]), reads=[kki], writes=[kk])
            op("dve", lambda e: e.scalar_tensor_tensor(out=ang[:], in0=kk[:], scalar=-C1, in1=ang[:], op0=ALU.mult, op1=ALU.add),
               reads=[kk, ang], writes=[ang])
            op("dve", lambda e: e.scalar_tensor_tensor(out=ang[:], in0=kk[:], scalar=-C2, in1=ang[:], op0=ALU.mult, op1=ALU.add),
               reads=[kk, ang], writes=[ang])
            for _ in range(2):
                for cmpop, sgn in ((ALU.is_gt, -1.0), (ALU.is_lt, 1.0)):
                    thr = np.pi if sgn < 0 else -np.pi
                    op("dve", lambda e, cmpop=cmpop, thr=thr, sgn=sgn: e.tensor_scalar(
                        out=kk[:], in0=ang[:], scalar1=float(thr), scalar2=float(sgn * TWO_PI), op0=cmpop, op1=ALU.mult),
                       reads=[ang], writes=[kk])
                    op("dve", lambda e: e.tensor_tensor(out=ang[:], in0=ang[:], in1=kk[:], op=ALU.add),
                       reads=[kk, ang], writes=[ang])
            op("act", lambda e: e.activation(out=cossin[:, 1, :, :], in_=ang[:], func=AF.Sin), reads=[ang], writes=[cossin])
            op("dve", lambda e: e.tensor_scalar(out=ang[:], in0=ang[:], scalar1=float(np.pi / 2), scalar2=None, op0=ALU.add),
               reads=[ang, cossin], writes=[ang])
            op("dve", lambda e: e.tensor_scalar(out=kk[:], in0=ang[:], scalar1=float(np.pi), scalar2=float(-TWO_PI),
                                                op0=ALU.is_gt, op1=ALU.mult), reads=[ang], writes=[kk])
            op("dve", lambda e: e.tensor_tensor(out=ang[:], in0=ang[:], in1=kk[:], op=ALU.add), reads=[kk, ang], writes=[ang])
            op("act", lambda e: e.activation(out=cossin[:, 0, :, :], in_=ang[:], func=AF.Sin), reads=[ang], writes=[cossin])
            op("dve", lambda e: e.tensor_scalar(out=maskp0[:], in0=MASKP, scalar1=flags[:, HALO:HALO + 1], scalar2=None,
                                                op0=ALU.mult), reads=[cbf, flags], writes=[maskp0])
            for mi, (msrc, mb_) in enumerate(((MASKP, cbf), (MASKC, cbf), (maskp0[:], maskp0))):
                for i4 in range(4):
                    op("dve", lambda e, mi=mi, i4=i4, msrc=msrc: e.tensor_scalar(out=negm[:, mi, i4, :], in0=msrc, scalar1=-1.0, scalar2=30000.0,
                                                                                op0=ALU.add, op1=ALU.mult), reads=[mb_], writes=[negm])
            if DEBUG_X2 and STOP_AFTER == 1:
                op("sp", lambda e: e.dma_start(out=dbg_d[0:128, :], in_=SHIFT1), reads=[mod1], dma=True)
                op("sp", lambda e: e.dma_start(out=dbg_d[128:256, :], in_=G1), reads=[mod1], dma=True)
                op("sp", lambda e: e.dma_start(out=dbg_d[256:384, 0:272], in_=cossin[:].rearrange("p a b c -> p (a b c)")), reads=[cossin], dma=True)
                op("sp", lambda e: e.dma_start(out=dbg_d[384:512, 0:48], in_=small[:]), reads=[small], dma=True)
                op("sp", lambda e: e.dma_start(out=dbg_d[640:768, :], in_=mod[:, 2048:3072]), reads=[mod], dma=True)
            S.barrier(lambda e: e.memset(neghalf[:, 16:32], -0.5))

        shr = sb(p1, "shr", [128, D])
        tmpf1 = shr
        xpf = [sb(p1, "xpf%d" % i, [128, D]) for i in range(2)]
        yo = sb(p1, "yo", [128, 512])
        ss = [sb(p1, "ss%d" % i, [128, 4]) for i in range(2)]
        hb = [sb(p1, "hb%d" % i, [128, D], BF16) for i in range(2)]
        hT2 = [sb(p1, "hT2_%d" % i, [128, 8, 256], BF16) for i in range(2)]
        hT = [View(hT2[i][:, :, 0:128], hT2[i].b) for i in range(2)]
        xpre2 = [sb(p1, "xpre2_%d" % i, [128, 8, 260], BF16) for i in range(2)]
        xpre = [View(xpre2[i][:, :, 0:132], xpre2[i].b) for i in range(2)]
        tcv2 = sb(p1, "tcv2", [128, 6, 256], BF16)
        tcv = View(tcv2[:].rearrange("p a b -> p (a b)")[:, 0:1024].rearrange("p (a b) -> p a b", b=128), tcv2.b)
        xc2 = sb(p1, "xc2", [128, 8, 256], BF16)
        xc_ = [View(xc2[:, :, i * 128:(i + 1) * 128], Buf("xc%d" % i)) for i in range(2)]
        dtw2 = sb(p1, "dtw2", [128, 12, 16])
        dtw_ = [View(dtw2[:, :, i * 8:(i + 1) * 8], Buf("dtw%d" % i)) for i in range(2)]
        xdt_ = [sb(p1, "xdt%d" % i, [128, 8, 64], BF16) for i in range(2)]
        xdte2 = sb(p1, "xdte2", [128, 2, 8, 64], BF16)
        xdte_ = [View(xdte2[:, i, :, :], Buf("xdte%d" % i)) for i in range(2)]
        xstok_ = [sb(p1, "xstok%d" % i, [128, 8, 64], BF16) for i in range(2)]
        btok2 = sb(p1, "btok2", [128, 2, 2, 128], BF16)
        btok_ = [View(btok2[:, i, :, :], Buf("btok%d" % i)) for i in range(2)]
        R = sb(p1, "R", [128, 8, 64])
        Rb = sb(p1, "Rb", [128, 8, 64], BF16)
        gtm = sb(p1, "gtm", [128, 2, 128], BF16)
        tz = sb(p1, "tz", [128, 512], BF16)
        sz_ = [sb(p1, "sz%d" % i, [128, 512], BF16) for i in range(2)]
        ssg = sb(p1, "ssg", [128, 4])
        sq = sb(p1, "sq", [128, 10, 64], BF16)
        junk = View(sq[:].rearrange("p a b -> p (a b)")[:, 0:512], sq.b)
        ssq = sb(p1, "ssq", [128, 16])
        qn = sb(p1, "qn", [128, 10, 64])
        Lm = View(qn[:].rearrange("p a b -> p (a b)")[:, 0:512].rearrange("p (a b) -> p a b", b=128), qn.b)
        rt = sb(p1, "rt", [128, 4, 10, 8])
        qr_ = [sb(p1, "qr%d" % i, [128, 10, 64], BF16) for i in range(2)]
        qT = sb(p1, "qT", [128, 4, 128], BF16)
        kT = [sb(p1, "kT%d" % i, [128, 128], BF16) for i in range(2)]
        vaug = [sb(p1, "vaug%d" % i, [128, 2, 65], BF16) for i in range(2)]
        pT = sb(p1, "pT", [128, 4, 512], BF16)
        decT = View(pT[:].rearrange("p a b -> p (a b)")[:, 0:1024].rearrange("p (a b) -> p a b", b=128), pT.b)
        den = sb(p1, "den", [128, 8])
        mix = sb(p1, "mix", [128, D], BF16)
        mixT = sb(p1, "mixT", [128, 8, 128], BF16)

        op("dve", lambda e: e.memset(R[:], 0.0), writes=[R])
        for i in range(2):
            op("dve", lambda e, i=i: e.memset(xpre[i][:], 0.0), writes=[xpre[i]])
            op("dve", lambda e, i=i: e.memset(vaug[i][:], 1.0), writes=[vaug[i]])
            op("dve", lambda e, i=i: e.memset(kT[i][:], 0.0), writes=[kT[i]])

        B = banks
        bank_rr = [0]

        def nb():
            bk = banks[bank_rr[0] % 8]
            bank_rr[0] += 1
            return bk

        def tview(bk):
            return bfv(bk).rearrange("p (a b) -> p a b", b=128)

        def rstd_from_ss(ssap, n, width, tile, reads):
            op("dve", lambda e: e.tensor_scalar(out=ssap, in0=ssap, scalar1=1.0 / n, scalar2=EPS, op0=ALU.mult, op1=ALU.add),
               reads=reads, writes=[tile])
            if USE_POW:
                op("pool", lambda e: e.tensor_tensor(out=ssap, in0=ssap, in1=neghalf[:, 0:width], op=ALU.pow),
                   reads=[tile, neghalf], writes=[tile])
            else:
                op("act", lambda e: e.activation(out=ssap, in_=ssap, func=AF.Sqrt), reads=[tile], writes=[tile])
                op("dve", lambda e: e.reciprocal(out=ssap, in_=ssap), reads=[tile], writes=[tile])

        NPAIR = 23

        def frontend(s):
            par = s % 2
            main = s >= NPRE
            if s < 2 * NPAIR:
                hdst = hT2[(s // 2) % 2]
                hdst_ap = hdst[:, :, (s % 2) * 128:(s % 2 + 1) * 128]
            elif main and MAIN_PAIRS:
                hdst = hT2[((s - NPRE) // 2) % 2]
                hdst_ap = hdst[:, :, ((s - NPRE) % 2) * 128:((s - NPRE) % 2 + 1) * 128]
            else:
                hdst = hT[par]
                hdst_ap = hdst[:]
            j = s - NPRE
            if main:
                xt = xres_t[:, j, :]; xb = xres_b[j]
            else:
                xt = xpf[par][:]; xb = xpf[par].b
            op("sp", lambda e: e.dma_start(out=xt, in_=xe[s * 128:(s + 1) * 128, :]), writes=[xb], dma=True)
            sst = ss[par]
            hbt = hb[par]
            op("act", lambda e: e.activation(out=hbt[:], in_=xt, func=AF.Square, accum_out=sst[:, 0:1]),
               reads=[xb], writes=[hbt, sst])
            rstd_from_ss(sst[:, 0:1], float(D), 1, sst, [sst])
            if main:
                tfa = tmpf1[:]; tfb = tmpf1.b
            else:
                tfa = xt; tfb = xb
            op("dve", lambda e: e.scalar_tensor_tensor(out=tfa, in0=xt, scalar=sst[:, 0:1], in1=G1, op0=ALU.mult, op1=ALU.mult),
               reads=[xb, sst, mod1], writes=[tfb])
            op("pool", lambda e: e.tensor_tensor(out=hbt[:], in0=tfa, in1=SHIFT1, op=ALU.add), reads=[tfb, mod1], writes=[hbt])
            TH = nb()
            tv = tview(TH)
            for kt in range(8):
                op("pe", lambda e, kt=kt: e.transpose(out=tv[:, kt, :], in_=hbt[:, kt::8], identity=IDENT),
                   reads=[hbt, cbf], writes=[TH])
            op("act", lambda e: e.activation(out=hdst_ap, in_=tv, func=AF.Copy), reads=[TH], writes=[hdst])

        def proj_tok(bank, ncols, col0, h):
            for kt in range(8):
                op("pe", lambda e, kt=kt: e.matmul(bank[:, 0:ncols], lhsT=h[:, kt, :], rhs=win[:, kt, col0:col0 + ncols],
                                                   start=(kt == 0), stop=(kt == 7)), reads=[h, win], writes=[bank])

        def backend2(s, pre_done=False):
            par = s % 2
            main = s >= NPRE
            halo = s == HALO
            j = s - NPRE
            h = hT[par]
            if pre_done:
                h = View(hT2[(j // 2) % 2][:, :, (j % 2) * 128:(j % 2 + 1) * 128], hT2[(j // 2) % 2].b)
            fl = flags[:, s:s + 1]
            nct = 8 if (main or halo) else 6
            xc = xc_[par]; dtw = dtw_[par]; xdt = xdt_[par]; xdte = xdte_[par]; xstok = xstok_[par]; btok = btok_[par]
            sz = sz_[par]; qr = qr_[par]
            V = dtw[:, 0, :]; U = dtw[:, 1, :]; W = dtw[:, 2, :]; Y = dtw[:, 3, :]; T1 = dtw[:, 4, :]
            DT = dtw[:, 5, :]; AA = dtw[:, 6, :]; ACS = dtw[:, 7, :]; EA = dtw[:, 8, :]; DTE = dtw[:, 9, :]
            CD = dtw[:, 10, :]; DTF = dtw[:, 11, :]
            qrf = qr[:].rearrange("p a b -> p (a b)")

            PK = nb()
            if main or halo:
                proj_tok(PK, 256, 512, h)
            for kt in range(0 if pre_done else 8):
                op("pe", lambda e, kt=kt: e.matmul(PK[:, 256:264], lhsT=h[:, kt, :], rhs=win[:, kt, 2304:2312],
                                                   start=(kt == 0), stop=(kt == 7)), reads=[h, win], writes=[PK])
            XB = [None, None] if pre_done else [nb(), nb()]
            for ct in range(0 if pre_done else nct):
                bk = XB[ct // 4]
                for kt in range(8):
                    op("pe", lambda e, kt=kt, ct=ct, bk=bk: e.matmul(
                        bk[:, (ct % 4) * 128:(ct % 4 + 1) * 128], lhsT=win[:, kt, 1280 + ct * 128:1280 + (ct + 1) * 128],
                        rhs=h[:, kt, :], start=(kt == 0), stop=(kt == 7)), reads=[h, win], writes=[bk])
            if main:
                PQ = nb(); PZ = nb()
                proj_tok(PQ, 512, 0, h)
                proj_tok(PZ, 512, 768, h)

            if not pre_done:
                xp = xpre[par]; xpn = xpre[1 - par]
                for hb2 in range(2):
                    n4 = min(4, nct - hb2 * 4)
                    op("act", lambda e, hb2=hb2, n4=n4: e.activation(
                        out=xp[:, hb2 * 4:hb2 * 4 + n4, 3:131],
                        in_=XB[hb2][:, 0:n4 * 128].rearrange("p (a b) -> p a b", b=128), func=AF.Copy, scale=fl),
                       reads=[XB[hb2], flags], writes=[xp])
                op("pool", lambda e: e.tensor_copy(out=xpn[:, :, 0:3], in_=xp[:, :, 128:131]), reads=[xp], writes=[xpn])

                op("dve", lambda e: e.tensor_tensor(out=V, in0=PK[:, 256:264], in1=DTB, op=ALU.add), reads=[PK, small], writes=[dtw])
                op("act", lambda e: e.activation(out=U, in_=V, func=AF.Abs), reads=[dtw], writes=[dtw])
                op("act", lambda e: e.activation(out=U, in_=U, func=AF.Exp, scale=-1.0), reads=[dtw], writes=[dtw])
                op("dve", lambda e: e.tensor_scalar(out=W, in0=U, scalar1=1.0, scalar2=None, op0=ALU.add), reads=[dtw], writes=[dtw])
                op("dve", lambda e: e.tensor_scalar(out=Y, in0=U, scalar1=-0.33, scalar2=0.99, op0=ALU.mult, op1=ALU.add),
                   reads=[dtw], writes=[dtw])
                op("dve", lambda e: e.tensor_tensor(out=Y, in0=Y, in1=U, op=ALU.mult), reads=[dtw], writes=[dtw])
                for _ in range(3):
                    op("act", lambda e: e.activation(out=T1, in_=Y, func=AF.Exp, scale=-1.0), reads=[dtw], writes=[dtw])
                    op("dve", lambda e: e.tensor_tensor(out=T1, in0=T1, in1=W, op=ALU.mult), reads=[dtw], writes=[dtw])
                    op("dve", lambda e: e.scalar_tensor_tensor(out=Y, in0=Y, scalar=-1.0, in1=T1, op0=ALU.add, op1=ALU.add),
                       reads=[dtw], writes=[dtw])
                op("dve", lambda e: e.scalar_tensor_tensor(out=DT, in0=V, scalar=0.0, in1=Y, op0=ALU.max, op1=ALU.add),
                   reads=[dtw], writes=[dtw])
                op("dve", lambda e: e.tensor_tensor(out=AA, in0=DT, in1=ABC, op=ALU.mult), reads=[dtw, small], writes=[dtw])
                op("dve", lambda e: e.tensor_scalar(out=DTF, in0=DT, scalar1=fl, scalar2=None, op0=ALU.mult), reads=[dtw, flags], writes=[dtw])

            if main or halo:
                vg = vaug[par]
                op("act", lambda e: e.activation(out=vg[:, :, 0:64], in_=PK[:, 128:256].rearrange("p (a b) -> p a b", b=64),
                                                 func=AF.Copy), reads=[PK], writes=[vg])
                op("act", lambda e: e.activation(out=sq[:, 8:10, :], in_=PK[:, 0:128].rearrange("p (a b) -> p a b", b=64),
                                                 func=AF.Square), reads=[PK], writes=[sq])
                h0 = 0 if main else 8
                if main:
                    op("act", lambda e: e.activation(out=sq[:, 0:8, :], in_=PQ[:, :].rearrange("p (a b) -> p a b", b=64),
                                                     func=AF.Square), reads=[PQ], writes=[sq])
                op("dve", lambda e: e.tensor_reduce(out=ssq[:, h0:10], in_=sq[:, h0:10, :], axis=AX.X, op=ALU.add),
                   reads=[sq], writes=[ssq])
                rstd_from_ss(ssq[:, h0:10], 64.0, 10 - h0, ssq, [ssq])
                if main:
                    op("dve", lambda e: e.tensor_tensor(
                        out=qn[:, 0:8, :].rearrange("p (i g) d -> p g i d", g=2),
                        in0=PQ[:, :].rearrange("p (g i d) -> p g i d", g=2, i=4),
                        in1=ssq[:, 0:8].rearrange("p (g i) -> p g i", g=2)[:, :, :, None].to_broadcast([128, 2, 4, 64]),
                        op=ALU.mult), reads=[PQ, ssq], writes=[qn])
                op("dve", lambda e: e.tensor_tensor(out=qn[:, 8:10, :], in0=PK[:, 0:128].rearrange("p (a b) -> p a b", b=64),
                                                    in1=ssq[:, 8:10, None].to_broadcast([128, 2, 64]), op=ALU.mult),
                   reads=[PK, ssq], writes=[qn])
                if main:
                    op("pool", lambda e: e.tensor_tensor(out=qn[:, 0:8, :], in0=qn[:, 0:8, :],
                                                         in1=qkw[:, 0, None, :].to_broadcast([128, 8, 64]), op=ALU.mult),
                       reads=[qkw], writes=[qn])
                op("pool", lambda e: e.tensor_tensor(out=qn[:, 8:10, :], in0=qn[:, 8:10, :],
                                                     in1=qkw[:, 1, None, :].to_broadcast([128, 2, 64]), op=ALU.mult),
                   reads=[qkw], writes=[qn])
                jj = s - HALO
                nhh = 10 - h0
                cosb = cossin[:, 0, jj, None, :].to_broadcast([128, nhh, 8])
                sinb = cossin[:, 1, jj, None, :].to_broadcast([128, nhh, 8])
                x1 = qn[:, h0:10, 0:8]; x2 = qn[:, h0:10, 8:16]
                op("pool", lambda e: e.tensor_tensor(out=rt[:, 0, h0:10, :], in0=x1, in1=cosb, op=ALU.mult), reads=[qn, cossin], writes=[rt])
                op("pool", lambda e: e.tensor_tensor(out=rt[:, 1, h0:10, :], in0=x2, in1=sinb, op=ALU.mult), reads=[qn, cossin], writes=[rt])
                op("pool", lambda e: e.tensor_tensor(out=rt[:, 2, h0:10, :], in0=x2, in1=cosb, op=ALU.mult), reads=[qn, cossin], writes=[rt])
                op("pool", lambda e: e.tensor_tensor(out=rt[:, 3, h0:10, :], in0=x1, in1=sinb, op=ALU.mult), reads=[qn, cossin], writes=[rt])
                op("pool", lambda e: e.tensor_tensor(out=qr[:, h0:10, 0:8], in0=rt[:, 0, h0:10, :], in1=rt[:, 1, h0:10, :], op=ALU.subtract),
                   reads=[rt], writes=[qr])
                op("pool", lambda e: e.tensor_tensor(out=qr[:, h0:10, 8:16], in0=rt[:, 2, h0:10, :], in1=rt[:, 3, h0:10, :], op=ALU.add),
                   reads=[rt], writes=[qr])
                op("pool", lambda e: e.tensor_copy(out=qr[:, h0:10, 16:64], in_=qn[:, h0:10, 16:64]), reads=[qn], writes=[qr])
            if main:
                op("act", lambda e: e.activation(out=tz[:], in_=PZ[:, :], func=AF.Tanh), reads=[PZ], writes=[tz])
                op("dve", lambda e: e.scalar_tensor_tensor(out=sz[:], in0=tz[:], scalar=1.0, in1=PZ[:, :], op0=ALU.add, op1=ALU.mult),
                   reads=[tz, PZ], writes=[sz])

            if not pre_done:
                CB = [nb(), nb()]
                for ct in range(nct):
                    bk = CB[ct // 4]
                    o = bk[:, (ct % 4) * 128:(ct % 4 + 1) * 128]
                    for k in range(4):
                        op("pe", lambda e, k=k, ct=ct, o=o: e.matmul(o, lhsT=convdiag[:, k * 8 + ct, :], rhs=xp[:, ct, k:k + 128],
                                                                    start=(k == 0), stop=False), reads=[convdiag, xp], writes=[bk])
                    op("pe", lambda e, ct=ct, o=o: e.matmul(o, lhsT=cbrow[0:1, ct * 128:(ct + 1) * 128], rhs=onesrow[0:1, 0:128],
                                                            start=False, stop=True), reads=[cbrow, onesrow], writes=[bk])
                for hb2 in range(2):
                    n4 = min(4, nct - hb2 * 4)
                    pv = CB[hb2][:, 0:n4 * 128].rearrange("p (a b) -> p a b", b=128)
                    op("act", lambda e, hb2=hb2, n4=n4, pv=pv: e.activation(out=tcv[:, hb2 * 4:hb2 * 4 + n4, :], in_=pv, func=AF.Tanh),
                       reads=[CB[hb2]], writes=[tcv])
                    op("dve", lambda e, hb2=hb2, n4=n4, pv=pv: e.scalar_tensor_tensor(
                        out=xc[:, hb2 * 4:hb2 * 4 + n4, :], in0=tcv[:, hb2 * 4:hb2 * 4 + n4, :], scalar=1.0, in1=pv,
                        op0=ALU.add, op1=ALU.mult), reads=[tcv, CB[hb2]], writes=[xc])

            DB = nb()
            if not pre_done:
                op("pe", lambda e: e.matmul(DB[:, 0:8], lhsT=TRI, rhs=AA, start=True, stop=True), reads=[cf32, dtw], writes=[DB])
                op("pe", lambda e: e.matmul(DB[:, 8:16], lhsT=ONES, rhs=AA, start=True, stop=True), reads=[cf32, dtw], writes=[DB])
            if main:
                for g in range(2):
                    op("pe", lambda e, g=g: e.matmul(DB[:, 128 + g * 128:256 + g * 128], lhsT=xc[:, 4 + g, :], rhs=xc[:, 6 + g, :],
                                                     start=True, stop=True), reads=[xc], writes=[DB])
            if not pre_done:
                op("act", lambda e: e.activation(out=ACS, in_=DB[:, 0:8], func=AF.Copy), reads=[DB], writes=[dtw])
                op("act", lambda e: e.activation(out=CD, in_=DB[:, 8:16], func=AF.Exp), reads=[DB], writes=[dtw])
                op("dve", lambda e: e.tensor_tensor(out=DTE, in0=DB[:, 8:16], in1=ACS, op=ALU.subtract), reads=[DB, dtw], writes=[dtw])
                op("act", lambda e: e.activation(out=DTE, in_=DTE, func=AF.Exp), reads=[dtw], writes=[dtw])
            if main:
                if not pre_done:
                    op("act", lambda e: e.activation(out=EA, in_=ACS, func=AF.Exp), reads=[dtw], writes=[dtw])
                op("dve", lambda e: e.tensor_tensor(out=gtm[:], in0=DB[:, 128:384].rearrange("p (a b) -> p a b", b=128),
                                                    in1=MASKC[:, None, :].to_broadcast([128, 2, 128]), op=ALU.mult),
                   reads=[DB, cbf], writes=[gtm])
            if not pre_done:
                op("dve", lambda e: e.tensor_tensor(out=DTE, in0=DTE, in1=DTF, op=ALU.mult), reads=[dtw], writes=[dtw])

            TB = nb()
            tv = tview(TB)
            for i in range(6):
                op("pe", lambda e, i=i: e.transpose(out=tv[:, i, :], in_=xc[:, i, :], identity=IDENT), reads=[xc, cbf], writes=[TB])
            xps = bfv(TB)[:, 0:512].rearrange("p (a b) -> p a b", b=64)
            op("dve", lambda e: e.tensor_tensor(out=xdte[:], in0=xps, in1=DTE[:, :, None].to_broadcast([128, 8, 64]), op=ALU.mult),
               reads=[TB, dtw], writes=[xdte])
            if main:
                op("dve", lambda e: e.tensor_tensor(out=xdt[:], in0=xps, in1=DTF[:, :, None].to_broadcast([128, 8, 64]), op=ALU.mult),
                   reads=[TB, dtw], writes=[xdt])
                op("act", lambda e: e.activation(out=xstok[:], in_=xps, func=AF.Copy), reads=[TB], writes=[xstok])
            op("act", lambda e: e.activation(out=btok[:], in_=tv[:, 4:6, :], func=AF.Copy), reads=[TB], writes=[btok])
            if main:
                op("act", lambda e: e.activation(out=Rb[:], in_=R[:], func=AF.Copy), reads=[R], writes=[Rb])

            SBK = nb()
            for g in range(2):
                op("pe", lambda e, g=g: e.matmul(SBK[:, g * 256:(g + 1) * 256], lhsT=btok[:, g, :],
                                                 rhs=xdte[:, g * 4:(g + 1) * 4, :], start=True, stop=True),
                   reads=[btok, xdte], writes=[SBK])
            op("dve", lambda e: e.tensor_tensor(out=R[:], in0=R[:], in1=CD[:, :, None].to_broadcast([128, 8, 64]), op=ALU.mult),
               reads=[dtw, Rb], writes=[R])
            op("dve", lambda e: e.tensor_tensor(out=R[:], in0=R[:], in1=SBK[:, :].rearrange("p (a b) -> p a b", b=64), op=ALU.add),
               reads=[SBK], writes=[R])

            if main or halo:
                TQ = nb()
                tq = tview(TQ)
                if main:
                    for i in range(4):
                        op("pe", lambda e, i=i: e.transpose(out=tq[:, i, :], in_=qrf[:, i * 128:(i + 1) * 128], identity=IDENT),
                           reads=[qr, cbf], writes=[TQ])
                op("pe", lambda e: e.transpose(out=tq[:, 4, :], in_=qrf[:, 512:640], identity=IDENT), reads=[qr, cbf], writes=[TQ])
                if main:
                    op("act", lambda e: e.activation(out=qT[:], in_=tq[:, 0:4, :], func=AF.Copy), reads=[TQ], writes=[qT])
                op("act", lambda e: e.activation(out=kT[par][:], in_=tq[:, 4, :], func=AF.Copy), reads=[TQ], writes=[kT[par]])
            if not main:
                return

            kprev = kT[1 - par]; vprev = vaug[1 - par]
            kcur = kT[par]; vcur = vaug[par]
            sbanks = [nb(), nb(), nb(), nb()]
            for g in range(2):
                for bi, kt_ in enumerate((kprev, kcur)):
                    bk = sbanks[g * 2 + bi]
                    op("pe", lambda e, g=g, kt_=kt_, bk=bk: e.matmul(
                        bk[:, :], lhsT=kt_[g * 64:(g + 1) * 64, :], rhs=qT[g * 64:(g + 1) * 64, :, :], start=True, stop=False),
                       reads=[kt_, qT], writes=[bk])
                    mi = (2 if j == 0 else 0) if bi == 0 else 1
                    op("pe", lambda e, bk=bk, mi=mi: e.matmul(bk[:, :], lhsT=IDENT, rhs=negm[:, mi, :, :], start=False, stop=True),
                       reads=[cbf, negm], writes=[bk])

            SEG = [nb(), nb()]
            for hf in range(2):
                op("pool", lambda e, hf=hf: e.tensor_tensor(out=Lm[:], in0=SLM[:, None, :].to_broadcast([128, 4, 128]),
                                                            in1=AA[:, hf * 4:(hf + 1) * 4, None].to_broadcast([128, 4, 128]), op=ALU.mult),
                   reads=[cf32, dtw], writes=[Lm])
                bk = SEG[hf]
                for h4 in range(4):
                    op("pe", lambda e, h4=h4, bk=bk: e.matmul(bk[:, h4 * 128:(h4 + 1) * 128], lhsT=Lm[:, h4, :], rhs=TRI,
                                                              start=True, stop=True), reads=[Lm, cf32], writes=[bk])
            for g in range(2):
                for bi in range(2):
                    bk = sbanks[g * 2 + bi]
                    idx = g * 2 + bi
                    op("act", lambda e, bk=bk, idx=idx: e.activation(out=pT[:, idx, :], in_=bk[:, :], func=AF.Exp, scale=0.125,
                                                                     bias=negc[:]), reads=[bk, negc], writes=[pT])
            obanks = [nb(), nb()]
            for g in range(2):
                ob = obanks[g]
                for i in range(4):
                    for bi, vt in enumerate((vprev, vcur)):
                        idx = g * 2 + bi
                        op("pe", lambda e, g=g, i=i, bi=bi, vt=vt, idx=idx, ob=ob: e.matmul(
                            ob[:, i * 65:(i + 1) * 65], lhsT=pT[:, idx, i * 128:(i + 1) * 128], rhs=vt[:, g, :],
                            start=(bi == 0), stop=(bi == 1)), reads=[pT, vt], writes=[ob])
                ov = ob[:, 0:260].rearrange("p (a b) -> p a b", b=65)
                op("dve", lambda e, g=g, ov=ov: e.tensor_tensor(out=den[:, g * 4:(g + 1) * 4], in0=ov[:, :, 64],
                                                               in1=ESINK[:, g * 4:(g + 1) * 4], op=ALU.add),
                   reads=[ob, small], writes=[den])
                op("dve", lambda e, g=g: e.reciprocal(out=den[:, g * 4:(g + 1) * 4], in_=den[:, g * 4:(g + 1) * 4]),
                   reads=[den], writes=[den])
                op("dve", lambda e, g=g, ov=ov: e.tensor_tensor(
                    out=mix[:, g * 256:(g + 1) * 256].rearrange("p (a b) -> p a b", b=64), in0=ov[:, :, 0:64],
                    in1=den[:, g * 4:(g + 1) * 4, None].to_broadcast([128, 4, 64]), op=ALU.mult), reads=[ob, den], writes=[mix])
            for hb2 in range(2):
                op("act", lambda e, hb2=hb2: e.activation(out=decT[:, hb2 * 4:(hb2 + 1) * 4, :],
                                                          in_=SEG[hb2][:, :].rearrange("p (a b) -> p a b", b=128), func=AF.Exp),
                   reads=[SEG[hb2]], writes=[decT])
            for g in range(2):
                op("dve", lambda e, g=g: e.tensor_tensor(out=decT[:, g * 4:(g + 1) * 4, :], in0=decT[:, g * 4:(g + 1) * 4, :],
                                                          in1=gtm[:, g, None, :].to_broadcast([128, 4, 128]), op=ALU.mult),
                   reads=[gtm], writes=[decT])
            YB = nb(); YOFF = nb()
            for hh in range(8):
                o = YB[:, hh * 64:(hh + 1) * 64]
                op("pe", lambda e, hh=hh, o=o: e.matmul(o, lhsT=decT[:, hh, :], rhs=xdt[:, hh, :], start=True, stop=False),
                   reads=[decT, xdt], writes=[YB])
                op("pe", lambda e, hh=hh, o=o: e.matmul(o, lhsT=dI[:, hh, :], rhs=xstok[:, hh, :], start=False, stop=True),
                   reads=[dI, xstok], writes=[YB])
            for g in range(2):
                op("pe", lambda e, g=g: e.matmul(YOFF[:, g * 256:(g + 1) * 256], lhsT=xc[:, 6 + g, :], rhs=Rb[:, g * 4:(g + 1) * 4, :],
                                                 start=True, stop=True), reads=[xc, Rb], writes=[YOFF])
            op("dve", lambda e: e.tensor_tensor(out=yo[:].rearrange("p (a b) -> p a b", b=64),
                                                in0=YOFF[:, :].rearrange("p (a b) -> p a b", b=64),
                                                in1=EA[:, :, None].to_broadcast([128, 8, 64]), op=ALU.mult),
               reads=[YOFF, dtw], writes=[yo])
            yy = yo
            op("dve", lambda e: e.tensor_tensor(out=yy[:], in0=YB[:, :], in1=yo[:], op=ALU.add), reads=[YB], writes=[yy])
            op("dve", lambda e: e.tensor_tensor(out=yy[:], in0=yy[:], in1=sz[:], op=ALU.mult), reads=[sz], writes=[yy])
            for g in range(2):
                op("act", lambda e, g=g: e.activation(out=junk[:, g * 256:(g + 1) * 256], in_=yy[:, g * 256:(g + 1) * 256],
                                                      func=AF.Square, accum_out=ssg[:, g:g + 1]), reads=[yy], writes=[junk, ssg])
            rstd_from_ss(ssg[:, 0:2], 256.0, 2, ssg, [ssg])
            for g in range(2):
                op("dve", lambda e, g=g: e.scalar_tensor_tensor(out=mix[:, 512 + g * 256:768 + g * 256], in0=yy[:, g * 256:(g + 1) * 256],
                                                                scalar=ssg[:, g:g + 1], in1=snw[:, g * 256:(g + 1) * 256],
                                                                op0=ALU.mult, op1=ALU.mult), reads=[yy, ssg, snw], writes=[mix])

            TM = nb()
            tm = tview(TM)
            for kt in range(8):
                op("pe", lambda e, kt=kt: e.transpose(out=tm[:, kt, :], in_=mix[:, kt::8], identity=IDENT), reads=[mix, cbf], writes=[TM])
            op("act", lambda e: e.activation(out=mixT[:], in_=tm, func=AF.Copy), reads=[TM], writes=[mixT])
            for half in range(2):
                bk = nb()
                for kt in range(8):
                    op("pe", lambda e, kt=kt, half=half, bk=bk: e.matmul(bk[:, :], lhsT=mixT[:, kt, :],
                                                                        rhs=wout[:, kt, half * 512:(half + 1) * 512],
                                                                        start=(kt == 0), stop=(kt == 7)), reads=[mixT, wout], writes=[bk])
                op("dve", lambda e, half=half, bk=bk: e.tensor_tensor(out=xres_t[:, j, half * 512:(half + 1) * 512],
                                                                      in0=bk[:, :], in1=xres_t[:, j, half * 512:(half + 1) * 512],
                                                                      op=ALU.add), reads=[bk], writes=[xres_b[j]])
            if DEBUG_X2:
                op("sp", lambda e: e.dma_start(out=dbg_d[j * 128:(j + 1) * 128, :], in_=xres_t[:, j, :]), reads=[xres_b[j]], dma=True)

        def prefix_pair(p):
            s0 = 2 * p
            pp = p % 2
            h2 = hT2[pp]
            fl = flags[:, s0:s0 + 1]
            xp = xpre2[pp]
            last = (p == NPAIR - 1)
            xpn = xpre2[0] if last else xpre2[1 - pp]
            XCB = [xc_[0].b, xc_[1].b]; DTB_ = [dtw_[0].b, dtw_[1].b]
            XDB = [xdte_[0].b, xdte_[1].b]; BTB = [btok_[0].b, btok_[1].b]
            D2 = lambda r: dtw2[:, r, :]
            V, U, W, Y, T1, DT, AA, ACS, DTE, CD, DTF = (D2(0), D2(1), D2(2), D2(3), D2(4), D2(5), D2(6), D2(7), D2(9), D2(10), D2(11))
            PK = nb()
            for hf in range(2):
                for kt in range(8):
                    op("pe", lambda e, kt=kt, hf=hf: e.matmul(PK[:, hf * 8:(hf + 1) * 8], lhsT=h2[:, kt, hf * 128:(hf + 1) * 128],
                                                              rhs=win[:, kt, 2304:2312], start=(kt == 0), stop=(kt == 7)),
                       reads=[h2, win], writes=[PK])
            XB = [nb(), nb(), nb()]
            for ct in range(6):
                bk = XB[ct // 2]
                for kt in range(8):
                    op("pe", lambda e, kt=kt, ct=ct, bk=bk: e.matmul(
                        bk[:, (ct % 2) * 256:(ct % 2 + 1) * 256], lhsT=win[:, kt, 1280 + ct * 128:1280 + (ct + 1) * 128],
                        rhs=h2[:, kt, :], start=(kt == 0), stop=(kt == 7)), reads=[h2, win], writes=[bk])
            for b3 in range(3):
                op("act", lambda e, b3=b3: e.activation(out=xp[:, 2 * b3:2 * b3 + 2, 3:259],
                                                        in_=XB[b3][:, :].rearrange("p (a b) -> p a b", b=256), func=AF.Copy, scale=fl),
                   reads=[XB[b3], flags], writes=[xp])
            op("pool", lambda e: e.tensor_copy(out=xpn[:, 0:6, 0:3], in_=xp[:, 0:6, 256:259]), reads=[xp], writes=[xpn])
            op("dve", lambda e: e.tensor_tensor(out=V.rearrange("p (a b) -> p a b", b=8), in0=PK[:, 0:16].rearrange("p (a b) -> p a b", b=8),
                                                in1=DTB[:, None, :].to_broadcast([128, 2, 8]), op=ALU.add), reads=[PK, small], writes=DTB_)
            op("act", lambda e: e.activation(out=U, in_=V, func=AF.Abs), reads=DTB_, writes=DTB_)
            op("act", lambda e: e.activation(out=U, in_=U, func=AF.Exp, scale=-1.0), reads=DTB_, writes=DTB_)
            op("dve", lambda e: e.tensor_scalar(out=W, in0=U, scalar1=1.0, scalar2=None, op0=ALU.add), reads=DTB_, writes=DTB_)
            op("dve", lambda e: e.tensor_scalar(out=Y, in0=U, scalar1=-0.33, scalar2=0.99, op0=ALU.mult, op1=ALU.add), reads=DTB_, writes=DTB_)
            op("dve", lambda e: e.tensor_tensor(out=Y, in0=Y, in1=U, op=ALU.mult), reads=DTB_, writes=DTB_)
            for _ in range(3):
                op("act", lambda e: e.activation(out=T1, in_=Y, func=AF.Exp, scale=-1.0), reads=DTB_, writes=DTB_)
                op("dve", lambda e: e.tensor_tensor(out=T1, in0=T1, in1=W, op=ALU.mult), reads=DTB_, writes=DTB_)
                op("dve", lambda e: e.scalar_tensor_tensor(out=Y, in0=Y, scalar=-1.0, in1=T1, op0=ALU.add, op1=ALU.add), reads=DTB_, writes=DTB_)
            op("dve", lambda e: e.scalar_tensor_tensor(out=DT, in0=V, scalar=0.0, in1=Y, op0=ALU.max, op1=ALU.add), reads=DTB_, writes=DTB_)
            op("dve", lambda e: e.tensor_tensor(out=AA.rearrange("p (a b) -> p a b", b=8), in0=DT.rearrange("p (a b) -> p a b", b=8),
                                                in1=ABC[:, None, :].to_broadcast([128, 2, 8]), op=ALU.mult), reads=DTB_ + [small.b], writes=DTB_)
            op("dve", lambda e: e.tensor_scalar(out=DTF, in0=DT, scalar1=fl, scalar2=None, op0=ALU.mult), reads=DTB_ + [flags.b], writes=DTB_)
            CB = [nb(), nb(), nb()]
            for ct in range(6):
                bk = CB[ct // 2]
                o = bk[:, (ct % 2) * 256:(ct % 2 + 1) * 256]
                for k in range(4):
                    op("pe", lambda e, k=k, ct=ct, o=o: e.matmul(o, lhsT=convdiag[:, k * 8 + ct, :], rhs=xp[:, ct, k:k + 256],
                                                                start=(k == 0), stop=False), reads=[convdiag, xp], writes=[bk])
                op("pe", lambda e, ct=ct, o=o: e.matmul(o, lhsT=cbrow[0:1, ct * 128:(ct + 1) * 128], rhs=onesrow[0:1, :],
                                                        start=False, stop=True), reads=[cbrow, onesrow], writes=[bk])
            for b3 in range(3):
                pv = CB[b3][:, :].rearrange("p (a b) -> p a b", b=256)
                op("act", lambda e, b3=b3, pv=pv: e.activation(out=tcv2[:, 2 * b3:2 * b3 + 2, :], in_=pv, func=AF.Tanh),
                   reads=[CB[b3]], writes=[tcv2])
                op("dve", lambda e, b3=b3, pv=pv: e.scalar_tensor_tensor(out=xc2[:, 2 * b3:2 * b3 + 2, :], in0=tcv2[:, 2 * b3:2 * b3 + 2, :],
                                                                         scalar=1.0, in1=pv, op0=ALU.add, op1=ALU.mult),
                   reads=[tcv2, CB[b3]], writes=XCB)
            DB = nb()
            op("pe", lambda e: e.matmul(DB[:, 0:16], lhsT=TRI, rhs=AA, start=True, stop=True), reads=[cf32] + DTB_, writes=[DB])
            op("pe", lambda e: e.matmul(DB[:, 16:32], lhsT=ONES, rhs=AA, start=True, stop=True), reads=[cf32] + DTB_, writes=[DB])
            op("act", lambda e: e.activation(out=ACS, in_=DB[:, 0:16], func=AF.Copy), reads=[DB], writes=DTB_)
            op("act", lambda e: e.activation(out=CD, in_=DB[:, 16:32], func=AF.Exp), reads=[DB], writes=DTB_)
            op("dve", lambda e: e.tensor_tensor(out=DTE, in0=DB[:, 16:32], in1=ACS, op=ALU.subtract), reads=[DB] + DTB_, writes=DTB_)
            op("act", lambda e: e.activation(out=DTE, in_=DTE, func=AF.Exp), reads=DTB_, writes=DTB_)
            op("dve", lambda e: e.tensor_tensor(out=DTE, in0=DTE, in1=DTF, op=ALU.mult), reads=DTB_, writes=DTB_)
            op("dve", lambda e: e.tensor_tensor(out=DTE[:, 0:8], in0=DTE[:, 0:8], in1=CD[:, 8:16], op=ALU.mult), reads=DTB_, writes=DTB_)
            op("dve", lambda e: e.tensor_tensor(out=CD[:, 0:8], in0=CD[:, 0:8], in1=CD[:, 8:16], op=ALU.mult), reads=DTB_, writes=DTB_)
            for hf in range(2):
                TB = nb()
                tv = tview(TB)
                for i in range(6):
                    op("pe", lambda e, i=i, hf=hf, tv=tv: e.transpose(out=tv[:, i, :], in_=xc2[:, i, hf * 128:(hf + 1) * 128], identity=IDENT),
                       reads=XCB + [cbf.b], writes=[TB])
                xps = bfv(TB)[:, 0:512].rearrange("p (a b) -> p a b", b=64)
                op("dve", lambda e, hf=hf, xps=xps: e.tensor_tensor(out=xdte2[:, hf, :, :], in0=xps,
                                                                   in1=DTE[:, hf * 8:(hf + 1) * 8, None].to_broadcast([128, 8, 64]), op=ALU.mult),
                   reads=[TB] + DTB_, writes=XDB)
                op("act", lambda e, hf=hf, tv=tv: e.activation(out=btok2[:, hf, :, :], in_=tv[:, 4:6, :], func=AF.Copy), reads=[TB], writes=BTB)
            SBK = nb()
            for g in range(2):
                for hf in range(2):
                    op("pe", lambda e, g=g, hf=hf: e.matmul(SBK[:, g * 256:(g + 1) * 256], lhsT=btok2[:, hf, g, :],
                                                            rhs=xdte2[:, hf, g * 4:(g + 1) * 4, :], start=(hf == 0), stop=(hf == 1)),
                       reads=BTB + XDB, writes=[SBK])
            op("dve", lambda e: e.tensor_tensor(out=R[:], in0=R[:], in1=CD[:, 0:8, None].to_broadcast([128, 8, 64]), op=ALU.mult),
               reads=DTB_, writes=[R])
            op("dve", lambda e: e.tensor_tensor(out=R[:], in0=R[:], in1=SBK[:, :].rearrange("p (a b) -> p a b", b=64), op=ALU.add),
               reads=[SBK], writes=[R])

        def main_pair_pre(pm):
            s0 = NPRE + 2 * pm
            pp = pm % 2
            h2 = hT2[pp]
            fl = flags[:, s0:s0 + 1]
            xp = xpre2[pp]
            xpn = xpre2[1 - pp]
            XCB = [xc_[0].b, xc_[1].b]; DTB_ = [dtw_[0].b, dtw_[1].b]
            tcvb = [Buf("tcvs%d" % i) for i in range(6)]
            for tb_ in tcvb:
                tb_.w = list(tcv2.b.w); tb_.r = list(tcv2.b.r)
            xcsl = [Buf("xcsl%d" % i) for i in range(8)]
            for xb_ in xcsl:
                xb_.w = list(xc_[0].b.w) + list(xc_[1].b.w); xb_.r = list(xc_[0].b.r) + list(xc_[1].b.r)
            D2 = lambda r: dtw2[:, r, :]
            V, U, W, Y, T1, DT, AA, ACS, EA, DTE, CD, DTF = (D2(0), D2(1), D2(2), D2(3), D2(4), D2(5), D2(6), D2(7), D2(8), D2(9), D2(10), D2(11))
            PK = nb()
            for hf in range(2):
                for kt in range(8):
                    op("pe", lambda e, kt=kt, hf=hf: e.matmul(PK[:, hf * 8:(hf + 1) * 8], lhsT=h2[:, kt, hf * 128:(hf + 1) * 128],
                                                              rhs=win[:, kt, 2304:2312], start=(kt == 0), stop=(kt == 7)),
                       reads=[h2, win], writes=[PK])
            XB = [nb(), nb(), nb(), nb()]
            for ct in range(8):
                bk = XB[ct // 2]
                for kt in range(8):
                    op("pe", lambda e, kt=kt, ct=ct, bk=bk: e.matmul(
                        bk[:, (ct % 2) * 256:(ct % 2 + 1) * 256], lhsT=win[:, kt, 1280 + ct * 128:1280 + (ct + 1) * 128],
                        rhs=h2[:, kt, :], start=(kt == 0), stop=(kt == 7)), reads=[h2, win], writes=[bk])
            for b3 in range(4):
                op("act", lambda e, b3=b3: e.activation(out=xp[:, 2 * b3:2 * b3 + 2, 3:259],
                                                        in_=XB[b3][:, :].rearrange("p (a b) -> p a b", b=256), func=AF.Copy, scale=fl),
                   reads=[XB[b3], flags], writes=[xp])
            op("pool", lambda e: e.tensor_copy(out=xpn[:, :, 0:3], in_=xp[:, :, 256:259]), reads=[xp], writes=[xpn])
            op("dve", lambda e: e.tensor_tensor(out=V.rearrange("p (a b) -> p a b", b=8), in0=PK[:, 0:16].rearrange("p (a b) -> p a b", b=8),
                                                in1=DTB[:, None, :].to_broadcast([128, 2, 8]), op=ALU.add), reads=[PK, small], writes=DTB_)
            op("act", lambda e: e.activation(out=U, in_=V, func=AF.Abs), reads=DTB_, writes=DTB_)
            op("act", lambda e: e.activation(out=U, in_=U, func=AF.Exp, scale=-1.0), reads=DTB_, writes=DTB_)
            op("dve", lambda e: e.tensor_scalar(out=W, in0=U, scalar1=1.0, scalar2=None, op0=ALU.add), reads=DTB_, writes=DTB_)
            op("dve", lambda e: e.tensor_scalar(out=Y, in0=U, scalar1=-0.33, scalar2=0.99, op0=ALU.mult, op1=ALU.add), reads=DTB_, writes=DTB_)
            op("dve", lambda e: e.tensor_tensor(out=Y, in0=Y, in1=U, op=ALU.mult), reads=DTB_, writes=DTB_)
            for _ in range(3):
                op("act", lambda e: e.activation(out=T1, in_=Y, func=AF.Exp, scale=-1.0), reads=DTB_, writes=DTB_)
                op("dve", lambda e: e.tensor_tensor(out=T1, in0=T1, in1=W, op=ALU.mult), reads=DTB_, writes=DTB_)
                op("dve", lambda e: e.scalar_tensor_tensor(out=Y, in0=Y, scalar=-1.0, in1=T1, op0=ALU.add, op1=ALU.add), reads=DTB_, writes=DTB_)
            op("dve", lambda e: e.scalar_tensor_tensor(out=DT, in0=V, scalar=0.0, in1=Y, op0=ALU.max, op1=ALU.add), reads=DTB_, writes=DTB_)
            op("dve", lambda e: e.tensor_tensor(out=AA.rearrange("p (a b) -> p a b", b=8), in0=DT.rearrange("p (a b) -> p a b", b=8),
                                                in1=ABC[:, None, :].to_broadcast([128, 2, 8]), op=ALU.mult), reads=DTB_ + [small.b], writes=DTB_)
            op("dve", lambda e: e.tensor_scalar(out=DTF, in0=DT, scalar1=fl, scalar2=None, op0=ALU.mult), reads=DTB_ + [flags.b], writes=DTB_)
            CB = [nb(), nb(), nb(), nb()]
            for ct in range(8):
                bk = CB[ct // 2]
                o = bk[:, (ct % 2) * 256:(ct % 2 + 1) * 256]
                for k in range(4):
                    op("pe", lambda e, k=k, ct=ct, o=o: e.matmul(o, lhsT=convdiag[:, k * 8 + ct, :], rhs=xp[:, ct, k:k + 256],
                                                                start=(k == 0), stop=False), reads=[convdiag, xp], writes=[bk])
                op("pe", lambda e, ct=ct, o=o: e.matmul(o, lhsT=cbrow[0:1, ct * 128:(ct + 1) * 128], rhs=onesrow[0:1, :],
                                                        start=False, stop=True), reads=[cbrow, onesrow], writes=[bk])
            for b3 in range(4):
                pv = CB[b3][:, :].rearrange("p (a b) -> p a b", b=256)
                for q2 in range(2):
                    pvh = pv[:, q2:q2 + 1, :]
                    sl = (2 * b3 + q2) % 6
                    op("act", lambda e, pvh=pvh, sl=sl: e.activation(out=tcv2[:, sl:sl + 1, :], in_=pvh, func=AF.Tanh),
                       reads=[CB[b3]], writes=[tcvb[sl]])
                    op("dve", lambda e, b3=b3, q2=q2, pvh=pvh, sl=sl: e.scalar_tensor_tensor(
                        out=xc2[:, 2 * b3 + q2:2 * b3 + q2 + 1, :], in0=tcv2[:, sl:sl + 1, :], scalar=1.0, in1=pvh, op0=ALU.add, op1=ALU.mult),
                       reads=[tcvb[sl], CB[b3]], writes=[xcsl[2 * b3 + q2]])
            allw = sorted(set(i for xb_ in xcsl for i in xb_.w))
            for hf_ in range(2):
                xc_[hf_].b.w = list(allw); xc_[hf_].b.r = []
            tcv2.b.w = sorted(set(i for tb_ in tcvb for i in tb_.w))
            tcv2.b.r = sorted(set(i for tb_ in tcvb for i in tb_.r))
            DB = nb()
            op("pe", lambda e: e.matmul(DB[:, 0:16], lhsT=TRI, rhs=AA, start=True, stop=True), reads=[cf32] + DTB_, writes=[DB])
            op("pe", lambda e: e.matmul(DB[:, 16:32], lhsT=ONES, rhs=AA, start=True, stop=True), reads=[cf32] + DTB_, writes=[DB])
            op("act", lambda e: e.activation(out=ACS, in_=DB[:, 0:16], func=AF.Copy), reads=[DB], writes=DTB_)
            op("act", lambda e: e.activation(out=CD, in_=DB[:, 16:32], func=AF.Exp), reads=[DB], writes=DTB_)
            op("dve", lambda e: e.tensor_tensor(out=DTE, in0=DB[:, 16:32], in1=ACS, op=ALU.subtract), reads=[DB] + DTB_, writes=DTB_)
            op("act", lambda e: e.activation(out=DTE, in_=DTE, func=AF.Exp), reads=DTB_, writes=DTB_)
            op("act", lambda e: e.activation(out=EA, in_=ACS, func=AF.Exp), reads=DTB_, writes=DTB_)
            op("dve", lambda e: e.tensor_tensor(out=DTE, in0=DTE, in1=DTF, op=ALU.mult), reads=DTB_, writes=DTB_)

        if STOP_AFTER != 1 and MAIN_PAIRS:
            frontend(0); frontend(1)
            for p in range(NPAIR):
                frontend(2 * p + 2)
                if p + 1 < NPAIR:
                    frontend(2 * p + 3)
                prefix_pair(p)
            frontend(2 * NPAIR + 1)
            backend2(2 * NPAIR)
            frontend(NPRE); frontend(NPRE + 1)
            backend2(2 * NPAIR + 1)
            for pm in range(NMAIN // 2):
                if pm + 1 < NMAIN // 2:
                    frontend(NPRE + 2 * pm + 2); frontend(NPRE + 2 * pm + 3)
                main_pair_pre(pm)
                backend2(NPRE + 2 * pm, pre_done=True)
                backend2(NPRE + 2 * pm + 1, pre_done=True)
        elif STOP_AFTER != 1:
            frontend(0); frontend(1)
            for p in range(NPAIR):
                frontend(2 * p + 2)
                if p + 1 < NPAIR:
                    frontend(2 * p + 3)
                prefix_pair(p)
            for s in range(2 * NPAIR, NSLOT):
                if s + 1 < NSLOT:
                    frontend(s + 1)
                backend2(s)

        S.barrier(lambda e: e.memset(neghalf[:, 16:32], -0.5))
        p1.close()

        p2 = root.enter_context(contextlib.ExitStack())
        mod2 = sb(p2, "mod2", [128, 3 * D])
        SHIFT2 = mod2[:, 0:1024]; G2 = mod2[:, 1024:2048]; GATE2 = mod2[:, 2048:3072]
        op("sp", lambda e: e.dma_start(out=mod2[:], in_=modscr), reads=[modscr_b], writes=[mod2], dma=True)
        h2T = sb(p2, "h2T", [128, 8, NMAIN * 128], BF16)
        comb = sb(p2, "comb", [128, NMAIN, 32])
        wr = sb(p2, "wr", [128, 8, 36], BF16)
        brbc = sb(p2, "brbc", [128, 36])
        wg = [sb(p2, "wg%d" % i, [128, 8, 512], BF16) for i in range(2)]
        wd = [sb(p2, "wd%d" % i, [128, 2, D], BF16) for i in range(2)]
        sg = [sb(p2, "sg%d" % i, [128, 2, 512], BF16) for i in range(2)]
        actT = [sb(p2, "actT%d" % i, [128, 2, 512], BF16) for i in range(2)]
        junk2 = sb(p2, "junk2", [128, D], BF16)
        ss2 = [sb(p2, "ss2_%d" % i, [128, 4]) for i in range(2)]
        tmp2 = [sb(p2, "tmp2_%d" % i, [128, D]) for i in range(2)]
        hb2t = [sb(p2, "hb2_%d" % i, [128, D], BF16) for i in range(2)]
        lgA = sb(p2, "lgA", [128, NMAIN, 36])
        rwa = sb(p2, "rwa", [128, 416])
        rwb = [sb(p2, "rwb%d" % i, [128, NMAIN, 32]) for i in range(2)]

        op("pool", lambda e: e.dma_start(out=wr[:, :, 0:4], in_=w_group.rearrange("(p k) n -> p k n", k=8)), writes=[wr], dma=True)
        op("pool", lambda e: e.dma_start(out=wr[:, :, 4:36], in_=w_expert.rearrange("(p k) n -> p k n", k=8)), writes=[wr], dma=True)
        op("sp", lambda e: e.dma_start(out=brbc[:, 0:4], in_=b_group.partition_broadcast(128)), writes=[brbc], dma=True)
        op("sp", lambda e: e.dma_start(out=brbc[:, 4:36], in_=b_expert.partition_broadcast(128)), writes=[brbc], dma=True)

        def load_expert(ei):
            slot = ei % 2
            op("pool", lambda e: e.dma_start(out=wg[slot][:, :, 0:256], in_=w_gate[ei].rearrange("(p k) f -> p k f", k=8)),
               writes=[wg[slot]], dma=True)
            op("pool", lambda e: e.dma_start(out=wg[slot][:, :, 256:512], in_=w_up[ei].rearrange("(p k) f -> p k f", k=8)),
               writes=[wg[slot]], dma=True)
            op("pool", lambda e: e.dma_start(out=wd[slot][:], in_=w_down[ei].rearrange("(j t) d -> j t d", t=2)),
               writes=[wd[slot]], dma=True)
            for t in range(2):
                op("pool", lambda e, t=t: e.tensor_tensor(out=wd[slot][:, t, :], in0=wd[slot][:, t, :], in1=GATE2, op=ALU.mult),
                   reads=[mod2], writes=[wd[slot]])

        if STOP_AFTER == 0:
            load_expert(0)
        BT = B[0]
        tv = bfv(BT).rearrange("p (a b) -> p a b", b=128)
        for j in range(NMAIN if STOP_AFTER == 0 else 0):
            par = j % 2
            xt = xres_t[:, j, :]; xb = xres_b[j]
            sst = ss2[par]
            op("act", lambda e, xt=xt, sst=sst: e.activation(out=junk2[:], in_=xt, func=AF.Square, accum_out=sst[:, 0:1]),
               reads=[xb], writes=[junk2, sst])
            rstd_from_ss(sst[:, 0:1], float(D), 1, sst, [sst])
            tf = tmp2[par]
            op("dve", lambda e, xt=xt, sst=sst, tf=tf: e.scalar_tensor_tensor(out=tf[:], in0=xt, scalar=sst[:, 0:1], in1=G2,
                                                                              op0=ALU.mult, op1=ALU.mult),
               reads=[xb, sst, mod2], writes=[tf])
            hbt = hb2t[par]
            op("pool", lambda e, tf=tf, hbt=hbt: e.tensor_tensor(out=hbt[:], in0=tf[:], in1=SHIFT2, op=ALU.add),
               reads=[tf, mod2], writes=[hbt])
            for kt in range(8):
                op("pe", lambda e, kt=kt, hbt=hbt: e.transpose(out=tv[:, kt, :], in_=hbt[:, kt::8], identity=IDENT),
                   reads=[hbt, cbf], writes=[BT])
            op("act", lambda e, j=j: e.activation(out=h2T[:, :, j * 128:(j + 1) * 128], in_=tv, func=AF.Copy), reads=[BT], writes=[h2T])
            for kt in range(8):
                op("pe", lambda e, kt=kt, j=j: e.matmul(B[1][:, 0:36], lhsT=h2T[:, kt, j * 128:(j + 1) * 128], rhs=wr[:, kt, :],
                                                        start=(kt == 0), stop=(kt == 7)), reads=[h2T, wr], writes=[B[1]])
            op("dve", lambda e, j=j: e.tensor_tensor(out=lgA[:, j, :], in0=B[1][:, 0:36], in1=brbc[:], op=ALU.add),
               reads=[B[1], brbc], writes=[lgA])

        if STOP_AFTER == 0:
            GL = lgA[:, :, 0:4]
            EL = lgA[:, :, 4:36]
            EL4 = EL.rearrange("p c (a b) -> p c a b", b=8)
            g1 = rwa[:, 0:16]; gs = rwa[:, 16:32]
            gd = rwa[:, 32:96].rearrange("p (c a) -> p c a", a=4)
            oh = rwa[:, 96:160].rearrange("p (c a) -> p c a", a=4)
            gw = rwa[:, 160:224].rearrange("p (c a) -> p c a", a=4)
            m1 = rwa[:, 224:288].rearrange("p (c a) -> p c a", a=4)
            m2 = rwa[:, 288:352].rearrange("p (c a) -> p c a", a=4)
            dn = rwa[:, 352:416].rearrange("p (c a) -> p c a", a=4)
            b4 = lambda t: t[:, :, :, None].to_broadcast([128, NMAIN, 4, 8])
            v4 = lambda t: t[:].rearrange("p c (a b) -> p c a b", b=8)
            RWA = [rwa, rwb[0], rwb[1]]
            op("dve", lambda e: e.tensor_reduce(out=g1, in_=GL, axis=AX.X, op=ALU.max), reads=[lgA], writes=RWA)
            op("dve", lambda e: e.tensor_tensor(out=gd, in0=GL, in1=g1[:, :, None].to_broadcast([128, NMAIN, 4]), op=ALU.subtract),
               reads=[lgA] + RWA, writes=RWA)
            op("act", lambda e: e.activation(out=gd, in_=gd, func=AF.Exp), reads=RWA, writes=RWA)
            op("dve", lambda e: e.tensor_reduce(out=gs, in_=gd, axis=AX.X, op=ALU.add), reads=RWA, writes=RWA)
            op("dve", lambda e: e.reciprocal(out=gs, in_=gs), reads=RWA, writes=RWA)
            op("dve", lambda e: e.tensor_tensor(out=oh, in0=GL, in1=g1[:, :, None].to_broadcast([128, NMAIN, 4]), op=ALU.is_equal),
               reads=[lgA] + RWA, writes=RWA)
            op("dve", lambda e: e.tensor_tensor(out=gw, in0=oh, in1=gs[:, :, None].to_broadcast([128, NMAIN, 4]), op=ALU.mult),
               reads=RWA, writes=RWA)
            op("dve", lambda e: e.tensor_reduce(out=m1, in_=EL4, axis=AX.X, op=ALU.max), reads=[lgA], writes=RWA)
            op("dve", lambda e: e.tensor_tensor(out=v4(rwb[0]), in0=EL4, in1=b4(m1), op=ALU.is_equal), reads=[lgA] + RWA, writes=RWA)
            op("dve", lambda e: e.scalar_tensor_tensor(out=rwb[1][:], in0=rwb[0][:], scalar=-1e30, in1=EL, op0=ALU.mult, op1=ALU.add),
               reads=[lgA] + RWA, writes=RWA)
            op("dve", lambda e: e.tensor_reduce(out=m2, in_=v4(rwb[1]), axis=AX.X, op=ALU.max), reads=RWA, writes=RWA)
            op("dve", lambda e: e.tensor_tensor(out=v4(rwb[0]), in0=EL4, in1=b4(m2), op=ALU.is_ge), reads=[lgA] + RWA, writes=RWA)
            op("dve", lambda e: e.tensor_tensor(out=v4(rwb[1]), in0=EL4, in1=b4(m1), op=ALU.subtract), reads=[lgA] + RWA, writes=RWA)
            op("act", lambda e: e.activation(out=rwb[1][:], in_=rwb[1][:], func=AF.Exp), reads=RWA, writes=RWA)
            op("dve", lambda e: e.tensor_tensor(out=rwb[1][:], in0=rwb[1][:], in1=rwb[0][:], op=ALU.mult), reads=RWA, writes=RWA)
            op("dve", lambda e: e.tensor_reduce(out=dn, in_=v4(rwb[1]), axis=AX.X, op=ALU.add), reads=RWA, writes=RWA)
            op("dve", lambda e: e.reciprocal(out=dn, in_=dn), reads=RWA, writes=RWA)
            op("dve", lambda e: e.tensor_tensor(out=dn, in0=dn, in1=gw, op=ALU.mult), reads=RWA, writes=RWA)
            op("dve", lambda e: e.tensor_tensor(out=v4(comb), in0=v4(rwb[1]), in1=b4(dn), op=ALU.mult), reads=RWA, writes=[comb])

        GU = [B[2], B[3], B[4], B[5]]
        YD = [[B[6], B[7]], [B[0], B[1]]]
        ydi = 0
        for ei in range(N_EXPERTS_RUN if STOP_AFTER == 0 else 0):
            slot = ei % 2
            if ei + 1 < N_EXPERTS_RUN:
                load_expert(ei + 1)
            wgt = wg[slot]; wdt = wd[slot]
            for G in range(4):
                sgt = sg[G % 2]; at = actT[G % 2]
                for part in range(4):
                    bk = GU[part]
                    c0 = (part // 2) * 256 + (part % 2)
                    for kt in range(8):
                        op("pe", lambda e, kt=kt, bk=bk, c0=c0, G=G, wgt=wgt: e.matmul(
                            bk[:, :], lhsT=wgt[:, kt, c0:c0 + 255:2], rhs=h2T[:, kt, G * 512:(G + 1) * 512],
                            start=(kt == 0), stop=(kt == 7)), reads=[wgt, h2T], writes=[bk])
                for ft in range(2):
                    op("act", lambda e, ft=ft, sgt=sgt: e.activation(out=sgt[:, ft, :], in_=GU[ft][:, :], func=AF.Silu),
                       reads=[GU[ft]], writes=[sgt])
                    op("dve", lambda e, ft=ft, sgt=sgt, at=at: e.tensor_tensor(out=at[:, ft, :], in0=GU[2 + ft][:, :], in1=sgt[:, ft, :],
                                                                              op=ALU.mult), reads=[GU[2 + ft], sgt], writes=[at])
                for tt in range(4):
                    jt = G * 4 + tt
                    yb = YD[ydi % 2]; ydi += 1
                    for half in range(2):
                        bk = yb[half]
                        for ft in range(2):
                            op("pe", lambda e, ft=ft, half=half, bk=bk, tt=tt, at=at, wdt=wdt: e.matmul(
                                bk[:, :], lhsT=at[:, ft, tt * 128:(tt + 1) * 128], rhs=wdt[:, ft, half * 512:(half + 1) * 512],
                                start=(ft == 0), stop=(ft == 1)), reads=[at, wdt], writes=[bk])
                        op("dve", lambda e, half=half, bk=bk, jt=jt, ei=ei: e.scalar_tensor_tensor(
                            out=xres_t[:, jt, half * 512:(half + 1) * 512], in0=bk[:, :], scalar=comb[:, jt, ei:ei + 1],
                            in1=xres_t[:, jt, half * 512:(half + 1) * 512], op0=ALU.mult, op1=ALU.add),
                           reads=[bk, comb], writes=[xres_b[jt]])
        for j in range(NMAIN):
            op("sp", lambda e, j=j: e.dma_start(out=out_d[j * 128:(j + 1) * 128, :], in_=xres_t[:, j, :]), reads=[xres_b[j]], dma=True)
        S.run()
        print('[sched] ops=%d sim_makespan=%.1f us' % (len(S.all), getattr(S, 'sim_makespan', 0.0)))
    return nc


_CONST = {}


def _consts():
    if not _CONST:
        i = np.arange(128)
        tri = (i[:, None] <= i[None, :]).astype(np.float32)
        slm = (i[:, None] > i[None, :]).astype(np.float32)
        ones = np.ones((128, 128), np.float32)
        invf = (500000.0 ** (-np.arange(8, dtype=np.float32) * 2.0 / 16.0)).astype(np.float32)
        cf32 = np.concatenate([tri, slm, ones, np.broadcast_to(invf, (128, 8))], axis=1).astype(np.float32)
        ident = np.eye(128, dtype=np.float32)
        cbf = np.concatenate([ident, tri, slm], axis=1).astype(ml_dtypes.bfloat16)
        _CONST["cf32"] = np.ascontiguousarray(cf32)
        _CONST["cbf"] = np.ascontiguousarray(cbf)
    return _CONST


_NC_CACHE = {}


def kernel(x, c, positions, norm1_w, norm2_w, w_ada, b_ada, w_in, conv_w, conv_b, dt_bias, a_log, d_skip,
           ssd_norm_w, q_norm_w, k_norm_w, sinks, w_out, w_group, b_group, w_expert, b_expert, w_gate, w_up, w_down):
    f32 = lambda a: np.ascontiguousarray(np.asarray(a, dtype=np.float32))
    x = f32(x); c = f32(c)
    positions = np.ascontiguousarray(np.asarray(positions, dtype=np.int32))
    cst = _consts()
    shared = {
        "cf32": cst["cf32"], "cbf": cst["cbf"],
        "norm1_w": f32(norm1_w), "norm2_w": f32(norm2_w), "w_ada": f32(w_ada), "b_ada": f32(b_ada),
        "w_in": f32(w_in), "conv_w": f32(conv_w), "conv_b": f32(conv_b), "dt_bias": f32(dt_bias),
        "a_log": f32(a_log), "d_skip": f32(d_skip), "ssd_norm_w": f32(ssd_norm_w), "q_norm_w": f32(q_norm_w),
        "k_norm_w": f32(k_norm_w), "sinks": f32(sinks), "w_out": f32(w_out), "w_group": f32(w_group),
        "b_group": f32(b_group), "w_expert": f32(w_expert), "b_expert": f32(b_expert),
        "w_gate": f32(w_gate[:N_EXPERTS_RUN]), "w_up": f32(w_up[:N_EXPERTS_RUN]), "w_down": f32(w_down[:N_EXPERTS_RUN]),
    }
    in_maps = []
    SEQ = x.shape[1]
    for core in range(NCORES):
        b, q = divmod(core, 4)
        t0 = q * NMAIN * 128 - NPRE * 128
        xe = np.zeros((NSLOT * 128, D), np.float32)
        flg = np.zeros((128, NSLOT), np.float32)
        posi = np.zeros((128, NMAIN + 1), np.int32)
        for s in range(NSLOT):
            ts = t0 + s * 128
            if ts >= 0:
                xe[s * 128:(s + 1) * 128] = x[b, ts:ts + 128]
                flg[:, s] = 1.0
                if s >= HALO:
                    posi[:, s - HALO] = positions[b, ts:ts + 128]
        m = dict(shared)
        m["xe"] = xe; m["flags"] = flg; m["posi"] = posi
        m["cvec"] = np.ascontiguousarray(c[b].reshape(128, 8))

        in_maps.append(m)
    if "nc" not in _NC_CACHE:
        _NC_CACHE["nc"] = build_program()
    nc = _NC_CACHE["nc"]
    res = run_bass_kernel_spmd(nc, in_maps, core_ids=list(range(NCORES)))
    out = np.empty((x.shape[0], SEQ, D), np.float32)
    for core in range(NCORES):
        b, q = divmod(core, 4)
        out[b, q * 2048:(q + 1) * 2048] = res.results[core]["out"]
    kernel.last_results = res
    return out
```
